# Optimizing a Trainium2 kernel written in Bass

```python
import math
import jax, jax.numpy as jnp
from jax import lax
import numpy as np

D_MODEL = 1024
BATCH = 8
SEQ = 8192
DEPTH = 2

CHUNK = 64
N_META = 16
N_MIXERS = 2
EPS = 1e-6
GLA_HEADS = 4
GLA_KEY = D_MODEL // 2
GLA_DK = GLA_KEY // GLA_HEADS
GLA_DV = D_MODEL // GLA_HEADS
GLA_GATE_RANK = 16
GLA_TAU = 16.0
GLA_IN = 2 * GLA_KEY + 2 * D_MODEL
ATT_HEADS = 8
ATT_HEAD_DIM = D_MODEL // ATT_HEADS
KV_LATENT = 256
IDX_HEADS = 8
IDX_DIM = 64
TOPK_MAX = 256
Q_BLOCK = 128
DSA_IN = ATT_HEADS * ATT_HEAD_DIM + KV_LATENT + IDX_HEADS * IDX_DIM + IDX_DIM + IDX_HEADS
REL_BUCKETS = 32
REL_MAX_DIST = 128
D_FF = -(-(8 * D_MODEL) // (3 * 256)) * 256
N_GLA = (DEPTH + 1) // 2
N_DSA = DEPTH // 2

kernel_name = "hybrid_gla_dsa_streaming_trunk"


def rmsnorm(x, g):
    xf = x.astype(jnp.float32)
    y = xf * lax.rsqrt(jnp.mean(xf * xf, axis=-1, keepdims=True) + EPS) * g.astype(jnp.float32)
    return y.astype(x.dtype)


def chunk_ids(pos):
    return jnp.where(pos < N_META, 0, 1 + (pos - N_META) // CHUNK)


def rel_bucket(rel):
    nb = REL_BUCKETS // 2
    max_exact = nb // 2
    ret = jnp.where(rel > 0, nb, 0)
    n = jnp.abs(rel)
    nf = jnp.maximum(n, 1).astype(jnp.float32)
    large = max_exact + (jnp.log(nf / max_exact) / math.log(REL_MAX_DIST / max_exact)
                         * (nb - max_exact)).astype(jnp.int32)
    large = jnp.minimum(large, nb - 1)
    return ret + jnp.where(n < max_exact, n, large)


def gla_mixer(h, w_in, w_a1, w_a2, b_a, g_out, w_out):
    B, T, _ = h.shape
    proj = h @ w_in
    q, k, v, r = jnp.split(proj, [GLA_KEY, 2 * GLA_KEY, 2 * GLA_KEY + D_MODEL], axis=-1)
    log_a = jax.nn.log_sigmoid((h @ w_a1) @ w_a2 + b_a).astype(jnp.float32) / GLA_TAU
    n_pad = CHUNK - N_META
    Tp = T + n_pad
    nc = Tp // CHUNK

    def to_chunks(a, d):
        a = jnp.pad(a, ((0, 0), (n_pad, 0), (0, 0)))
        return a.reshape(B, nc, CHUNK, GLA_HEADS, d).transpose(1, 0, 3, 2, 4)

    qc = to_chunks(q * GLA_DK ** -0.5, GLA_DK)
    kc = to_chunks(k, GLA_DK)
    vc = to_chunks(v, GLA_DV)
    gc = to_chunks(log_a, GLA_DK)

    def step(S, inp):
        qi, ki, vi, gi = inp
        cum = jnp.cumsum(gi, axis=2)
        tot = cum[:, :, -1:, :]
        S = jnp.exp(tot[:, :, 0, :, None]) * S + jnp.einsum(
            'bhcd,bhce->bhde', ki.astype(jnp.float32) * jnp.exp(tot - cum), vi.astype(jnp.float32))
        o = jnp.einsum('bhcd,bhde->bhce', qi.astype(jnp.float32), S)
        return S, o

    S0 = jnp.zeros((B, GLA_HEADS, GLA_DK, GLA_DV), jnp.float32)
    _, o = lax.scan(step, S0, (qc, kc, vc, gc))
    o = o.transpose(1, 0, 3, 2, 4).reshape(B, Tp, GLA_HEADS, GLA_DV)[:, n_pad:]
    o = rmsnorm(o, g_out.reshape(GLA_HEADS, GLA_DV)).astype(h.dtype).reshape(B, T, D_MODEL)
    return (o * jax.nn.silu(r)) @ w_out


def dsa_mixer(h, w_in, g_kv, w_uk, w_uv, w_out, rel_bias):
    B, T, _ = h.shape
    s1 = ATT_HEADS * ATT_HEAD_DIM
    s2 = s1 + KV_LATENT
    s3 = s2 + IDX_HEADS * IDX_DIM
    s4 = s3 + IDX_DIM
    proj = h @ w_in
    q, c, q_idx, k_idx, w_idx = jnp.split(proj, [s1, s2, s3, s4], axis=-1)
    c = rmsnorm(c, g_kv)
    q = q.reshape(B, T, ATT_HEADS, ATT_HEAD_DIM)
    q_idx = q_idx.reshape(B, T, IDX_HEADS, IDX_DIM)
    w_idx = w_idx * IDX_HEADS ** -0.5
    n_sel = min(TOPK_MAX, SEQ // 4)
    nblk = -(-T // Q_BLOCK)
    Tq = nblk * Q_BLOCK

    def blocks(a):
        a = jnp.pad(a, [(0, 0), (0, Tq - T)] + [(0, 0)] * (a.ndim - 2))
        return jnp.moveaxis(a.reshape(B, nblk, Q_BLOCK, *a.shape[2:]), 1, 0)

    key_chunk = chunk_ids(jnp.arange(T))

    def attend(args):
        qb, qib, wb, start = args
        qpos = start + jnp.arange(Q_BLOCK)
        q_chunk = chunk_ids(qpos)
        s_idx = jnp.einsum('bqhd,bsd->bqhs', qib, k_idx) * IDX_DIM ** -0.5
        score = jnp.einsum('bqhs,bqh->bqs', jax.nn.relu(s_idx), wb).astype(jnp.float32)
        admissible = key_chunk[None, :] <= q_chunk[:, None]
        score = jnp.where(admissible[None], score, -jnp.inf)
        top_val, top_idx = lax.top_k(score, n_sel)
        valid = jnp.isfinite(top_val)
        c_sel = jax.vmap(lambda cb, ib: cb[ib])(c, top_idx)
        q_lat = jnp.einsum('bqhd,hcd->bqhc', qb, w_uk)
        logits = jnp.einsum('bqhc,bqkc->bhqk', q_lat, c_sel).astype(jnp.float32) * ATT_HEAD_DIM ** -0.5
        bias = rel_bias[rel_bucket(top_idx - qpos[None, :, None])]
        logits = logits + jnp.moveaxis(bias, -1, 1).astype(jnp.float32)
        logits = jnp.where(valid[:, None], logits, -jnp.inf)
        p = jax.nn.softmax(logits, axis=-1).astype(c.dtype)
        u = jnp.einsum('bhqk,bqkc->bqhc', p, c_sel)
        return jnp.einsum('bqhc,hcd->bqhd', u, w_uv)

    starts = jnp.arange(nblk, dtype=jnp.int32) * Q_BLOCK
    o = lax.map(attend, (blocks(q), blocks(q_idx), blocks(w_idx), starts))
    o = jnp.moveaxis(o, 0, 1).reshape(B, Tq, ATT_HEADS * ATT_HEAD_DIM)[:, :T]
    return o @ w_out


def swiglu(h, w_in, w_out):
    gate, up = jnp.split(h @ w_in, 2, axis=-1)
    return (jax.nn.silu(gate) * up) @ w_out


def setup_inputs(seed: int = 0) -> dict:
    key = jax.random.key(seed)
    ks = jax.random.split(key, 20)
    nrm = lambda k, shape, fan_in: jax.random.normal(k, shape, jnp.float32) * fan_in ** -0.5
    gain = lambda k, shape: 1.0 + 0.05 * jax.random.normal(k, shape, jnp.float32)
    return {
        "x": jax.random.normal(ks[0], (BATCH, SEQ, D_MODEL), jnp.float32),
        "meta": jax.random.normal(ks[1], (N_META, D_MODEL), jnp.float32),
        "norm_mix": gain(ks[2], (DEPTH, D_MODEL)),
        "norm_ffn": gain(ks[3], (DEPTH, D_MODEL)),
        "norm_final": gain(ks[4], (D_MODEL,)),
        "gla_w_in": nrm(ks[5], (N_GLA, D_MODEL, GLA_IN), D_MODEL),
        "gla_w_a1": nrm(ks[6], (N_GLA, D_MODEL, GLA_GATE_RANK), D_MODEL),
        "gla_w_a2": nrm(ks[7], (N_GLA, GLA_GATE_RANK, GLA_KEY), GLA_GATE_RANK),
        "gla_b_a": 0.1 * jax.random.normal(ks[8], (N_GLA, GLA_KEY), jnp.float32),
        "gla_g_out": gain(ks[9], (N_GLA, D_MODEL)),
        "gla_w_out": nrm(ks[10], (N_GLA, D_MODEL, D_MODEL), D_MODEL),
        "dsa_w_in": nrm(ks[11], (N_DSA, D_MODEL, DSA_IN), D_MODEL),
        "dsa_g_kv": gain(ks[12], (N_DSA, KV_LATENT)),
        "dsa_w_uk": nrm(ks[13], (N_DSA, ATT_HEADS, KV_LATENT, ATT_HEAD_DIM), KV_LATENT),
        "dsa_w_uv": nrm(ks[14], (N_DSA, ATT_HEADS, KV_LATENT, ATT_HEAD_DIM), KV_LATENT),
        "dsa_w_out": nrm(ks[15], (N_DSA, ATT_HEADS * ATT_HEAD_DIM, D_MODEL), ATT_HEADS * ATT_HEAD_DIM),
        "rel_bias": 0.5 * jax.random.normal(ks[16], (REL_BUCKETS, ATT_HEADS), jnp.float32),
        "ffn_w_in": nrm(ks[17], (DEPTH, D_MODEL, 2 * D_FF), D_MODEL),
        "ffn_w_out": nrm(ks[18], (DEPTH, D_FF, D_MODEL), D_FF),
    }


def reference(x, meta, norm_mix, norm_ffn, norm_final, gla_w_in, gla_w_a1, gla_w_a2, gla_b_a,
              gla_g_out, gla_w_out, dsa_w_in, dsa_g_kv, dsa_w_uk, dsa_w_uv, dsa_w_out, rel_bias,
              ffn_w_in, ffn_w_out):
    B = x.shape[0]
    h = jnp.concatenate(
        [jnp.broadcast_to(meta[None].astype(x.dtype), (B, N_META, D_MODEL)), x], axis=1)
    for i in range(DEPTH):
        j = i // N_MIXERS
        hn = rmsnorm(h, norm_mix[i])
        if i % N_MIXERS == 0:
            h = h + gla_mixer(hn, gla_w_in[j], gla_w_a1[j], gla_w_a2[j], gla_b_a[j],
                              gla_g_out[j], gla_w_out[j])
        else:
            h = h + dsa_mixer(hn, dsa_w_in[j], dsa_g_kv[j], dsa_w_uk[j], dsa_w_uv[j],
                              dsa_w_out[j], rel_bias)
        h = h + swiglu(rmsnorm(h, norm_ffn[i]), ffn_w_in[i], ffn_w_out[i])
    return rmsnorm(h, norm_final)[:, N_META:]
```

```python
import contextlib
import numpy as np
import concourse.bass as bass
import concourse.mybir as mybir
from concourse.bass_utils import run_bass_kernel_spmd

F32 = mybir.dt.float32
BF16 = mybir.dt.bfloat16
AF = mybir.ActivationFunctionType
ALU = mybir.AluOpType

D = 1024
DFF = 2816
EPS = 1e-6
P = 128
NEG = -30000.0

ENGS = ("pe", "act", "dve", "pool", "sp")
LIMIT = [10 ** 9]
LAST = {}
UID = [0]
DEBUG = False


class Buf:
    __slots__ = ("name", "last_w", "readers")

    def __init__(self, name):
        self.name = name
        self.last_w = None
        self.readers = {}


class Prog:
    NSLOT = 6

    def __init__(self, nc):
        self.nc = nc
        self.streams = {e: [] for e in ENGS}
        self.sems = {}
        self.cnt = {}
        for e in ("pe", "act", "dve", "pool"):
            self.sems[e] = nc.alloc_semaphore("s_" + e)
            self.cnt[e] = 0
        self.slots = {}
        self.slot_rr = {}
        for e in ("sp", "pool", "act"):
            self.slots[e] = []
            for i in range(self.NSLOT):
                k = "d_%s%d" % (e, i)
                self.sems[k] = nc.alloc_semaphore(k)
                self.cnt[k] = 0
                self.slots[e].append(k)
            self.slot_rr[e] = 0
        self.waited = {}
        self.pending = {e: False for e in ENGS}
        self.nops = 0

    def _wait(self, eng, tok):
        if tok is None:
            return
        teng, key, val = tok
        if self.waited.get((eng, key), 0) >= val:
            return
        self.waited[(eng, key)] = val
        self.streams[eng].append(("wait", key, val))

    def _deps(self, eng, reads, writes):
        for b in reads:
            t = b.last_w
            if t is not None:
                if t[0] == eng and t[1] == eng and eng == "pe":
                    continue
                self._wait(eng, t)
        for b in writes:
            t = b.last_w
            if t is not None and not (t[0] == eng and t[1] == eng):
                self._wait(eng, t)
            for re, rt in b.readers.items():
                if re == eng and rt[1] == eng:
                    continue
                self._wait(eng, rt)

    def _mark(self, eng, tok, reads, writes):
        for b in reads:
            old = b.readers.get(eng)
            if old is None or old[1] != tok[1] or old[2] < tok[2]:
                if old is not None and old[1] != tok[1]:
                    pass
                b.readers[eng] = tok
        for b in writes:
            b.last_w = tok
            b.readers = {}

    def op(self, eng, fn, reads=(), writes=(), signal=True):
        if self.nops >= LIMIT[0]:
            if not (eng == "pe" and self.pending["pe"]):
                return None
        self._deps(eng, reads, writes)
        if signal:
            self.cnt[eng] += 1
            tok = (eng, eng, self.cnt[eng])
            self.pending[eng] = False
        else:
            tok = (eng, eng, self.cnt[eng] + 1)
            self.pending[eng] = True
        self.streams[eng].append(("op", fn, eng if signal else None, 1))
        if DEBUG:
            import sys as _s
            LAST.setdefault("log", []).append((self.nops, eng, _s._getframe(1).f_lineno))
        self._mark(eng, tok, reads, writes)
        self.nops += 1
        return tok

    def dma(self, eng, fn, reads=(), writes=()):
        if self.nops >= LIMIT[0]:
            return None
        self._deps(eng, reads, writes)
        for b in reads:
            old = b.readers.get(eng)
            if old is not None and old[1] != eng:
                self._wait(eng, old)
        slot = self.slots[eng][self.slot_rr[eng] % self.NSLOT]
        self.slot_rr[eng] += 1
        if self.cnt[slot] > 0:
            self._wait(eng, (eng, slot, self.cnt[slot]))
        self.cnt[slot] += 16
        tok = (eng, slot, self.cnt[slot])
        self.streams[eng].append(("op", fn, slot, 16))
        if DEBUG:
            import sys as _s
            LAST.setdefault("log", []).append((self.nops, eng + "-dma", _s._getframe(1).f_lineno))
        self._mark(eng, tok, reads, writes)
        self.nops += 1
        return tok

    def wait_tok(self, eng, tok):
        self._wait(eng, tok)

    def barrier(self, bufs=()):
        toks = []
        for e in ("pe", "act", "dve", "pool"):
            if self.cnt[e] > 0:
                toks.append((e, e, self.cnt[e]))
        for e, sl in self.slots.items():
            for k in sl:
                if self.cnt[k] > 0:
                    toks.append((e, k, self.cnt[k]))
        for e in ENGS:
            for t in toks:
                self._wait(e, t)

    def emit(self):
        nc = self.nc
        for e in ENGS:
            assert not self.pending[e], "unsignalled trailing op on " + e
        streams = self.streams
        sems = self.sems

        def run(engobj, lst):
            for it in lst:
                if it[0] == "wait":
                    engobj.wait_ge(sems[it[1]], it[2])
                else:
                    ins = it[1](engobj)
                    if it[2] is not None:
                        ins.then_inc(sems[it[2]], it[3])

        with nc.Block() as block:
            @block.tensor
            def _(e):
                run(e, streams["pe"])

            @block.scalar
            def _(e):
                run(e, streams["act"])

            @block.vector
            def _(e):
                run(e, streams["dve"])

            @block.gpsimd
            def _(e):
                run(e, streams["pool"])

            @block.sync
            def _(e):
                run(e, streams["sp"])


class Ctx:
    def __init__(self, nc, pg, stack):
        self.nc, self.pg, self.stack = nc, pg, stack
        self.n = 0

    def sb(self, shape, dt, name=None):
        UID[0] += 1
        t = self.stack.enter_context(self.nc.sbuf_tensor("%s_%d" % (name or "t", UID[0]), list(shape), dt))
        return t

    def ps(self, shape, dt, name=None):
        UID[0] += 1
        t = self.stack.enter_context(self.nc.psum_tensor("%s_%d" % (name or "p", UID[0]), list(shape), dt))
        return t


def load_weight(cx, dst, dst_buf, src, KC, N, gain=None, stage=None, rr=[0]):
    pg = cx.pg
    CB = 1024
    srcv = src.rearrange("(k p) n -> p k n", p=P)
    for k in range(KC):
        for c0 in range(0, N, CB):
            cw = min(CB, N - c0)
            st, stb = stage[rr[0] % len(stage)]
            pg.dma("sp", lambda e, st=st, k=k, c0=c0, cw=cw: e.dma_start(out=st[:, 0:cw], in_=srcv[:, k, c0:c0 + cw]),
                   writes=[stb])
            which = rr[0] % 2
            rr[0] += 1
            o = dst[:, k, c0:c0 + cw]
            i = st[:, 0:cw]
            if gain is None:
                if which == 0:
                    pg.op("dve", lambda e, o=o, i=i: e.tensor_copy(out=o, in_=i), reads=[stb], writes=[dst_buf])
                elif which == 1:
                    pg.op("act", lambda e, o=o, i=i: e.copy(out=o, in_=i), reads=[stb], writes=[dst_buf])
                else:
                    pg.op("pool", lambda e, o=o, i=i: e.tensor_copy(out=o, in_=i), reads=[stb], writes=[dst_buf])
            else:
                g = gain[:, k:k + 1]
                if which == 0:
                    pg.op("dve", lambda e, o=o, i=i, g=g: e.tensor_scalar(out=o, in0=i, scalar1=g, scalar2=None, op0=ALU.mult),
                          reads=[stb], writes=[dst_buf])
                elif which == 1:
                    pg.op("act", lambda e, o=o, i=i, g=g: e.activation(out=o, in_=i, func=AF.Copy, scale=g),
                          reads=[stb], writes=[dst_buf])
                else:
                    pg.op("pool", lambda e, o=o, i=i, g=g: e.tensor_scalar(out=o, in0=i, scalar1=g, scalar2=None, op0=ALU.mult),
                          reads=[stb], writes=[dst_buf])


def rms_rstd(cx, x_ap, xbuf, junk, junkb, ss, ssb, rstd, rstdb, n):
    pg = cx.pg
    pg.op("act", lambda e: e.activation(out=junk, in_=x_ap, func=AF.Square, accum_out=ss),
          reads=[xbuf], writes=[junkb, ssb])
    pg.op("act", lambda e: e.activation(out=ss, in_=ss, func=AF.Sqrt, scale=1.0 / n, bias=cx.eps_ap),
          reads=[ssb], writes=[ssb])
    pg.op("dve", lambda e: e.reciprocal(out=rstd, in_=ss), reads=[ssb], writes=[rstdb])


def phase_ffn(nc, pg, NTT, h_in, h_out, w_in, w_out, gainT, gcol, final=None, tiles=None):
    G = 4
    KC = D // P
    FC = DFF // P
    with contextlib.ExitStack() as stack:
        cx = Ctx(nc, pg, stack)
        Win = cx.sb([P, KC, 2 * DFF], BF16, "Win"); Win_b = Buf("Win")
        Wout = cx.sb([P, FC, D], BF16, "Wout"); Wout_b = Buf("Wout")
        aT = cx.sb([P, FC, G * P], BF16, "aT"); aT_b = Buf("aT")
        stg_v = aT[:, :, :].rearrange("p f n -> p (f n)").bitcast(F32)
        stage = [(stg_v[:, i * 1024:(i + 1) * 1024], Buf("stg%d" % i)) for i in range(5)]
        gT = cx.sb([P, gainT.shape[1]], F32, "gT"); gT_b = Buf("gT")
        ident = cx.sb([P, P], BF16, "ident"); ident_b = Buf("ident")
        identf = cx.sb([P, P], F32, "identf"); identf_b = Buf("identf")
        epsT = cx.sb([P, 1], F32, "eps"); eps_b = Buf("eps")
        cx.eps_ap = epsT[:, 0:1]
        pg.op("pool", lambda e: e.memset(epsT[:], EPS), writes=[eps_b])
        pg.dma("sp", lambda e: e.dma_start(out=gT[:], in_=gainT[:, :]), writes=[gT_b])
        pg.dma("sp", lambda e: e.dma_start(out=identf[:], in_=cx_ident(nc)[:, :]), writes=[identf_b])
        pg.op("dve", lambda e: e.tensor_copy(out=ident[:], in_=identf[:]), reads=[identf_b], writes=[ident_b])
        pg.wait_tok("dve", gT_b.last_w); pg.wait_tok("act", gT_b.last_w); pg.wait_tok("pool", gT_b.last_w)
        pg.wait_tok("act", eps_b.last_w)
        load_weight(cx, Win, Win_b, w_in, KC, 2 * DFF, gain=gT[:, gcol:gcol + KC], stage=stage)
        load_weight(cx, Wout, Wout_b, w_out, FC, D, gain=None, stage=stage)
        if final is not None:
            gfin = cx.sb([P, D], F32, "gfin"); gfin_b = Buf("gfin")
            pg.dma("sp", lambda e: e.dma_start(out=gfin[:], in_=final["gB"][:, :]), writes=[gfin_b])

        NB = 2
        hin = [(cx.sb([P, G, D], F32, "hin"), Buf("hin")) for _ in range(NB)]
        hn = [(cx.sb([P, D], BF16, "hn"), Buf("hn")) for _ in range(2)]
        ss = [(cx.sb([P, 1], F32, "ss"), Buf("ss")) for _ in range(2)]
        rstd = [(cx.sb([P, 1], F32, "rstd"), Buf("rstd")) for _ in range(2)]
        hnT = cx.sb([P, KC, G * P], BF16, "hnT"); hnT_b = Buf("hnT")
        sg = [(cx.sb([P, G * P], F32, "sg"), Buf("sg")) for _ in range(2)]
        junk_t = sg[0][0][:, :].bitcast(BF16); junk_b = sg[0][1]
        pT = [(cx.ps([P, KC, P], BF16, "pT"), Buf("pT")) for _ in range(2)]
        pg_ = [(cx.ps([P, 512], F32, "pg"), Buf("pg")) for _ in range(2)]
        pu_ = [(cx.ps([P, 512], F32, "pu"), Buf("pu")) for _ in range(2)]
        po_ = [(cx.ps([P, 512], F32, "po"), Buf("po")) for _ in range(2)]

        groups = []
        t = 0
        while t < NTT:
            g = min(G, NTT - t)
            groups.append((t, g))
            t += g
        cnt = {"t": 0, "c": 0, "o": 0}

        def front(gi):
            t0, g = groups[gi]
            hi, hi_b = hin[gi % NB]
            pg.dma("sp", lambda e: e.dma_start(
                out=hi[:, 0:g, :], in_=h_in[t0 * P:(t0 + g) * P, :].rearrange("(g p) d -> p g d", p=P)),
                writes=[hi_b])
            for j in range(g):
                x_ap = hi[:, j, :]
                s_, s_b = ss[cnt["t"] % 2]; r_, r_b = rstd[cnt["t"] % 2]
                h_, h_b = hn[cnt["t"] % 2]; p_, p_b = pT[cnt["t"] % 2]
                cnt["t"] += 1
                rms_rstd(cx, x_ap, hi_b, junk_t, junk_b, s_[:, 0:1], s_b, r_[:, 0:1], r_b, D)
                pg.op("act", lambda e, h_=h_, x_ap=x_ap, r_=r_: e.activation(out=h_[:], in_=x_ap, func=AF.Copy, scale=r_[:, 0:1]),
                      reads=[hi_b, r_b], writes=[h_b])
                for k in range(KC):
                    pg.op("pe", lambda e, p_=p_, h_=h_, k=k: e.transpose(out=p_[:, k, :], in_=h_[:, k * P:(k + 1) * P], identity=ident[:]),
                          reads=[h_b, ident_b], writes=[p_b], signal=(k == KC - 1))
                pg.op("dve", lambda e, p_=p_, j=j: e.tensor_copy(out=hnT[:, :, j * P:(j + 1) * P], in_=p_[:]),
                      reads=[p_b], writes=[hnT_b])

        def mm1(gi):
            t0, g = groups[gi]
            N = g * P
            for i in range(FC):
                pgt, pg_b = pg_[cnt["c"] % 2]; put, pu_b = pu_[cnt["c"] % 2]
                sgt, sg_b = sg[cnt["c"] % 2]
                cnt["c"] += 1
                for k in range(KC):
                    pg.op("pe", lambda e, pgt=pgt, k=k, i=i: e.matmul(
                        pgt[:, 0:N], lhsT=Win[:, k, i * P:(i + 1) * P], rhs=hnT[:, k, 0:N], start=(k == 0), stop=(k == KC - 1)),
                        reads=[Win_b, hnT_b], writes=[pg_b], signal=(k == KC - 1))
                for k in range(KC):
                    pg.op("pe", lambda e, put=put, k=k, i=i: e.matmul(
                        put[:, 0:N], lhsT=Win[:, k, DFF + i * P:DFF + (i + 1) * P], rhs=hnT[:, k, 0:N], start=(k == 0), stop=(k == KC - 1)),
                        reads=[Win_b, hnT_b], writes=[pu_b], signal=(k == KC - 1))
                pg.op("act", lambda e, sgt=sgt, pgt=pgt: e.activation(out=sgt[:, 0:N], in_=pgt[:, 0:N], func=AF.Silu),
                      reads=[pg_b], writes=[sg_b])
                pg.op("dve", lambda e, sgt=sgt, put=put, i=i: e.tensor_tensor(out=aT[:, i, 0:N], in0=sgt[:, 0:N], in1=put[:, 0:N], op=ALU.mult),
                      reads=[sg_b, pu_b], writes=[aT_b])

        def mm2(gi):
            t0, g = groups[gi]
            hi, hi_b = hin[gi % NB]
            for j in range(g):
                for n in range(2):
                    pot, po_b = po_[cnt["o"] % 2]
                    cnt["o"] += 1
                    for i in range(FC):
                        pg.op("pe", lambda e, pot=pot, i=i, j=j, n=n: e.matmul(
                            pot[:, :], lhsT=aT[:, i, j * P:(j + 1) * P], rhs=Wout[:, i, n * 512:(n + 1) * 512], start=(i == 0), stop=(i == FC - 1)),
                            reads=[aT_b, Wout_b], writes=[po_b], signal=(i == FC - 1))
                    pg.op("dve", lambda e, j=j, n=n, pot=pot: e.tensor_tensor(
                        out=hi[:, j, n * 512:(n + 1) * 512], in0=hi[:, j, n * 512:(n + 1) * 512], in1=pot[:, :], op=ALU.add),
                        reads=[po_b, hi_b], writes=[hi_b])
            if final is None:
                pg.dma("sp", lambda e: e.dma_start(
                    out=h_out[t0 * P:(t0 + g) * P, :].rearrange("(g p) d -> p g d", p=P), in_=hi[:, 0:g, :]),
                    reads=[hi_b])
            else:
                for j in range(g):
                    tt = t0 + j
                    if tt == 0:
                        continue
                    x_ap = hi[:, j, :]
                    s_, s_b = ss[cnt["t"] % 2]; r_, r_b = rstd[cnt["t"] % 2]
                    cnt["t"] += 1
                    rms_rstd(cx, x_ap, hi_b, junk_t, junk_b, s_[:, 0:1], s_b, r_[:, 0:1], r_b, D)
                    pg.op("dve", lambda e, x_ap=x_ap, r_=r_: e.scalar_tensor_tensor(
                        out=x_ap, in0=x_ap, scalar=r_[:, 0:1], in1=gfin[:], op0=ALU.mult, op1=ALU.mult),
                        reads=[hi_b, r_b, gfin_b], writes=[hi_b])
                    pg.dma("sp", lambda e, x_ap=x_ap, tt=tt: e.dma_start(out=final["out"][(tt - 1) * P:tt * P, :], in_=x_ap),
                           reads=[hi_b])

        front(0)
        for gi in range(len(groups)):
            mm1(gi)
            if gi + 1 < len(groups):
                front(gi + 1)
            mm2(gi)
        pg.barrier()


_IDENT = {}


def cx_ident(nc):
    return _IDENT[id(nc)]


GAIN_COLS = {"mix0": 0, "ffn0": 8, "mix1": 16, "ffn1": 24, "gout": 32, "gkv": 40}


def build(SEQ, phases, ext_in=(), ext_out=()):
    nc = bass.Bass("TRN2", target_bir_lowering=False)
    NTT = 1 + SEQ // P
    R = NTT * P

    def dram(name, shape, dt, kind="Internal"):
        if name in ext_in:
            kind = "ExternalInput"
        elif name in ext_out:
            kind = "ExternalOutput"
        return nc.dram_tensor(name, list(shape), dt, kind=kind).ap()

    W = {}
    W["gainT"] = dram("gainT", [P, 48], F32, "ExternalInput")
    W["identf"] = dram("identf", [P, P], F32, "ExternalInput")
    _IDENT[id(nc)] = W["identf"]
    W["ffn_w_in0"] = dram("ffn_w_in0", [D, 2 * DFF], F32, "ExternalInput")
    W["ffn_w_out0"] = dram("ffn_w_out0", [DFF, D], F32, "ExternalInput")
    W["ffn_w_in1"] = dram("ffn_w_in1", [D, 2 * DFF], F32, "ExternalInput")
    W["ffn_w_out1"] = dram("ffn_w_out1", [DFF, D], F32, "ExternalInput")
    W["gfinB"] = dram("gfinB", [P, D], F32, "ExternalInput")
    W["gla_w_in"] = dram("gla_w_in", [D, 3072], F32, "ExternalInput")
    W["gla_w_a1"] = dram("gla_w_a1", [D, 16], F32, "ExternalInput")
    W["gla_w_a2aug"] = dram("gla_w_a2aug", [17, 512], F32, "ExternalInput")
    W["gla_w_out"] = dram("gla_w_out", [D, D], F32, "ExternalInput")
    consts = {}
    consts["Dm"] = dram("c_Dm", [P, 2, P], F32, "ExternalInput")
    consts["Ind"] = dram("c_Ind", [P, 2, 2], F32, "ExternalInput")
    x = dram("x", [SEQ, D], F32, "ExternalInput")
    meta = dram("meta", [16, D], F32, "ExternalInput")
    W["dsa_w_in"] = dram("dsa_w_in", [D, DSA_IN], F32, "ExternalInput")
    W["dsa_w_ukT"] = dram("dsa_w_ukT", [P, 2048], F32, "ExternalInput")
    W["dsa_w_uv"] = dram("dsa_w_uv", [8, 256, P], F32, "ExternalInput")
    W["dsa_w_out"] = dram("dsa_w_out", [D, D], F32, "ExternalInput")
    W["gkvB"] = dram("gkvB", [P, 256], F32, "ExternalInput")
    consts["m0"] = dram("c_m0", [P, P], F32, "ExternalInput")
    consts["mdiag"] = dram("c_mdiag", [P, P], F32, "ExternalInput")
    consts["b15"] = dram("c_b15", [P, 1024], F32, "ExternalInput")
    consts["pow2"] = dram("c_pow2", [P, 32], F32, "ExternalInput")
    consts["biasT"] = dram("c_biasT", [3, P, 1024], F32, "ExternalInput")
    scr = {}
    scr["qlat"] = dram("s_qlat", [NTT, P, 2048], BF16)
    scr["qi"] = dram("s_qi", [NTT, P, 1024], BF16)
    scr["oT"] = dram("s_oT", [NTT, P, 1024], BF16)
    h1 = dram("h1", [R, D], F32)
    h2 = dram("h2", [R, D], F32)
    h3 = dram("h3", [R, D], F32)
    out = dram("out", [SEQ, D], F32, "ExternalOutput" if "E" in phases else "Internal")

    pg = Prog(nc)
    if "A" in phases:
        phase_gla(nc, pg, NTT, x, meta, h1, W, consts)
    if "B" in phases:
        phase_ffn(nc, pg, NTT, h1, h2, W["ffn_w_in0"], W["ffn_w_out0"], W["gainT"], GAIN_COLS["ffn0"])
    if "C" in phases or "D" in phases:
        with contextlib.ExitStack() as cstack:
            ccx = Ctx(nc, pg, cstack)
            caches = {
                "kidx2": (ccx.sb([P, R], BF16, "kidx2"), Buf("kidx2")),
                "cT": (ccx.sb([P, 2, R], BF16, "cT"), Buf("cT")),
                "ctok": (ccx.sb([P, NTT, 258], BF16, "ctok"), Buf("ctok")),
                "wabs": (ccx.sb([P, NTT, 8], F32, "wabs"), Buf("wabs")),
                "sgn": (ccx.sb([P, NTT, 8], F32, "sgn"), Buf("sgn")),
            }
            if "C" in phases:
                phase_dsa_proj(nc, pg, NTT, h2, W, consts, caches, scr)
            if "D" in phases:
                phase_dsa_attn(nc, pg, NTT, W, consts, caches, scr)
    if "F" in phases:
        phase_dsa_out(nc, pg, NTT, h2, h3, W, scr)
    if "E" in phases:
        phase_ffn(nc, pg, NTT, h3, None, W["ffn_w_in1"], W["ffn_w_out1"], W["gainT"], GAIN_COLS["ffn1"],
                  final=dict(out=out, gB=W["gfinB"]))
    pg.emit()
    LAST['nops'] = pg.nops
    return nc


def phase_gla(nc, pg, NTT, x, meta, h_out, W, consts):
    KC = D // P
    H = 4
    with contextlib.ExitStack() as stack:
        cx = Ctx(nc, pg, stack)
        Win = cx.sb([P, KC, 3072], BF16, "Win"); Win_b = Buf("Win")
        Wa1 = cx.sb([P, KC, 16], BF16, "Wa1"); Wa1_b = Buf("Wa1")
        Wa2 = cx.sb([17, 512], F32, "Wa2"); Wa2_b = Buf("Wa2")
        Wout = cx.sb([P, KC, D], BF16, "Wout"); Wout_b = Buf("Wout")
        stage = [(cx.sb([P, 1024], F32, "stg"), Buf("stg")) for _ in range(5)]
        gT = cx.sb([P, 48], F32, "gT"); gT_b = Buf("gT")
        ident = cx.sb([P, P], BF16, "ident"); ident_b = Buf("ident")
        identf = cx.sb([P, P], F32, "identf"); identf_b = Buf("identf")
        Dm = cx.sb([P, 2, P], F32, "Dm"); Dm_b = Buf("Dm")
        Ind = cx.sb([P, 2, 2], F32, "Ind"); Ind_b = Buf("Ind")
        epsT = cx.sb([P, 1], F32, "eps"); eps_b = Buf("eps")
        cx.eps_ap = epsT[:, 0:1]
        pg.op("pool", lambda e: e.memset(epsT[:], EPS), writes=[eps_b])
        pg.dma("sp", lambda e: e.dma_start(out=gT[:], in_=W["gainT"][:, :]), writes=[gT_b])
        pg.dma("sp", lambda e: e.dma_start(out=identf[:], in_=W["identf"][:, :]), writes=[identf_b])
        pg.dma("sp", lambda e: e.dma_start(out=Dm[:], in_=consts["Dm"][:, :, :]), writes=[Dm_b])
        pg.dma("sp", lambda e: e.dma_start(out=Ind[:], in_=consts["Ind"][:, :, :]), writes=[Ind_b])
        pg.dma("sp", lambda e: e.dma_start(out=Wa2[:], in_=W["gla_w_a2aug"][:, :]), writes=[Wa2_b])
        pg.op("dve", lambda e: e.tensor_copy(out=ident[:], in_=identf[:]), reads=[identf_b], writes=[ident_b])
        for en in ("dve", "act", "pool"):
            pg.wait_tok(en, gT_b.last_w)
        pg.wait_tok("act", eps_b.last_w)
        g0 = GAIN_COLS["mix0"]
        load_weight(cx, Win, Win_b, W["gla_w_in"], KC, 3072, gain=gT[:, g0:g0 + KC], stage=stage)
        load_weight(cx, Wa1, Wa1_b, W["gla_w_a1"], KC, 16, gain=gT[:, g0:g0 + KC], stage=stage)
        go = GAIN_COLS["gout"]
        load_weight(cx, Wout, Wout_b, W["gla_w_out"], KC, D, gain=gT[:, go:go + KC], stage=stage)

        def dbl(shape, dt, nm_, n=2):
            return [(cx.sb(shape, dt, nm_), Buf(nm_)) for _ in range(n)]
        xin = dbl([P, D], F32, "xin", 3)
        hn = dbl([P, D], BF16, "hn")
        hnT = dbl([P, KC, P], BF16, "hnT")
        qT = dbl([P, H, P], F32, "qT")
        kt = dbl([P, 2, 512], F32, "kt")
        vsb = dbl([P, D], F32, "vsb")
        sr = dbl([P, D], F32, "sr")
        etot = dbl([P, H, 2], F32, "etot")
        junk = cx.sb([P, D], BF16, "junk"); junk_b = Buf("junk")
        ss = cx.sb([P, 1], F32, "ss"); ss_b = Buf("ss")
        rstd = cx.sb([P, 1], F32, "rstd"); rstd_b = Buf("rstd")
        ksb = cx.sb([P, 512], F32, "ksb"); ksb_b = Buf("ksb")
        mb = cx.sb([P, 2], F32, "mb"); mb_b = Buf("mb")
        pg.op("pool", lambda e: e.memset(mb[:], NEG), writes=[mb_b])
        pg.op("pool", lambda e: e.memset(mb[0:64, 0:1], 0.0), writes=[mb_b])
        pg.op("pool", lambda e: e.memset(mb[64:128, 1:2], 0.0), writes=[mb_b])
        pg.wait_tok("act", mb_b.last_w)
        a1T = cx.sb([17, P], F32, "a1T"); a1T_b = Buf("a1T")
        e1 = cx.sb([P, 512], F32, "e1"); e1_b = Buf("e1")
        sp_ = cx.sb([P, 512], F32, "sp"); sp_b = Buf("sp")
        ed = cx.sb([P, 2, 512], F32, "ed"); ed_b = Buf("ed")
        S = cx.sb([P, H, 256], F32, "S"); S_b = [Buf("S%d" % h) for h in range(H)]
        osb = cx.sb([P, H, 256], F32, "osb"); osb_b = Buf("osb")
        ss4 = cx.sb([P, H], F32, "ss4"); ss4_b = Buf("ss4")
        rs4 = cx.sb([P, H], F32, "rs4"); rs4_b = Buf("rs4")
        y = cx.sb([P, D], BF16, "y"); y_b = Buf("y")
        yT = cx.sb([P, KC, P], BF16, "yT"); yT_b = Buf("yT")

        def bank(nm_):
            t_ = cx.ps([P, 512], F32, nm_)
            return (t_, Buf(nm_))
        pT32, pT_b = bank("pT")
        pTv = pT32[:, :].bitcast(BF16).rearrange("p (k t) -> p k t", k=KC)
        RB = [bank("pR%d" % i) for i in range(3)]
        KVB = [bank("pK%d" % i) for i in range(2)]
        OB = [bank("pO%d" % i) for i in range(2)]
        rcnt = [0]

        def nextR():
            r = RB[rcnt[0] % 3]
            rcnt[0] += 1
            return r

        pg.op("pool", lambda e: e.memset(S[:], 0.0), writes=S_b)
        pg.op("pool", lambda e: e.memset(a1T[:], 1.0), writes=[a1T_b])

        def front(t):
            xi, xi_b = xin[t % 3]
            hn_, hn_b = hn[t % 2]
            hT, hT_b = hnT[t % 2]
            if t == 0:
                pg.op("pool", lambda e: e.memset(xi[:], 0.0), writes=[xi_b])
                pg.dma("sp", lambda e: e.dma_start(out=xi[48:64, :], in_=meta[:, :]), writes=[xi_b])
            else:
                pg.dma("sp", lambda e: e.dma_start(out=xi[:], in_=x[(t - 1) * P:t * P, :]), writes=[xi_b])
            rms_rstd(cx, xi[:], xi_b, junk[:], junk_b, ss[:, 0:1], ss_b, rstd[:, 0:1], rstd_b, D)
            pg.op("act", lambda e: e.activation(out=hn_[:], in_=xi[:], func=AF.Copy, scale=rstd[:, 0:1]),
                  reads=[xi_b, rstd_b], writes=[hn_b])
            for k in range(KC):
                pg.op("pe", lambda e, k=k: e.transpose(out=pTv[:, k, :], in_=hn_[:, k * P:(k + 1) * P], identity=ident[:]),
                      reads=[hn_b, ident_b], writes=[pT_b], signal=(k == KC - 1))
            pg.op("dve", lambda e: e.tensor_copy(out=hT[:], in_=pTv), reads=[pT_b], writes=[hT_b])

        def proj_groups(t):
            hT, hT_b = hnT[t % 2]
            q_, q_b = qT[t % 2]
            kt_, kt_b = kt[t % 2]
            v_, v_b = vsb[t % 2]
            sr_, sr_b = sr[t % 2]
            et_, et_b = etot[t % 2]
            ci = 1 if t == 0 else 0
            gs = []

            def g_q():
                pb, pb_b = nextR()
                for h in range(H):
                    for k in range(KC):
                        pg.op("pe", lambda e, h=h, k=k: e.matmul(pb[:, h * P:(h + 1) * P], lhsT=Win[:, k, h * P:(h + 1) * P], rhs=hT[:, k, :],
                                                                  start=(k == 0), stop=(k == KC - 1)),
                              reads=[Win_b, hT_b], writes=[pb_b], signal=(h == H - 1 and k == KC - 1))
                pg.op("act", lambda e: e.activation(out=q_[:].rearrange("p h t -> p (h t)"), in_=pb[:, :], func=AF.Copy, scale=float(128 ** -0.5)),
                      reads=[pb_b], writes=[q_b])
            gs.append(g_q)

            def mm512(col0, evac):
                def g():
                    pb, pb_b = nextR()
                    for k in range(KC):
                        pg.op("pe", lambda e, k=k: e.matmul(pb[:, :], lhsT=hT[:, k, :], rhs=Win[:, k, col0:col0 + 512], start=(k == 0), stop=(k == KC - 1)),
                              reads=[Win_b, hT_b], writes=[pb_b], signal=(k == KC - 1))
                    evac(pb, pb_b)
                return g
            gs.append(mm512(512, lambda pb, pb_b: pg.op("dve", lambda e: e.tensor_copy(out=ksb[:], in_=pb[:, :]), reads=[pb_b], writes=[ksb_b])))
            gs.append(mm512(1024, lambda pb, pb_b: pg.op("act", lambda e: e.copy(out=v_[:, 0:512], in_=pb[:, :]), reads=[pb_b], writes=[v_b])))
            gs.append(mm512(1536, lambda pb, pb_b: pg.op("dve", lambda e: e.tensor_copy(out=v_[:, 512:1024], in_=pb[:, :]), reads=[pb_b], writes=[v_b])))
            gs.append(mm512(2048, lambda pb, pb_b: pg.op("act", lambda e: e.activation(out=sr_[:, 0:512], in_=pb[:, :], func=AF.Silu), reads=[pb_b], writes=[sr_b])))
            gs.append(mm512(2560, lambda pb, pb_b: pg.op("act", lambda e: e.activation(out=sr_[:, 512:1024], in_=pb[:, :], func=AF.Silu), reads=[pb_b], writes=[sr_b])))

            def g_a1():
                pb, pb_b = nextR()
                for k in range(KC):
                    pg.op("pe", lambda e, k=k: e.matmul(pb[0:16, 0:P], lhsT=Wa1[:, k, :], rhs=hT[:, k, :], start=(k == 0), stop=(k == KC - 1)),
                          reads=[Wa1_b, hT_b], writes=[pb_b], signal=(k == KC - 1))
                pg.op("dve", lambda e: e.tensor_copy(out=a1T[0:16, :], in_=pb[0:16, 0:P]), reads=[pb_b], writes=[a1T_b])
            gs.append(g_a1)

            def g_z():
                pb, pb_b = nextR()
                pg.op("pe", lambda e: e.matmul(pb[:, :], lhsT=a1T[:, :], rhs=Wa2[:, :], start=True, stop=True), reads=[a1T_b, Wa2_b], writes=[pb_b])
                pg.op("act", lambda e: e.activation(out=e1[:], in_=pb[:, :], func=AF.Exp, scale=-1.0), reads=[pb_b], writes=[e1_b])
                pg.op("act", lambda e: e.activation(out=sp_[:], in_=e1[:], func=AF.Ln, bias=1.0), reads=[e1_b], writes=[sp_b])
            gs.append(g_z)

            def g_dec():
                pb, pb_b = nextR()
                pg.op("pe", lambda e: e.matmul(pb[:, :], lhsT=Dm[:, ci, :], rhs=sp_[:], start=True, stop=True), reads=[Dm_b, sp_b], writes=[pb_b])
                for ch in range(2):
                    pg.op("act", lambda e, ch=ch: e.activation(out=ed[:, ch, :], in_=pb[:, :], func=AF.Exp, bias=mb[:, ch:ch + 1]), reads=[pb_b], writes=[ed_b])
                for ch in range(2):
                    pg.op("dve", lambda e, ch=ch: e.tensor_tensor(out=kt_[:, ch, :], in0=ksb[:], in1=ed[:, ch, :], op=ALU.mult), reads=[ksb_b, ed_b], writes=[kt_b])
            gs.append(g_dec)

            def g_tot(h0):
                def g():
                    for h in (h0, h0 + 1):
                        pb, pb_b = nextR()
                        pg.op("pe", lambda e, h=h, pb=pb: e.matmul(pb[:, 0:2], lhsT=sp_[:, h * P:(h + 1) * P], rhs=Ind[:, ci, :], start=True, stop=True),
                              reads=[sp_b, Ind_b], writes=[pb_b])
                        pg.op("act", lambda e, h=h, pb=pb: e.activation(out=et_[:, h, :], in_=pb[:, 0:2], func=AF.Exp), reads=[pb_b], writes=[et_b])
                return g
            gs.append(g_tot(0))
            gs.append(g_tot(2))
            return gs

        def scan_steps(t):
            q_, q_b = qT[t % 2]
            kt_, kt_b = kt[t % 2]
            v_, v_b = vsb[t % 2]
            et_, et_b = etot[t % 2]
            steps = []
            for ch in range(1 if t == 0 else 2):
                r0, r1 = ch * 64, (ch + 1) * 64
                for h in range(H):
                    def sa(h=h, ch=ch):
                        kvp, kvb = KVB[h % 2]
                        pg.op("pe", lambda e: e.matmul(kvp[:, 0:256], lhsT=kt_[:, ch, h * P:(h + 1) * P], rhs=v_[:, h * 256:(h + 1) * 256], start=True, stop=True),
                              reads=[kt_b, v_b], writes=[kvb])
                        pg.op("dve", lambda e: e.scalar_tensor_tensor(out=S[:, h, :], in0=S[:, h, :], scalar=et_[:, h, ch:ch + 1], in1=kvp[:, 0:256],
                                                                       op0=ALU.mult, op1=ALU.add), reads=[kvb, et_b, S_b[h]], writes=[S_b[h]])

                    def sb_(h=h, r0=r0, r1=r1):
                        op_, op_b = OB[h % 2]
                        pg.op("pe", lambda e: e.matmul(op_[:, 0:256], lhsT=q_[:, h, :], rhs=S[:, h, :], start=True, stop=True),
                              reads=[q_b, S_b[h]], writes=[op_b])
                        if h % 2 == 0:
                            pg.op("act", lambda e: e.copy(out=osb[r0:r1, h, :], in_=op_[r0:r1, 0:256]), reads=[op_b], writes=[osb_b])
                        else:
                            pg.op("dve", lambda e: e.tensor_copy(out=osb[r0:r1, h, :], in_=op_[r0:r1, 0:256]), reads=[op_b], writes=[osb_b])
                    steps.append(sa)
                    steps.append(sb_)
            return steps

        def tail(t):
            xi, xi_b = xin[t % 3]
            sr_, sr_b = sr[t % 2]
            for h in range(H):
                pg.op("act", lambda e, h=h: e.activation(out=junk[:, 0:256], in_=osb[:, h, :], func=AF.Square, accum_out=ss4[:, h:h + 1]),
                      reads=[osb_b], writes=[junk_b, ss4_b])
            pg.op("act", lambda e: e.activation(out=ss4[:], in_=ss4[:], func=AF.Sqrt, scale=1.0 / 256, bias=cx.eps_ap), reads=[ss4_b], writes=[ss4_b])
            pg.op("dve", lambda e: e.reciprocal(out=rs4[:], in_=ss4[:]), reads=[ss4_b], writes=[rs4_b])
            for h in range(H):
                pg.op("dve", lambda e, h=h: e.scalar_tensor_tensor(
                    out=y[:, h * 256:(h + 1) * 256], in0=osb[:, h, :], scalar=rs4[:, h:h + 1], in1=sr_[:, h * 256:(h + 1) * 256],
                    op0=ALU.mult, op1=ALU.mult), reads=[osb_b, rs4_b, sr_b], writes=[y_b])
            for k in range(KC):
                pg.op("pe", lambda e, k=k: e.transpose(out=pTv[:, k, :], in_=y[:, k * P:(k + 1) * P], identity=ident[:]),
                      reads=[y_b, ident_b], writes=[pT_b], signal=(k == KC - 1))
            pg.op("act", lambda e: e.copy(out=yT[:], in_=pTv), reads=[pT_b], writes=[yT_b])
            for n in range(2):
                pp, pp_b = OB[n]
                for k in range(KC):
                    pg.op("pe", lambda e, k=k, n=n, pp=pp: e.matmul(pp[:, :], lhsT=yT[:, k, :], rhs=Wout[:, k, n * 512:(n + 1) * 512],
                                                                    start=(k == 0), stop=(k == KC - 1)),
                          reads=[Wout_b, yT_b], writes=[pp_b], signal=(k == KC - 1))
                pg.op("dve", lambda e, n=n, pp=pp: e.tensor_tensor(out=xi[:, n * 512:(n + 1) * 512], in0=xi[:, n * 512:(n + 1) * 512], in1=pp[:, :], op=ALU.add),
                      reads=[pp_b, xi_b], writes=[xi_b])
            pg.dma("sp", lambda e: e.dma_start(out=h_out[t * P:(t + 1) * P, :], in_=xi[:]), reads=[xi_b])

        front(0)
        for g in proj_groups(0):
            g()
        for t in range(NTT):
            nxt = t + 1 < NTT
            if nxt:
                front(t + 1)
            A_ = scan_steps(t)
            B_ = proj_groups(t + 1) if nxt else []
            ia = ib = 0
            while ia < len(A_) or ib < len(B_):
                if ia < len(A_):
                    A_[ia](); ia += 1
                if ib < len(B_) and (ia % 2 == 1 or ia >= len(A_)):
                    B_[ib](); ib += 1
            tail(t)
        pg.barrier()


DSA_IN = 1864
IDX_C0 = float((64 ** -0.5) * (8 ** -0.5))


def phase_dsa_proj(nc, pg, NTT, h_in, W, consts, caches, scr):
    KC = D // P
    kidx2, kidx2_b = caches["kidx2"]
    cT, cT_b = caches["cT"]
    ctok, ctok_b = caches["ctok"]
    wabs, wabs_b = caches["wabs"]
    sgn, sgn_b = caches["sgn"]
    with contextlib.ExitStack() as stack:
        cx = Ctx(nc, pg, stack)
        NW = DSA_IN + 64
        Win = cx.sb([P, KC, NW], BF16, "Win"); Win_b = Buf("Win")
        Wuk = cx.sb([P, 1, 2048], BF16, "Wuk"); Wuk_b = Buf("Wuk")
        stage = [(cx.sb([P, 1024], F32, "stg"), Buf("stg")) for _ in range(5)]
        gT = cx.sb([P, 48], F32, "gT"); gT_b = Buf("gT")
        gkvB = cx.sb([P, 256], F32, "gkvB"); gkvB_b = Buf("gkvB")
        ident = cx.sb([P, P], BF16, "ident"); ident_b = Buf("ident")
        identf = cx.sb([P, P], F32, "identf"); identf_b = Buf("identf")
        epsT = cx.sb([P, 1], F32, "eps"); eps_b = Buf("eps")
        cx.eps_ap = epsT[:, 0:1]
        pg.op("pool", lambda e: e.memset(epsT[:], EPS), writes=[eps_b])
        pg.dma("sp", lambda e: e.dma_start(out=gT[:], in_=W["gainT"][:, :]), writes=[gT_b])
        pg.dma("sp", lambda e: e.dma_start(out=gkvB[:], in_=W["gkvB"][:, :]), writes=[gkvB_b])
        pg.dma("sp", lambda e: e.dma_start(out=identf[:], in_=W["identf"][:, :]), writes=[identf_b])
        pg.op("dve", lambda e: e.tensor_copy(out=ident[:], in_=identf[:]), reads=[identf_b], writes=[ident_b])
        for en in ("dve", "act", "pool"):
            pg.wait_tok(en, gT_b.last_w)
        pg.wait_tok("act", eps_b.last_w)
        g0 = GAIN_COLS["mix1"]
        load_weight(cx, Win, Win_b, W["dsa_w_in"], KC, DSA_IN, gain=gT[:, g0:g0 + KC], stage=stage)
        for k in range(KC):
            pg.op("pool", lambda e, k=k: e.tensor_copy(out=Win[:, k, DSA_IN:DSA_IN + 64], in_=Win[:, k, 1792:1856]),
                  reads=[Win_b], writes=[Win_b])
        Wk2 = cx.sb([P, KC, P], BF16, "Wk2"); Wk2_b = Buf("Wk2")
        for k in range(KC):
            pg.op("pool", lambda e, k=k: e.tensor_copy(out=Wk2[:, k, 0:64], in_=Win[:, k, 1792:1856]), reads=[Win_b], writes=[Wk2_b])
            pg.op("pool", lambda e, k=k: e.tensor_copy(out=Wk2[:, k, 64:128], in_=Win[:, k, 1792:1856]), reads=[Win_b], writes=[Wk2_b])
        load_weight(cx, Wuk, Wuk_b, W["dsa_w_ukT"], 1, 2048, gain=None, stage=stage)
        pg.op("pool", lambda e: e.memset(ctok[:, :, 256:258], 1.0), writes=[ctok_b])

        xin = [(cx.sb([P, D], F32, "xin"), Buf("xin")) for _ in range(2)]
        hn2 = [(cx.sb([P, D], BF16, "hn"), Buf("hn")) for _ in range(2)]
        junk = cx.sb([P, D], BF16, "junk"); junk_b = Buf("junk")
        ss = cx.sb([P, 1], F32, "ss"); ss_b = Buf("ss")
        rstd = cx.sb([P, 1], F32, "rstd"); rstd_b = Buf("rstd")
        hnT2 = [(cx.sb([P, KC, P], BF16, "hnT"), Buf("hnT")) for _ in range(2)]
        qT = cx.sb([P, 8, P], BF16, "qT"); qT_b = Buf("qT")
        qlat = [(cx.sb([P, 2, 8, P], BF16, "qlat"), Buf("qlat")) for _ in range(2)]
        qi = [(cx.sb([P, 8, P], BF16, "qi"), Buf("qi")) for _ in range(2)]
        for qit_, qib_ in qi:
            pg.op("pool", lambda e, qit_=qit_: e.memset(qit_[:], 0.0), writes=[qib_])
        csb = cx.sb([P, 256], F32, "csb"); csb_b = Buf("csb")
        ssc = cx.sb([P, 1], F32, "ssc"); ssc_b = Buf("ssc")
        rsc = cx.sb([P, 1], F32, "rsc"); rsc_b = Buf("rsc")
        wsb = cx.sb([P, 8], F32, "wsb"); wsb_b = Buf("wsb")

        pA32 = cx.ps([P, 512], F32, "pA"); pA_b = Buf("pA")
        pA = pA32[:, :].bitcast(BF16).rearrange("p (k t) -> p k t", k=KC)
        pQ = [(cx.ps([P, 512], F32, "pQ"), Buf("pQ")) for _ in range(2)]
        pL = [(cx.ps([P, 512], F32, "pL"), Buf("pL")) for _ in range(2)]
        pM = cx.ps([P, 512], F32, "pM"); pM_b = Buf("pM")
        pI = cx.ps([P, 512], F32, "pI"); pI_b = Buf("pI")

        def front(t):
            xi, xi_b = xin[t % 2]
            hn, hn_b = hn2[t % 2]
            hnT, hnT_b = hnT2[t % 2]
            pg.dma("sp", lambda e: e.dma_start(out=xi[:], in_=h_in[t * P:(t + 1) * P, :]), writes=[xi_b])
            rms_rstd(cx, xi[:], xi_b, junk[:], junk_b, ss[:, 0:1], ss_b, rstd[:, 0:1], rstd_b, D)
            pg.op("act", lambda e: e.activation(out=hn[:], in_=xi[:], func=AF.Copy, scale=rstd[:, 0:1]),
                  reads=[xi_b, rstd_b], writes=[hn_b])
            for k in range(KC):
                pg.op("pe", lambda e, k=k: e.transpose(out=pA[:, k, :], in_=hn[:, k * P:(k + 1) * P], identity=ident[:]),
                      reads=[hn_b, ident_b], writes=[pA_b], signal=(k == KC - 1))
            pg.op("dve", lambda e: e.tensor_copy(out=hnT[:], in_=pA), reads=[pA_b], writes=[hnT_b])

        def tile_body(t):
            hnT, hnT_b = hnT2[t % 2]
            ql, ql_b = qlat[t % 2]
            qit, qi_b = qi[t % 2]
            for half in range(2):
                pq, pq_b = pQ[half]
                for hl in range(4):
                    h = half * 4 + hl
                    for k in range(KC):
                        pg.op("pe", lambda e, pq=pq, hl=hl, h=h, k=k: e.matmul(pq[:, hl * P:(hl + 1) * P], lhsT=Win[:, k, h * P:(h + 1) * P], rhs=hnT[:, k, :],
                                                                              start=(k == 0), stop=(k == KC - 1)),
                              reads=[Win_b, hnT_b], writes=[pq_b], signal=(hl == 3 and k == KC - 1))
                if half == 0:
                    pg.op("act", lambda e, pq=pq: e.copy(out=qT[:, 0:4, :].rearrange("p h t -> p (h t)"), in_=pq[:, :]), reads=[pq_b], writes=[qT_b])
                else:
                    pg.op("dve", lambda e, pq=pq: e.tensor_copy(out=qT[:, 4:8, :].rearrange("p h t -> p (h t)"), in_=pq[:, :]), reads=[pq_b], writes=[qT_b])
            if t + 1 < NTT:
                front(t + 1)
            for cc in range(2):
                for half in range(2):
                    pl, pl_b = pL[half]
                    for hl in range(4):
                        h = half * 4 + hl
                        pg.op("pe", lambda e, pl=pl, hl=hl, h=h, cc=cc: e.matmul(
                            pl[:, hl * P:(hl + 1) * P], lhsT=Wuk[:, 0, h * 256 + cc * P:h * 256 + (cc + 1) * P], rhs=qT[:, h, :], start=True, stop=True),
                            reads=[Wuk_b, qT_b], writes=[pl_b], signal=(hl == 3))
                    if half == 0:
                        pg.op("act", lambda e, pl=pl, ql=ql, cc=cc: e.activation(out=ql[:, cc, 0:4, :].rearrange("p h t -> p (h t)"), in_=pl[:, :],
                                                                               func=AF.Copy, scale=float(128 ** -0.5)), reads=[pl_b], writes=[ql_b])
                    else:
                        pg.op("dve", lambda e, pl=pl, ql=ql, cc=cc: e.tensor_scalar(out=ql[:, cc, 4:8, :].rearrange("p h t -> p (h t)"), in0=pl[:, :],
                                                                                  scalar1=float(128 ** -0.5), scalar2=None, op0=ALU.mult), reads=[pl_b], writes=[ql_b])
            pg.dma("sp", lambda e, ql=ql, t=t: e.dma_start(out=scr["qlat"][t], in_=ql[:].rearrange("p a h t -> p (a h t)")), reads=[ql_b])
            for k in range(KC):
                pg.op("pe", lambda e, k=k: e.matmul(pM[:, 0:256], lhsT=hnT[:, k, :], rhs=Win[:, k, 1024:1280], start=(k == 0), stop=(k == KC - 1)),
                      reads=[Win_b, hnT_b], writes=[pM_b], signal=(k == KC - 1))
            pg.op("dve", lambda e: e.tensor_copy(out=csb[:], in_=pM[:, 0:256]), reads=[pM_b], writes=[csb_b])
            rms_rstd(cx, csb[:], csb_b, junk[:, 0:256], junk_b, ssc[:, 0:1], ssc_b, rsc[:, 0:1], rsc_b, 256)
            pg.op("dve", lambda e, t=t: e.scalar_tensor_tensor(out=ctok[:, t, 0:256], in0=csb[:], scalar=rsc[:, 0:1], in1=gkvB[:],
                                                               op0=ALU.mult, op1=ALU.mult), reads=[csb_b, rsc_b, gkvB_b], writes=[ctok_b])
            for cc in range(2):
                pg.op("pe", lambda e, cc=cc, t=t: e.transpose(out=pA[:, cc, :], in_=ctok[:, t, cc * P:(cc + 1) * P], identity=ident[:]),
                      reads=[ctok_b, ident_b], writes=[pA_b], signal=(cc == 1))
            pg.op("act", lambda e, t=t: e.copy(out=cT[:, :, t * P:(t + 1) * P], in_=pA[:, 0:2, :]), reads=[pA_b], writes=[cT_b])
            for p in range(4):
                for k in range(KC):
                    pg.op("pe", lambda e, p=p, k=k: e.matmul(pI[:, p * P:(p + 1) * P], lhsT=Win[:, k, 1280 + p * P:1280 + (p + 1) * P], rhs=hnT[:, k, :],
                                                              start=(k == 0), stop=(k == KC - 1)),
                          reads=[Win_b, hnT_b], writes=[pI_b], signal=(p == 3 and k == KC - 1))
            pg.op("act", lambda e, qit=qit: e.copy(out=qit[0:64, :, :].rearrange("p (a two) t -> p a two t", two=2)[:, :, 0, :],
                                                   in_=pI[0:64, :].rearrange("p (a t) -> p a t", a=4)), reads=[pI_b], writes=[qi_b])
            pg.op("dve", lambda e, qit=qit: e.tensor_copy(out=qit[64:128, :, :].rearrange("p (a two) t -> p a two t", two=2)[:, :, 1, :],
                                                          in_=pI[64:128, :].rearrange("p (a t) -> p a t", a=4)), reads=[pI_b], writes=[qi_b])
            pg.dma("sp", lambda e, qit=qit, t=t: e.dma_start(out=scr["qi"][t], in_=qit[:].rearrange("p a t -> p (a t)")), reads=[qi_b])
            for k in range(KC):
                pg.op("pe", lambda e, k=k: e.matmul(pM[:, 256:384], lhsT=Wk2[:, k, :], rhs=hnT[:, k, :], start=(k == 0), stop=(k == KC - 1)),
                      reads=[Wk2_b, hnT_b], writes=[pM_b], signal=(k == KC - 1))
            pg.op("dve", lambda e, t=t: e.tensor_copy(out=kidx2[:, t * P:(t + 1) * P], in_=pM[:, 256:384]), reads=[pM_b], writes=[kidx2_b])
            for k in range(KC):
                pg.op("pe", lambda e, k=k: e.matmul(pM[:, 384:392], lhsT=hnT[:, k, :], rhs=Win[:, k, 1856:1864], start=(k == 0), stop=(k == KC - 1)),
                      reads=[Win_b, hnT_b], writes=[pM_b], signal=(k == KC - 1))
            pg.op("dve", lambda e: e.tensor_copy(out=wsb[:], in_=pM[:, 384:392]), reads=[pM_b], writes=[wsb_b])
            pg.op("act", lambda e, t=t: e.activation(out=wabs[:, t, :], in_=wsb[:], func=AF.Abs, scale=IDX_C0),
                  reads=[wsb_b], writes=[wabs_b])
            pg.op("act", lambda e, t=t: e.activation(out=sgn[:, t, :], in_=wsb[:], func=AF.Sign), reads=[wsb_b], writes=[sgn_b])
        front(0)
        for t in range(NTT):
            tile_body(t)
        pg.barrier()


NIT = 15
TOPK = 256


def phase_dsa_attn(nc, pg, NTT, W, consts, caches, scr):
    kidx2, kidx2_b = caches["kidx2"]
    cT, cT_b = caches["cT"]
    ctok, ctok_b = caches["ctok"]
    wabs, wabs_b = caches["wabs"]
    sgn, sgn_b = caches["sgn"]
    SMAX = NTT * P
    AX = mybir.AxisListType.X
    with contextlib.ExitStack() as stack:
        cx = Ctx(nc, pg, stack)
        Wuv = cx.sb([P, 2, 8, P], BF16, "Wuv"); Wuv_b = Buf("Wuv")
        ident = cx.sb([P, P], BF16, "ident"); ident_b = Buf("ident")
        identf = cx.sb([P, P], F32, "identf"); identf_b = Buf("identf")
        m0 = cx.sb([P, P], F32, "m0"); m0_b = Buf("m0")
        mdiag = cx.sb([P, P], F32, "mdiag"); mdiag_b = Buf("mdiag")
        scores = cx.sb([P, max(SMAX, 2048)], F32, "scores"); sc_b = Buf("scores")
        biasf = scores[:, 0:1024]; biasf_b = Buf("biasf")
        b15f = scores[:, 1024:2048]; b15f_b = Buf("b15f")
        bias = cx.sb([P, 3, 2, 1024], BF16, "bias"); bias_b = Buf("bias")
        mone = cx.sb([P, 8], F32, "mone"); mone_b = Buf("mone")
        pg.op("pool", lambda e: e.memset(mone[:], -1.0), writes=[mone_b])
        pg.dma("sp", lambda e: e.dma_start(out=identf[:], in_=W["identf"][:, :]), writes=[identf_b])
        pg.dma("sp", lambda e: e.dma_start(out=m0[:], in_=consts["m0"][:, :]), writes=[m0_b])
        pg.dma("sp", lambda e: e.dma_start(out=mdiag[:], in_=consts["mdiag"][:, :]), writes=[mdiag_b])
        pg.dma("sp", lambda e: e.dma_start(out=b15f, in_=consts["b15"][:, :]), writes=[b15f_b])
        pg.op("dve", lambda e: e.tensor_copy(out=ident[:], in_=identf[:]), reads=[identf_b], writes=[ident_b])
        for wi in range(3):
            pg.dma("sp", lambda e, wi=wi: e.dma_start(out=biasf, in_=consts["biasT"][wi]), writes=[biasf_b])
            pg.op("dve", lambda e: e.tensor_tensor(out=biasf, in0=biasf, in1=b15f, op=ALU.subtract), reads=[biasf_b, b15f_b], writes=[biasf_b])
            pg.op("dve", lambda e, wi=wi: e.tensor_copy(out=bias[:, wi, 0, :], in_=biasf), reads=[biasf_b], writes=[bias_b])
            pg.op("dve", lambda e, wi=wi: e.tensor_tensor(out=biasf, in0=biasf, in1=bias[:, wi, 0, :], op=ALU.subtract), reads=[biasf_b, bias_b], writes=[biasf_b])
            pg.op("dve", lambda e, wi=wi: e.tensor_copy(out=bias[:, wi, 1, :], in_=biasf), reads=[biasf_b], writes=[bias_b])
        wuv_v = W["dsa_w_uv"].rearrange("h (cc p) d -> p cc h d", p=P)
        stage = [(biasf, biasf_b), (b15f, b15f_b)]
        for cc in range(2):
            st, stb = stage[cc]
            pg.dma("sp", lambda e, st=st, cc=cc: e.dma_start(out=st.rearrange("p (h d) -> p h d", h=8), in_=wuv_v[:, cc, :, :]), writes=[stb])
            pg.op("dve", lambda e, st=st, cc=cc: e.tensor_copy(out=Wuv[:, cc, :, :].rearrange("p h d -> p (h d)"), in_=st), reads=[stb], writes=[Wuv_b])

        nm = cx.sb([P, SMAX], BF16, "nm"); nm_b = Buf("nm")
        nmT = cx.sb([P, NTT, P], BF16, "nmT"); nmT_b = Buf("nmT")
        qlat = [(cx.sb([P, 2, 8, P], BF16, "qlat"), Buf("qlat")) for _ in range(2)]
        qi = [(cx.sb([P, 8, P], BF16, "qi"), Buf("qi")) for _ in range(2)]
        dsg = [(cx.sb([P, 8, P], BF16, "dsg"), [Buf("dsg%d" % h) for h in range(8)]) for _ in range(2)]
        NA = 2
        A = [(cx.sb([P, 1024], BF16, "A"), Buf("A")) for _ in range(NA)]
        NKB = (SMAX + 511) // 512
        mxs = cx.sb([P, NKB], F32, "mxs"); mxs_b = Buf("mxs")
        mns = cx.sb([P, NKB], F32, "mns"); mns_b = Buf("mns")
        pow2 = cx.sb([P, 32], F32, "pow2"); pow2_b = Buf("pow2")
        ds = cx.sb([P, 32], F32, "ds"); ds_b = Buf("ds")
        tq = cx.sb([P, 1], F32, "tq"); tq_b = Buf("tq")
        pg.dma("sp", lambda e: e.dma_start(out=pow2[:], in_=consts["pow2"][:, :]), writes=[pow2_b])
        NPT = 5
        PT = [(cx.sb([P, 512], BF16, "PT"), Buf("PT")) for _ in range(NPT)]
        lo = cx.sb([P, 1], F32, "lo"); lo_b = Buf("lo")
        wd = cx.sb([P, 1], F32, "wd"); wd_b = Buf("wd")
        mid = cx.sb([P, 1], F32, "mid"); mid_b = Buf("mid")
        cnt = cx.sb([P, 1], F32, "cnt"); cnt_b = Buf("cnt")
        pw = cx.sb([P, 1], F32, "pw"); pw_b = Buf("pw")
        den = cx.sb([P, 8], F32, "den"); den_b = Buf("den")
        rden = cx.sb([P, 8], F32, "rden"); rden_b = Buf("rden")
        Un = cx.sb([P, 8, 256], BF16, "Un"); Un_b = Buf("Un")
        UnT = cx.sb([P, 16, P], BF16, "UnT"); UnT_b = Buf("UnT")
        oT = [(cx.sb([P, 8, P], BF16, "oT"), Buf("oT")) for _ in range(2)]

        def bank(nm_):
            t_ = cx.ps([P, 512], F32, nm_)
            return (t_, Buf(nm_), t_[:, :].bitcast(BF16).rearrange("p (k t) -> p k t", k=8))
        def bank2(nm_):
            t_ = cx.ps([P, 1024], F32, nm_)
            b0 = (t_[:, 0:512], Buf(nm_ + "a"), t_[:, 0:512].bitcast(BF16).rearrange("p (k t) -> p k t", k=8))
            b1 = (t_[:, 512:1024], Buf(nm_ + "b"), t_[:, 512:1024].bitcast(BF16).rearrange("p (k t) -> p k t", k=8))
            return t_, b0, b1
        pXX, pX0, pX1 = bank2("pXX")
        pUU, pU0, pU1 = bank2("pUU")
        pS, pU2, pU3, pDn = [bank(n_) for n_ in ("pS", "pU2", "pU3", "pDn")]
        X2 = [(pXX, pX0, pX1), (pUU, pU0, pU1)]
        SCB = [pS, pU2, pU3, pDn]
        TB = [pU3, pDn]
        LB = [pX0, pX1, pS]
        UB = [pU0, pU1, pU2, pU3]

        def build_dsg(j):
            dt_, db_ = dsg[j % 2]
            for h in range(8):
                pg.op("dve", lambda e, h=h, j=j, dt_=dt_: e.tensor_scalar(out=dt_[:, h, :], in0=ident[:], scalar1=sgn[:, j, h:h + 1], scalar2=None, op0=ALU.mult),
                      reads=[ident_b, sgn_b], writes=[db_[h]])

        def load_q(j):
            ql, ql_b = qlat[j % 2]
            qit, qi_b = qi[j % 2]
            pg.dma("sp", lambda e, qit=qit, j=j: e.dma_start(out=qit[:].rearrange("p a t -> p (a t)"), in_=scr["qi"][j]), writes=[qi_b])
            pg.dma("sp", lambda e, ql=ql, j=j: e.dma_start(out=ql[:].rearrange("p a h t -> p (a h t)"), in_=scr["qlat"][j]), writes=[ql_b])

        def stage_I(j):
            S = (j + 1) * P
            qit, qi_b = qi[j % 2]
            dt_, db_ = dsg[j % 2]
            nkb = (S + 511) // 512
            npair = (nkb + 1) // 2
            units = [(kp, h) for kp in range(npair) for h in range(8)]
            DX = 1
            n = len(units)

            def cols(kp):
                c0 = kp * 1024
                return c0, min(1024, S - c0)

            def emit_X(i):
                kp, h = units[i]
                c0, cw = cols(kp)
                xx, xa, xb = X2[i % 2]
                for q_, xq in enumerate((xa, xb)):
                    w_ = min(512, cw - q_ * 512)
                    if w_ <= 0:
                        continue
                    pg.op("pe", lambda e, xq=xq, h=h, c0=c0, q_=q_, w_=w_: e.matmul(
                        xq[0][:, 0:w_], lhsT=qit[:, h, :], rhs=kidx2[:, c0 + q_ * 512:c0 + q_ * 512 + w_], start=True, stop=True),
                        reads=[qi_b, kidx2_b], writes=[xq[1]])

            def emit_R(i):
                kp, h = units[i]
                c0, cw = cols(kp)
                xx, xa, xb = X2[i % 2]
                At, A_b = A[i % NA]
                rb = [xa[1]] + ([xb[1]] if cw > 512 else [])
                pg.op("act", lambda e, xx=xx, At=At, cw=cw, h=h: e.activation(out=At[:, 0:cw], in_=xx[:, 0:cw], func=AF.Relu, scale=wabs[:, j, h:h + 1]),
                      reads=rb + [wabs_b], writes=[A_b])
                for q_ in range(2):
                    w_ = min(512, cw - q_ * 512)
                    if w_ <= 0:
                        continue
                    sc, sc_pb, _ = SCB[(2 * kp + q_) % 4]
                    pg.op("pe", lambda e, At=At, w_=w_, h=h, sc=sc, q_=q_: e.matmul(sc[:, 0:w_], lhsT=dt_[:, h, :], rhs=At[:, q_ * 512:q_ * 512 + w_],
                                                                                  start=(h == 0), stop=(h == 7)),
                          reads=[A_b, db_[h]], writes=[sc_pb], signal=(h == 7))
                    if h == 7:
                        kb = 2 * kp + q_
                        cc0 = c0 + q_ * 512
                        pg.op("dve", lambda e, cc0=cc0, w_=w_, sc=sc, kb=kb: e.tensor_scalar(out=scores[:, cc0:cc0 + w_], in0=sc[:, 0:w_], scalar1=1.0, scalar2=None,
                                                                                         op0=ALU.mult, op1=ALU.max, accum_out=mxs[:, kb:kb + 1]),
                              reads=[sc_pb], writes=[sc_b, mxs_b])
                        pg.op("dve", lambda e, cc0=cc0, w_=w_, kb=kb: e.tensor_reduce(out=mns[:, kb:kb + 1], in_=scores[:, cc0:cc0 + w_], op=ALU.min, axis=AX),
                              reads=[sc_b], writes=[mns_b])

            for i in range(n + DX):
                if i < n:
                    emit_X(i)
                if i - DX >= 0:
                    emit_R(i - DX)
            pg.op("pool", lambda e: e.tensor_tensor(out=scores[:, 0:P], in0=scores[:, 0:P], in1=m0[:], op=ALU.add), reads=[sc_b, m0_b, mns_b], writes=[sc_b])
            if j >= 1:
                pg.op("pool", lambda e: e.tensor_tensor(out=scores[:, j * P:(j + 1) * P], in0=scores[:, j * P:(j + 1) * P], in1=mdiag[:], op=ALU.add),
                      reads=[sc_b, mdiag_b], writes=[sc_b])
            pg.op("dve", lambda e: e.tensor_reduce(out=lo[:], in_=mns[:, 0:nkb], op=ALU.min, axis=AX), reads=[mns_b], writes=[lo_b])
            pg.op("dve", lambda e: e.tensor_reduce(out=wd[:], in_=mxs[:, 0:nkb], op=ALU.max, axis=AX), reads=[mxs_b], writes=[wd_b])
            pg.op("dve", lambda e: e.tensor_tensor(out=wd[:], in0=wd[:], in1=lo[:], op=ALU.subtract), reads=[wd_b, lo_b], writes=[wd_b])
            pg.op("dve", lambda e: e.tensor_scalar(out=tq[:], in0=wd[:], scalar1=0.001, scalar2=1e-6, op0=ALU.mult, op1=ALU.add), reads=[wd_b], writes=[tq_b])
            pg.op("dve", lambda e: e.tensor_tensor(out=lo[:], in0=lo[:], in1=tq[:], op=ALU.subtract), reads=[lo_b, tq_b], writes=[lo_b])
            pg.op("dve", lambda e: e.tensor_scalar(out=wd[:], in0=wd[:], scalar1=1.002, scalar2=2e-6, op0=ALU.mult, op1=ALU.add), reads=[wd_b], writes=[wd_b])
            pg.op("dve", lambda e: e.tensor_scalar(out=ds[:], in0=pow2[:], scalar1=wd[:, 0:1], scalar2=None, op0=ALU.mult), reads=[wd_b, pow2_b], writes=[ds_b])
            pg.op("dve", lambda e: e.tensor_tensor(out=mid[:], in0=lo[:], in1=ds[:, 0:1], op=ALU.add), reads=[lo_b, ds_b], writes=[mid_b])

        def stage_B(j):
            S = (j + 1) * P
            for it in range(NIT):
                pg.op("dve", lambda e: e.tensor_scalar(out=nm[:, 0:S], in0=scores[:, 0:S], scalar1=mid[:, 0:1], scalar2=None, op0=ALU.is_ge, op1=ALU.add,
                                                       accum_out=cnt[:, 0:1]), reads=[sc_b, mid_b], writes=[nm_b, cnt_b])
                pg.op("dve", lambda e, it=it: e.tensor_scalar(out=tq[:], in0=cnt[:], scalar1=TOPK - 0.5, scalar2=ds[:, it:it + 1], op0=ALU.is_ge, op1=ALU.mult),
                      reads=[cnt_b, ds_b], writes=[tq_b])
                pg.op("dve", lambda e, it=it: e.scalar_tensor_tensor(out=mid[:], in0=tq[:], scalar=ds[:, it + 1:it + 2], in1=mid[:], op0=ALU.subtract, op1=ALU.add),
                      reads=[tq_b, ds_b, mid_b], writes=[mid_b])
            pg.op("dve", lambda e: e.tensor_scalar(out=nm[:, 0:S], in0=scores[:, 0:S], scalar1=ds[:, NIT:NIT + 1], scalar2=mid[:, 0:1], op0=ALU.add, op1=ALU.is_ge),
                  reads=[sc_b, ds_b, mid_b], writes=[nm_b])

        def stage_T(j):
            ib = 0
            for k0 in range(0, j + 1, 8):
                kn = min(8, j + 1 - k0)
                tb, tb_b, tbv = TB[ib % 2]
                ib += 1
                for kk in range(kn):
                    pg.op("pe", lambda e, kk=kk, k0=k0, tbv=tbv: e.transpose(out=tbv[:, kk, :], in_=nm[:, (k0 + kk) * P:(k0 + kk + 1) * P], identity=ident[:]),
                          reads=[nm_b, ident_b], writes=[tb_b], signal=(kk == kn - 1))
                pg.op("act", lambda e, k0=k0, kn=kn, tbv=tbv: e.copy(out=nmT[:, k0:k0 + kn, :], in_=tbv[:, 0:kn, :]), reads=[tb_b], writes=[nmT_b])

        def stage_W(j):
            ql, ql_b = qlat[j % 2]
            units = [(kt, half) for kt in range(j + 1) for half in range(2)]
            n = len(units)
            DL = 2

            def emit_QK(i):
                kt, half = units[i]
                wi = None
                if kt == j:
                    wi = 0
                elif kt == j - 1 and j >= 2:
                    wi = 1
                elif j == 1 and kt == 0:
                    wi = 2
                px, px_b, _ = LB[i % 3]
                for cc in range(2):
                    pg.op("pe", lambda e, px=px, cc=cc, kt=kt, half=half: e.matmul(
                        px[:, :], lhsT=cT[:, cc, kt * P:(kt + 1) * P], rhs=ql[:, cc, half * 4:half * 4 + 4, :].rearrange("p h t -> p (h t)"),
                        start=(cc == 0), stop=(cc == 1 and wi is None)), reads=[cT_b, ql_b], writes=[px_b], signal=(cc == 1 and wi is None))
                if wi is not None:
                    for hl in range(2):
                        pg.op("pe", lambda e, px=px, wi=wi, hl=hl, half=half: e.matmul(px[:, :], lhsT=ident[:], rhs=bias[:, wi, hl, half * 512:(half + 1) * 512],
                                                                                    start=False, stop=(hl == 1)), reads=[ident_b, bias_b], writes=[px_b], signal=(hl == 1))

            def emit_PV(i):
                kt, half = units[i]
                px, px_b, _ = LB[i % 3]
                Pt, Pt_b = PT[i % NPT]
                pg.op("act", lambda e, px=px, Pt=Pt: e.activation(out=Pt[:], in_=px[:, :], func=AF.Exp), reads=[px_b], writes=[Pt_b])
                pg.op("pool", lambda e, Pt=Pt, kt=kt: e.tensor_tensor(out=Pt[:].rearrange("p (h t) -> p h t", h=4), in0=Pt[:].rearrange("p (h t) -> p h t", h=4),
                                                                    in1=nmT[:, kt:kt + 1, :].broadcast_to([P, 4, P]), op=ALU.mult),
                      reads=[Pt_b, nmT_b], writes=[Pt_b])
                for hl in range(4):
                    h = half * 4 + hl
                    pu, pu_b, _ = UB[h // 2]
                    first = (kt == 0 and h % 2 == 0)
                    pg.op("pe", lambda e, pu=pu, h=h, hl=hl, Pt=Pt, kt=kt, first=first: e.matmul(
                        pu[:, (h % 2) * 256:(h % 2 + 1) * 256], lhsT=Pt[:, hl * P:(hl + 1) * P], rhs=ctok[:, kt, 0:256], start=first, stop=(kt == j),
                        skip_group_check=True), reads=[Pt_b, ctok_b], writes=[pu_b], signal=False)
                    firstd = (kt == 0 and h == 0)
                    pg.op("pe", lambda e, h=h, hl=hl, Pt=Pt, kt=kt, firstd=firstd: e.matmul(
                        pDn[0][:, 2 * h:2 * h + 2], lhsT=Pt[:, hl * P:(hl + 1) * P], rhs=ctok[:, kt, 256:258], start=firstd, stop=(kt == j),
                        skip_group_check=True), reads=[Pt_b, ctok_b], writes=[pDn[1]], signal=(hl == 3))

            for i in range(n + DL):
                if i < n:
                    emit_QK(i)
                if i - DL >= 0:
                    emit_PV(i - DL)

        def stage_Z(j):
            pg.op("act", lambda e: e.copy(out=den[:], in_=pDn[0][:, 0:16].rearrange("p (h two) -> p h two", two=2)[:, :, 0]), reads=[pDn[1]], writes=[den_b])
            pg.op("pool", lambda e: e.tensor_tensor(out=rden[:], in0=den[:], in1=mone[:], op=ALU.pow), reads=[den_b, mone_b], writes=[rden_b])
            for h in range(8):
                pu, pu_b, _ = UB[h // 2]
                pg.op("act", lambda e, pu=pu, h=h: e.activation(out=Un[:, h, :], in_=pu[:, (h % 2) * 256:(h % 2 + 1) * 256], func=AF.Copy, scale=rden[:, h:h + 1]),
                      reads=[pu_b, rden_b], writes=[Un_b])
            for g in range(2):
                tb, tb_b, tbv = (pX0, pX1)[g]
                for kk in range(8):
                    idx = g * 8 + kk
                    h, cc = idx // 2, idx % 2
                    pg.op("pe", lambda e, kk=kk, h=h, cc=cc, tbv=tbv: e.transpose(out=tbv[:, kk, :], in_=Un[:, h, cc * P:(cc + 1) * P], identity=ident[:]),
                          reads=[Un_b, ident_b], writes=[tb_b], signal=(kk == 7))
                pg.op("act", lambda e, g=g, tbv=tbv: e.copy(out=UnT[:, g * 8:(g + 1) * 8, :], in_=tbv[:, :, :]), reads=[tb_b], writes=[UnT_b])
            ot, ot_b = oT[j % 2]
            for half in range(2):
                px, px_b, _ = (pS, pX0)[half]
                for hl in range(4):
                    h = half * 4 + hl
                    for cc in range(2):
                        pg.op("pe", lambda e, px=px, hl=hl, h=h, cc=cc: e.matmul(px[:, hl * P:(hl + 1) * P], lhsT=Wuv[:, cc, h, :], rhs=UnT[:, h * 2 + cc, :],
                                                                              start=(cc == 0), stop=(cc == 1)), reads=[Wuv_b, UnT_b], writes=[px_b],
                              signal=(hl == 3 and cc == 1))
                pg.op("act", lambda e, px=px, ot=ot, half=half: e.copy(out=ot[:, half * 4:half * 4 + 4, :].rearrange("p h t -> p (h t)"), in_=px[:, :]),
                      reads=[px_b], writes=[ot_b])
            pg.dma("sp", lambda e, ot=ot, j=j: e.dma_start(out=scr["oT"][j], in_=ot[:].rearrange("p h t -> p (h t)")), reads=[ot_b])

        build_dsg(0)
        load_q(0)
        stage_I(0)
        stage_B(0)
        stage_T(0)
        for j in range(NTT):
            if j + 1 < NTT:
                build_dsg(j + 1)
                load_q(j + 1)
                stage_I(j + 1)
            stage_W(j)
            if j + 1 < NTT:
                stage_B(j + 1)
            stage_Z(j)
            if j + 1 < NTT:
                stage_T(j + 1)
        pg.barrier()


def phase_dsa_out(nc, pg, NTT, h_in, h_out, W, scr):
    with contextlib.ExitStack() as stack:
        cx = Ctx(nc, pg, stack)
        Wo = cx.sb([P, 8, D], BF16, "Wo"); Wo_b = Buf("Wo")
        stage = [(cx.sb([P, 1024], F32, "stg"), Buf("stg")) for _ in range(5)]
        load_weight(cx, Wo, Wo_b, W["dsa_w_out"], 8, D, gain=None, stage=stage)
        xin = [(cx.sb([P, D], F32, "xin"), Buf("xin")) for _ in range(3)]
        ot = [(cx.sb([P, 8, P], BF16, "ot"), Buf("ot")) for _ in range(3)]
        po = [(cx.ps([P, 512], F32, "po"), Buf("po")) for _ in range(4)]
        ic = 0
        for t in range(NTT):
            xi, xi_b = xin[t % 3]
            o_, o_b = ot[t % 3]
            pg.dma("sp", lambda e, xi=xi, t=t: e.dma_start(out=xi[:], in_=h_in[t * P:(t + 1) * P, :]), writes=[xi_b])
            pg.dma("sp", lambda e, o_=o_, t=t: e.dma_start(out=o_[:].rearrange("p h t -> p (h t)"), in_=scr["oT"][t]), writes=[o_b])
            for n in range(2):
                pp, pp_b = po[ic % 4]
                ic += 1
                for h in range(8):
                    pg.op("pe", lambda e, pp=pp, h=h, n=n, o_=o_: e.matmul(pp[:, :], lhsT=o_[:, h, :], rhs=Wo[:, h, n * 512:(n + 1) * 512], start=(h == 0), stop=(h == 7)),
                          reads=[o_b, Wo_b], writes=[pp_b], signal=(h == 7))
                pg.op("dve", lambda e, pp=pp, n=n, xi=xi: e.tensor_tensor(out=xi[:, n * 512:(n + 1) * 512], in0=xi[:, n * 512:(n + 1) * 512], in1=pp[:, :], op=ALU.add),
                      reads=[pp_b, xi_b], writes=[xi_b])
            pg.dma("sp", lambda e, xi=xi, t=t: e.dma_start(out=h_out[t * P:(t + 1) * P, :], in_=xi[:]), reads=[xi_b])
        pg.barrier()


import math


def _rel_bucket(rel):
    rel = np.asarray(rel, np.int64)
    nb = 16
    max_exact = 8
    ret = np.where(rel > 0, nb, 0)
    n = np.abs(rel)
    nf = np.maximum(n, 1).astype(np.float32)
    large = max_exact + (np.log(nf / np.float32(max_exact)) / np.float32(math.log(128 / max_exact))
                         * np.float32(nb - max_exact)).astype(np.int32)
    large = np.minimum(large, nb - 1)
    return ret + np.where(n < max_exact, n, large)


def _index_consts():
    c = {}
    Dm = np.zeros((128, 2, 128), np.float32)
    cp = np.arange(128)[:, None]
    cc = np.arange(128)[None, :]
    Dm[:, 0, :] = np.where((cp // 64 == cc // 64) & (cp > cc), -1.0 / 16, 0.0)
    Dm[:, 1, :] = np.where((cp >= 48) & (cp < 64) & (cc >= 48) & (cc < 64) & (cp > cc), -1.0 / 16, 0.0)
    Ind = np.zeros((128, 2, 2), np.float32)
    Ind[np.arange(128), 0, np.arange(128) // 64] = -1.0 / 16
    Ind[48:64, 1, 0] = -1.0 / 16
    c["c_Dm"] = Dm
    c["c_Ind"] = Ind
    m0 = np.full((128, 128), NEG, np.float32)
    m0[:, 48:64] = 0
    md = np.zeros((128, 128), np.float32)
    md[0:64, 64:128] = NEG
    c["c_m0"] = m0
    c["c_mdiag"] = md
    c["identf"] = np.eye(128, dtype=np.float32)
    c["c_pow2"] = np.ascontiguousarray(np.broadcast_to((2.0 ** -(np.arange(32) + 1.0)).astype(np.float32)[None, :], (128, 32)))
    return c


def _colchunk(g):
    return np.ascontiguousarray(np.asarray(g, np.float32).reshape(-1, 128).T)


def _prep_shared(inp):
    f = lambda a: np.ascontiguousarray(np.asarray(a, dtype=np.float32))
    sh = _index_consts()
    gainT = np.zeros((128, 48), np.float32)
    gainT[:, 0:8] = _colchunk(inp["norm_mix"][0])
    gainT[:, 8:16] = _colchunk(inp["norm_ffn"][0])
    gainT[:, 16:24] = _colchunk(inp["norm_mix"][1])
    gainT[:, 24:32] = _colchunk(inp["norm_ffn"][1])
    gainT[:, 32:40] = _colchunk(inp["gla_g_out"][0])
    sh["gainT"] = gainT
    sh["ffn_w_in0"] = f(inp["ffn_w_in"][0]); sh["ffn_w_out0"] = f(inp["ffn_w_out"][0])
    sh["ffn_w_in1"] = f(inp["ffn_w_in"][1]); sh["ffn_w_out1"] = f(inp["ffn_w_out"][1])
    sh["gfinB"] = np.ascontiguousarray(np.broadcast_to(f(inp["norm_final"])[None, :], (128, D)))
    sh["gla_w_in"] = f(inp["gla_w_in"][0]); sh["gla_w_a1"] = f(inp["gla_w_a1"][0])
    sh["gla_w_a2aug"] = np.ascontiguousarray(np.concatenate([f(inp["gla_w_a2"][0]), f(inp["gla_b_a"][0])[None, :]], 0))
    sh["gla_w_out"] = f(inp["gla_w_out"][0])
    sh["meta"] = f(inp["meta"])
    sh["dsa_w_in"] = f(inp["dsa_w_in"][0])
    sh["dsa_w_ukT"] = np.ascontiguousarray(f(inp["dsa_w_uk"][0]).transpose(2, 0, 1)).reshape(128, 2048)
    sh["dsa_w_uv"] = f(inp["dsa_w_uv"][0])
    sh["dsa_w_out"] = f(inp["dsa_w_out"][0])
    sh["gkvB"] = np.ascontiguousarray(np.broadcast_to(f(inp["dsa_g_kv"][0])[None, :], (128, 256)))
    rb = f(inp["rel_bias"])
    s = np.arange(128)[:, None]
    t = np.arange(128)[None, :]
    bt = np.zeros((3, 128, 8, 128), np.float32)
    for wi, off in enumerate((0, -128, -64)):
        bt[wi] = rb[_rel_bucket(s - t + off)].transpose(0, 2, 1)
    sh["c_biasT"] = np.ascontiguousarray(bt.reshape(3, 128, 1024))
    sh["c_b15"] = np.ascontiguousarray(np.broadcast_to(rb[15][None, :, None], (128, 8, 128)).reshape(128, 1024))
    return sh


_NC_CACHE = {}


def kernel(**inputs):
    x = np.asarray(inputs["x"], dtype=np.float32)
    B, SEQ, _ = x.shape
    sh = _prep_shared(inputs)
    key = (SEQ,)
    if key not in _NC_CACHE:
        _NC_CACHE[key] = build(SEQ, "ABCDFE")
    nc = _NC_CACHE[key]
    in_maps = []
    for b in range(B):
        m = dict(sh)
        m["x"] = np.ascontiguousarray(x[b])
        in_maps.append(m)
    res = run_bass_kernel_spmd(nc, in_maps, core_ids=list(range(B)))
    return np.stack([np.asarray(r["out"], dtype=np.float32) for r in res.results], 0)
```

```python
import contextlib
import numpy as np
import concourse.bass as bass
import concourse.mybir as mybir
from concourse.bass_utils import run_bass_kernel_spmd

F32 = mybir.dt.float32
BF16 = mybir.dt.bfloat16
AF = mybir.ActivationFunctionType
ALU = mybir.AluOpType

D = 1024
DFF = 2816
EPS = 1e-6
P = 128
NEG = -30000.0

ENGS = ("pe", "act", "dve", "pool", "sp")
LIMIT = [10 ** 9]
LAST = {}
UID = [0]
DEBUG = False


class Buf:
    __slots__ = ("name", "last_w", "readers")

    def __init__(self, name):
        self.name = name
        self.last_w = None
        self.readers = {}


class Prog:
    NSLOT = 6

    def __init__(self, nc):
        self.nc = nc
        self.streams = {e: [] for e in ENGS}
        self.sems = {}
        self.cnt = {}
        for e in ("pe", "act", "dve", "pool"):
            self.sems[e] = nc.alloc_semaphore("s_" + e)
            self.cnt[e] = 0
        self.slots = {}
        self.slot_rr = {}
        for e in ("sp", "pool", "act"):
            self.slots[e] = []
            for i in range(self.NSLOT):
                k = "d_%s%d" % (e, i)
                self.sems[k] = nc.alloc_semaphore(k)
                self.cnt[k] = 0
                self.slots[e].append(k)
            self.slot_rr[e] = 0
        self.waited = {}
        self.pending = {e: False for e in ENGS}
        self.nops = 0

    def _wait(self, eng, tok):
        if tok is None:
            return
        teng, key, val = tok
        if self.waited.get((eng, key), 0) >= val:
            return
        self.waited[(eng, key)] = val
        self.streams[eng].append(("wait", key, val))

    def _deps(self, eng, reads, writes):
        for b in reads:
            t = b.last_w
            if t is not None:
                if t[0] == eng and t[1] == eng and eng == "pe":
                    continue
                self._wait(eng, t)
        for b in writes:
            t = b.last_w
            if t is not None and not (t[0] == eng and t[1] == eng):
                self._wait(eng, t)
            for re, rt in b.readers.items():
                if re == eng and rt[1] == eng:
                    continue
                self._wait(eng, rt)

    def _mark(self, eng, tok, reads, writes):
        for b in reads:
            old = b.readers.get(eng)
            if old is None or old[1] != tok[1] or old[2] < tok[2]:
                if old is not None and old[1] != tok[1]:
                    pass
                b.readers[eng] = tok
        for b in writes:
            b.last_w = tok
            b.readers = {}

    def op(self, eng, fn, reads=(), writes=(), signal=True):
        if self.nops >= LIMIT[0]:
            if not (eng == "pe" and self.pending["pe"]):
                return None
        self._deps(eng, reads, writes)
        if signal:
            self.cnt[eng] += 1
            tok = (eng, eng, self.cnt[eng])
            self.pending[eng] = False
        else:
            tok = (eng, eng, self.cnt[eng] + 1)
            self.pending[eng] = True
        self.streams[eng].append(("op", fn, eng if signal else None, 1))
        if DEBUG:
            import sys as _s
            LAST.setdefault("log", []).append((self.nops, eng, _s._getframe(1).f_lineno))
        self._mark(eng, tok, reads, writes)
        self.nops += 1
        return tok

    def dma(self, eng, fn, reads=(), writes=()):
        if self.nops >= LIMIT[0]:
            return None
        self._deps(eng, reads, writes)
        for b in reads:
            old = b.readers.get(eng)
            if old is not None and old[1] != eng:
                self._wait(eng, old)
        slot = self.slots[eng][self.slot_rr[eng] % self.NSLOT]
        self.slot_rr[eng] += 1
        if self.cnt[slot] > 0:
            self._wait(eng, (eng, slot, self.cnt[slot]))
        self.cnt[slot] += 16
        tok = (eng, slot, self.cnt[slot])
        self.streams[eng].append(("op", fn, slot, 16))
        if DEBUG:
            import sys as _s
            LAST.setdefault("log", []).append((self.nops, eng + "-dma", _s._getframe(1).f_lineno))
        self._mark(eng, tok, reads, writes)
        self.nops += 1
        return tok

    def wait_tok(self, eng, tok):
        self._wait(eng, tok)

    def barrier(self, bufs=()):
        toks = []
        for e in ("pe", "act", "dve", "pool"):
            if self.cnt[e] > 0:
                toks.append((e, e, self.cnt[e]))
        for e, sl in self.slots.items():
            for k in sl:
                if self.cnt[k] > 0:
                    toks.append((e, k, self.cnt[k]))
        for e in ENGS:
            for t in toks:
                self._wait(e, t)

    def emit(self):
        nc = self.nc
        for e in ENGS:
            assert not self.pending[e], "unsignalled trailing op on " + e
        streams = self.streams
        sems = self.sems

        def run(engobj, lst):
            for it in lst:
                if it[0] == "wait":
                    engobj.wait_ge(sems[it[1]], it[2])
                else:
                    ins = it[1](engobj)
                    if it[2] is not None:
                        ins.then_inc(sems[it[2]], it[3])

        with nc.Block() as block:
            @block.tensor
            def _(e):
                run(e, streams["pe"])

            @block.scalar
            def _(e):
                run(e, streams["act"])

            @block.vector
            def _(e):
                run(e, streams["dve"])

            @block.gpsimd
            def _(e):
                run(e, streams["pool"])

            @block.sync
            def _(e):
                run(e, streams["sp"])


class Ctx:
    def __init__(self, nc, pg, stack):
        self.nc, self.pg, self.stack = nc, pg, stack
        self.n = 0

    def sb(self, shape, dt, name=None):
        UID[0] += 1
        t = self.stack.enter_context(self.nc.sbuf_tensor("%s_%d" % (name or "t", UID[0]), list(shape), dt))
        return t

    def ps(self, shape, dt, name=None):
        UID[0] += 1
        t = self.stack.enter_context(self.nc.psum_tensor("%s_%d" % (name or "p", UID[0]), list(shape), dt))
        return t


def load_weight(cx, dst, dst_buf, src, KC, N, gain=None, stage=None, rr=[0]):
    pg = cx.pg
    CB = 1024
    srcv = src.rearrange("(k p) n -> p k n", p=P)
    for k in range(KC):
        for c0 in range(0, N, CB):
            cw = min(CB, N - c0)
            st, stb = stage[rr[0] % len(stage)]
            pg.dma("sp", lambda e, st=st, k=k, c0=c0, cw=cw: e.dma_start(out=st[:, 0:cw], in_=srcv[:, k, c0:c0 + cw]),
                   writes=[stb])
            which = rr[0] % 2
            rr[0] += 1
            o = dst[:, k, c0:c0 + cw]
            i = st[:, 0:cw]
            if gain is None:
                if which == 0:
                    pg.op("dve", lambda e, o=o, i=i: e.tensor_copy(out=o, in_=i), reads=[stb], writes=[dst_buf])
                elif which == 1:
                    pg.op("act", lambda e, o=o, i=i: e.copy(out=o, in_=i), reads=[stb], writes=[dst_buf])
                else:
                    pg.op("pool", lambda e, o=o, i=i: e.tensor_copy(out=o, in_=i), reads=[stb], writes=[dst_buf])
            else:
                g = gain[:, k:k + 1]
                if which == 0:
                    pg.op("dve", lambda e, o=o, i=i, g=g: e.tensor_scalar(out=o, in0=i, scalar1=g, scalar2=None, op0=ALU.mult),
                          reads=[stb], writes=[dst_buf])
                elif which == 1:
                    pg.op("act", lambda e, o=o, i=i, g=g: e.activation(out=o, in_=i, func=AF.Copy, scale=g),
                          reads=[stb], writes=[dst_buf])
                else:
                    pg.op("pool", lambda e, o=o, i=i, g=g: e.tensor_scalar(out=o, in0=i, scalar1=g, scalar2=None, op0=ALU.mult),
                          reads=[stb], writes=[dst_buf])


def rms_rstd(cx, x_ap, xbuf, junk, junkb, ss, ssb, rstd, rstdb, n):
    pg = cx.pg
    pg.op("act", lambda e: e.activation(out=junk, in_=x_ap, func=AF.Square, accum_out=ss),
          reads=[xbuf], writes=[junkb, ssb])
    pg.op("act", lambda e: e.activation(out=ss, in_=ss, func=AF.Sqrt, scale=1.0 / n, bias=cx.eps_ap),
          reads=[ssb], writes=[ssb])
    pg.op("dve", lambda e: e.reciprocal(out=rstd, in_=ss), reads=[ssb], writes=[rstdb])


def phase_ffn(nc, pg, NTT, h_in, h_out, w_in, w_out, gainT, gcol, final=None, tiles=None):
    G = 4
    KC = D // P
    FC = DFF // P
    with contextlib.ExitStack() as stack:
        cx = Ctx(nc, pg, stack)
        Win = cx.sb([P, KC, 2 * DFF], BF16, "Win"); Win_b = Buf("Win")
        Wout = cx.sb([P, FC, D], BF16, "Wout"); Wout_b = Buf("Wout")
        aT = cx.sb([P, FC, G * P], BF16, "aT"); aT_b = Buf("aT")
        stg_v = aT[:, :, :].rearrange("p f n -> p (f n)").bitcast(F32)
        stage = [(stg_v[:, i * 1024:(i + 1) * 1024], Buf("stg%d" % i)) for i in range(5)]
        gT = cx.sb([P, gainT.shape[1]], F32, "gT"); gT_b = Buf("gT")
        ident = cx.sb([P, P], BF16, "ident"); ident_b = Buf("ident")
        identf = cx.sb([P, P], F32, "identf"); identf_b = Buf("identf")
        epsT = cx.sb([P, 1], F32, "eps"); eps_b = Buf("eps")
        cx.eps_ap = epsT[:, 0:1]
        pg.op("pool", lambda e: e.memset(epsT[:], EPS), writes=[eps_b])
        pg.dma("sp", lambda e: e.dma_start(out=gT[:], in_=gainT[:, :]), writes=[gT_b])
        pg.dma("sp", lambda e: e.dma_start(out=identf[:], in_=cx_ident(nc)[:, :]), writes=[identf_b])
        pg.op("dve", lambda e: e.tensor_copy(out=ident[:], in_=identf[:]), reads=[identf_b], writes=[ident_b])
        pg.wait_tok("dve", gT_b.last_w); pg.wait_tok("act", gT_b.last_w); pg.wait_tok("pool", gT_b.last_w)
        pg.wait_tok("act", eps_b.last_w)
        load_weight(cx, Win, Win_b, w_in, KC, 2 * DFF, gain=gT[:, gcol:gcol + KC], stage=stage)
        load_weight(cx, Wout, Wout_b, w_out, FC, D, gain=None, stage=stage)
        if final is not None:
            gfin = cx.sb([P, D], F32, "gfin"); gfin_b = Buf("gfin")
            pg.dma("sp", lambda e: e.dma_start(out=gfin[:], in_=final["gB"][:, :]), writes=[gfin_b])

        NB = 2
        hin = [(cx.sb([P, G, D], F32, "hin"), Buf("hin")) for _ in range(NB)]
        hn = [(cx.sb([P, D], BF16, "hn"), Buf("hn")) for _ in range(2)]
        ss = [(cx.sb([P, 1], F32, "ss"), Buf("ss")) for _ in range(2)]
        rstd = [(cx.sb([P, 1], F32, "rstd"), Buf("rstd")) for _ in range(2)]
        hnT = cx.sb([P, KC, G * P], BF16, "hnT"); hnT_b = Buf("hnT")
        sg = [(cx.sb([P, G * P], F32, "sg"), Buf("sg")) for _ in range(2)]
        junk_t = sg[0][0][:, :].bitcast(BF16); junk_b = sg[0][1]
        pT = [(cx.ps([P, KC, P], BF16, "pT"), Buf("pT")) for _ in range(2)]
        pg_ = [(cx.ps([P, 512], F32, "pg"), Buf("pg")) for _ in range(2)]
        pu_ = [(cx.ps([P, 512], F32, "pu"), Buf("pu")) for _ in range(2)]
        po_ = [(cx.ps([P, 512], F32, "po"), Buf("po")) for _ in range(2)]

        groups = []
        t = 0 if final is None else 1
        while t < NTT:
            g = min(G, NTT - t)
            groups.append((t, g))
            t += g
        cnt = {"t": 0, "c": 0, "o": 0}

        def front(gi):
            t0, g = groups[gi]
            hi, hi_b = hin[gi % NB]
            pg.dma("sp", lambda e: e.dma_start(
                out=hi[:, 0:g, :], in_=h_in[t0 * P:(t0 + g) * P, :].rearrange("(g p) d -> p g d", p=P)),
                writes=[hi_b])
            for j in range(g):
                x_ap = hi[:, j, :]
                s_, s_b = ss[cnt["t"] % 2]; r_, r_b = rstd[cnt["t"] % 2]
                h_, h_b = hn[cnt["t"] % 2]; p_, p_b = pT[cnt["t"] % 2]
                cnt["t"] += 1
                rms_rstd(cx, x_ap, hi_b, junk_t, junk_b, s_[:, 0:1], s_b, r_[:, 0:1], r_b, D)
                pg.op("act", lambda e, h_=h_, x_ap=x_ap, r_=r_: e.activation(out=h_[:], in_=x_ap, func=AF.Copy, scale=r_[:, 0:1]),
                      reads=[hi_b, r_b], writes=[h_b])
                for k in range(KC):
                    pg.op("pe", lambda e, p_=p_, h_=h_, k=k: e.transpose(out=p_[:, k, :], in_=h_[:, k * P:(k + 1) * P], identity=ident[:]),
                          reads=[h_b, ident_b], writes=[p_b], signal=(k == KC - 1))
                pg.op("dve", lambda e, p_=p_, j=j: e.tensor_copy(out=hnT[:, :, j * P:(j + 1) * P], in_=p_[:]),
                      reads=[p_b], writes=[hnT_b])

        def mm1(gi):
            t0, g = groups[gi]
            N = g * P
            for i in range(FC):
                pgt, pg_b = pg_[cnt["c"] % 2]; put, pu_b = pu_[cnt["c"] % 2]
                sgt, sg_b = sg[cnt["c"] % 2]
                cnt["c"] += 1
                for k in range(KC):
                    pg.op("pe", lambda e, pgt=pgt, k=k, i=i: e.matmul(
                        pgt[:, 0:N], lhsT=Win[:, k, i * P:(i + 1) * P], rhs=hnT[:, k, 0:N], start=(k == 0), stop=(k == KC - 1)),
                        reads=[Win_b, hnT_b], writes=[pg_b], signal=(k == KC - 1))
                for k in range(KC):
                    pg.op("pe", lambda e, put=put, k=k, i=i: e.matmul(
                        put[:, 0:N], lhsT=Win[:, k, DFF + i * P:DFF + (i + 1) * P], rhs=hnT[:, k, 0:N], start=(k == 0), stop=(k == KC - 1)),
                        reads=[Win_b, hnT_b], writes=[pu_b], signal=(k == KC - 1))
                pg.op("act", lambda e, sgt=sgt, pgt=pgt: e.activation(out=sgt[:, 0:N], in_=pgt[:, 0:N], func=AF.Silu),
                      reads=[pg_b], writes=[sg_b])
                pg.op("dve", lambda e, sgt=sgt, put=put, i=i: e.tensor_tensor(out=aT[:, i, 0:N], in0=sgt[:, 0:N], in1=put[:, 0:N], op=ALU.mult),
                      reads=[sg_b, pu_b], writes=[aT_b])

        def mm2(gi):
            t0, g = groups[gi]
            hi, hi_b = hin[gi % NB]
            for j in range(g):
                for n in range(2):
                    pot, po_b = po_[cnt["o"] % 2]
                    cnt["o"] += 1
                    for i in range(FC):
                        pg.op("pe", lambda e, pot=pot, i=i, j=j, n=n: e.matmul(
                            pot[:, :], lhsT=aT[:, i, j * P:(j + 1) * P], rhs=Wout[:, i, n * 512:(n + 1) * 512], start=(i == 0), stop=(i == FC - 1)),
                            reads=[aT_b, Wout_b], writes=[po_b], signal=(i == FC - 1))
                    pg.op("dve", lambda e, j=j, n=n, pot=pot: e.tensor_tensor(
                        out=hi[:, j, n * 512:(n + 1) * 512], in0=hi[:, j, n * 512:(n + 1) * 512], in1=pot[:, :], op=ALU.add),
                        reads=[po_b, hi_b], writes=[hi_b])
            if final is None:
                pg.dma("sp", lambda e: e.dma_start(
                    out=h_out[t0 * P:(t0 + g) * P, :].rearrange("(g p) d -> p g d", p=P), in_=hi[:, 0:g, :]),
                    reads=[hi_b])
            else:
                for j in range(g):
                    tt = t0 + j
                    if tt == 0:
                        continue
                    x_ap = hi[:, j, :]
                    s_, s_b = ss[cnt["t"] % 2]; r_, r_b = rstd[cnt["t"] % 2]
                    cnt["t"] += 1
                    rms_rstd(cx, x_ap, hi_b, junk_t, junk_b, s_[:, 0:1], s_b, r_[:, 0:1], r_b, D)
                    pg.op("dve", lambda e, x_ap=x_ap, r_=r_: e.scalar_tensor_tensor(
                        out=x_ap, in0=x_ap, scalar=r_[:, 0:1], in1=gfin[:], op0=ALU.mult, op1=ALU.mult),
                        reads=[hi_b, r_b, gfin_b], writes=[hi_b])
                    pg.dma("sp", lambda e, x_ap=x_ap, tt=tt: e.dma_start(out=final["out"][(tt - 1) * P:tt * P, :], in_=x_ap),
                           reads=[hi_b])

        front(0)
        for gi in range(len(groups)):
            mm1(gi)
            if gi + 1 < len(groups):
                front(gi + 1)
            mm2(gi)
        pg.barrier()


_IDENT = {}


def cx_ident(nc):
    return _IDENT[id(nc)]


GAIN_COLS = {"mix0": 0, "ffn0": 8, "mix1": 16, "ffn1": 24, "gout": 32, "gkv": 40}


def build(SEQ, phases, ext_in=(), ext_out=()):
    nc = bass.Bass("TRN2", target_bir_lowering=False)
    NTT = 1 + SEQ // P
    R = NTT * P

    def dram(name, shape, dt, kind="Internal"):
        if name in ext_in:
            kind = "ExternalInput"
        elif name in ext_out:
            kind = "ExternalOutput"
        return nc.dram_tensor(name, list(shape), dt, kind=kind).ap()

    W = {}
    W["gainT"] = dram("gainT", [P, 48], F32, "ExternalInput")
    W["identf"] = dram("identf", [P, P], F32, "ExternalInput")
    _IDENT[id(nc)] = W["identf"]
    W["ffn_w_in0"] = dram("ffn_w_in0", [D, 2 * DFF], F32, "ExternalInput")
    W["ffn_w_out0"] = dram("ffn_w_out0", [DFF, D], F32, "ExternalInput")
    W["ffn_w_in1"] = dram("ffn_w_in1", [D, 2 * DFF], F32, "ExternalInput")
    W["ffn_w_out1"] = dram("ffn_w_out1", [DFF, D], F32, "ExternalInput")
    W["gfinB"] = dram("gfinB", [P, D], F32, "ExternalInput")
    W["gla_w_in"] = dram("gla_w_in", [D, 3072], F32, "ExternalInput")
    W["gla_w_a1"] = dram("gla_w_a1", [D, 16], F32, "ExternalInput")
    W["gla_w_a2aug"] = dram("gla_w_a2aug", [17, 512], F32, "ExternalInput")
    W["gla_w_out"] = dram("gla_w_out", [D, D], F32, "ExternalInput")
    consts = {}
    consts["Dm"] = dram("c_Dm", [P, 2, P], F32, "ExternalInput")
    consts["Ind"] = dram("c_Ind", [P, 2, 2], F32, "ExternalInput")
    x = dram("x", [SEQ, D], F32, "ExternalInput")
    meta = dram("meta", [16, D], F32, "ExternalInput")
    W["dsa_w_in"] = dram("dsa_w_in", [D, DSA_IN], F32, "ExternalInput")
    W["dsa_w_ukT"] = dram("dsa_w_ukT", [P, 2048], F32, "ExternalInput")
    W["dsa_w_uv"] = dram("dsa_w_uv", [8, 256, P], F32, "ExternalInput")
    W["dsa_w_out"] = dram("dsa_w_out", [D, D], F32, "ExternalInput")
    W["gkvB"] = dram("gkvB", [P, 256], F32, "ExternalInput")
    consts["m0"] = dram("c_m0", [P, P], F32, "ExternalInput")
    consts["mdiag"] = dram("c_mdiag", [P, P], F32, "ExternalInput")
    consts["b15"] = dram("c_b15", [P, 1024], F32, "ExternalInput")
    consts["pow2"] = dram("c_pow2", [P, 32], F32, "ExternalInput")
    consts["biasT"] = dram("c_biasT", [3, P, 1024], F32, "ExternalInput")
    scr = {}
    scr["qlat"] = dram("s_qlat", [NTT, P, 2048], BF16)
    scr["qi"] = dram("s_qi", [NTT, P, 1024], BF16)
    scr["oT"] = dram("s_oT", [NTT, P, 1024], BF16)
    h1 = dram("h1", [R, D], F32)
    h2 = dram("h2", [R, D], F32)
    h3 = dram("h3", [R, D], F32)
    out = dram("out", [SEQ, D], F32, "ExternalOutput" if "E" in phases else "Internal")

    pg = Prog(nc)
    if "A" in phases:
        phase_gla(nc, pg, NTT, x, meta, h1, W, consts)
    if "B" in phases:
        phase_ffn(nc, pg, NTT, h1, h2, W["ffn_w_in0"], W["ffn_w_out0"], W["gainT"], GAIN_COLS["ffn0"])
    if "C" in phases or "D" in phases:
        with contextlib.ExitStack() as cstack:
            ccx = Ctx(nc, pg, cstack)
            caches = {
                "kidx2": (ccx.sb([P, R], BF16, "kidx2"), Buf("kidx2")),
                "cT": (ccx.sb([P, 2, R], BF16, "cT"), Buf("cT")),
                "ctok": (ccx.sb([P, NTT, 258], BF16, "ctok"), Buf("ctok")),
                "wabs": (ccx.sb([P, NTT, 8], F32, "wabs"), Buf("wabs")),
                "sgn": (ccx.sb([P, NTT, 8], F32, "sgn"), Buf("sgn")),
            }
            if "C" in phases:
                phase_dsa_proj(nc, pg, NTT, h2, W, consts, caches, scr)
            if "D" in phases:
                phase_dsa_attn(nc, pg, NTT, W, consts, caches, scr)
    if "F" in phases:
        phase_dsa_out(nc, pg, NTT, h2, h3, W, scr)
    if "E" in phases:
        phase_ffn(nc, pg, NTT, h3, None, W["ffn_w_in1"], W["ffn_w_out1"], W["gainT"], GAIN_COLS["ffn1"],
                  final=dict(out=out, gB=W["gfinB"]))
    pg.emit()
    LAST['nops'] = pg.nops
    return nc


def phase_gla(nc, pg, NTT, x, meta, h_out, W, consts):
    KC = D // P
    H = 4
    with contextlib.ExitStack() as stack:
        cx = Ctx(nc, pg, stack)
        Win = cx.sb([P, KC, 3072], BF16, "Win"); Win_b = Buf("Win")
        Wa1 = cx.sb([P, KC, 16], BF16, "Wa1"); Wa1_b = Buf("Wa1")
        Wa2 = cx.sb([17, 512], F32, "Wa2"); Wa2_b = Buf("Wa2")
        Wout = cx.sb([P, KC, D], BF16, "Wout"); Wout_b = Buf("Wout")
        stage = [(cx.sb([P, 1024], F32, "stg"), Buf("stg")) for _ in range(5)]
        gT = cx.sb([P, 48], F32, "gT"); gT_b = Buf("gT")
        ident = cx.sb([P, P], BF16, "ident"); ident_b = Buf("ident")
        identf = cx.sb([P, P], F32, "identf"); identf_b = Buf("identf")
        Dm = cx.sb([P, 2, P], F32, "Dm"); Dm_b = Buf("Dm")
        Ind = cx.sb([P, 2, 2], F32, "Ind"); Ind_b = Buf("Ind")
        epsT = cx.sb([P, 1], F32, "eps"); eps_b = Buf("eps")
        cx.eps_ap = epsT[:, 0:1]
        pg.op("pool", lambda e: e.memset(epsT[:], EPS), writes=[eps_b])
        pg.dma("sp", lambda e: e.dma_start(out=gT[:], in_=W["gainT"][:, :]), writes=[gT_b])
        pg.dma("sp", lambda e: e.dma_start(out=identf[:], in_=W["identf"][:, :]), writes=[identf_b])
        pg.dma("sp", lambda e: e.dma_start(out=Dm[:], in_=consts["Dm"][:, :, :]), writes=[Dm_b])
        pg.dma("sp", lambda e: e.dma_start(out=Ind[:], in_=consts["Ind"][:, :, :]), writes=[Ind_b])
        pg.dma("sp", lambda e: e.dma_start(out=Wa2[:], in_=W["gla_w_a2aug"][:, :]), writes=[Wa2_b])
        pg.op("dve", lambda e: e.tensor_copy(out=ident[:], in_=identf[:]), reads=[identf_b], writes=[ident_b])
        for en in ("dve", "act", "pool"):
            pg.wait_tok(en, gT_b.last_w)
        pg.wait_tok("act", eps_b.last_w)
        g0 = GAIN_COLS["mix0"]
        load_weight(cx, Win, Win_b, W["gla_w_in"], KC, 3072, gain=gT[:, g0:g0 + KC], stage=stage)
        load_weight(cx, Wa1, Wa1_b, W["gla_w_a1"], KC, 16, gain=gT[:, g0:g0 + KC], stage=stage)
        go = GAIN_COLS["gout"]
        load_weight(cx, Wout, Wout_b, W["gla_w_out"], KC, D, gain=gT[:, go:go + KC], stage=stage)

        def dbl(shape, dt, nm_, n=2):
            return [(cx.sb(shape, dt, nm_), Buf(nm_)) for _ in range(n)]
        xin = dbl([P, D], F32, "xin", 3)
        hn = dbl([P, D], BF16, "hn")
        hnT = dbl([P, KC, P], BF16, "hnT")
        qT = dbl([P, H, P], F32, "qT")
        kt = dbl([P, 2, 512], F32, "kt")
        vsb = dbl([P, D], F32, "vsb")
        sr = dbl([P, D], F32, "sr")
        etot = dbl([P, H, 2], F32, "etot")
        junk = cx.sb([P, D], BF16, "junk"); junk_b = Buf("junk")
        ss = cx.sb([P, 1], F32, "ss"); ss_b = Buf("ss")
        rstd = cx.sb([P, 1], F32, "rstd"); rstd_b = Buf("rstd")
        ksb = cx.sb([P, 512], F32, "ksb"); ksb_b = Buf("ksb")
        mb = cx.sb([P, 2], F32, "mb"); mb_b = Buf("mb")
        pg.op("pool", lambda e: e.memset(mb[:], NEG), writes=[mb_b])
        pg.op("pool", lambda e: e.memset(mb[0:64, 0:1], 0.0), writes=[mb_b])
        pg.op("pool", lambda e: e.memset(mb[64:128, 1:2], 0.0), writes=[mb_b])
        pg.wait_tok("act", mb_b.last_w)
        a1T = cx.sb([17, P], F32, "a1T"); a1T_b = Buf("a1T")
        e1 = cx.sb([P, 512], F32, "e1"); e1_b = Buf("e1")
        sp_ = cx.sb([P, 512], F32, "sp"); sp_b = Buf("sp")
        ed = cx.sb([P, 2, 512], F32, "ed"); ed_b = Buf("ed")
        S = cx.sb([P, H, 256], F32, "S"); S_b = [Buf("S%d" % h) for h in range(H)]
        osb = cx.sb([P, H, 256], F32, "osb"); osb_b = Buf("osb")
        ss4 = cx.sb([P, H], F32, "ss4"); ss4_b = Buf("ss4")
        rs4 = cx.sb([P, H], F32, "rs4"); rs4_b = Buf("rs4")
        y = cx.sb([P, D], BF16, "y"); y_b = Buf("y")
        yT = cx.sb([P, KC, P], BF16, "yT"); yT_b = Buf("yT")

        def bank(nm_):
            t_ = cx.ps([P, 512], F32, nm_)
            return (t_, Buf(nm_))
        pT32, pT_b = bank("pT")
        pTv = pT32[:, :].bitcast(BF16).rearrange("p (k t) -> p k t", k=KC)
        RB = [bank("pR%d" % i) for i in range(3)]
        KVB = [bank("pK%d" % i) for i in range(2)]
        OB = [bank("pO%d" % i) for i in range(2)]
        rcnt = [0]

        def nextR():
            r = RB[rcnt[0] % 3]
            rcnt[0] += 1
            return r

        pg.op("pool", lambda e: e.memset(S[:], 0.0), writes=S_b)
        pg.op("pool", lambda e: e.memset(a1T[:], 1.0), writes=[a1T_b])

        def front(t):
            xi, xi_b = xin[t % 3]
            hn_, hn_b = hn[t % 2]
            hT, hT_b = hnT[t % 2]
            if t == 0:
                pg.op("pool", lambda e: e.memset(xi[:], 0.0), writes=[xi_b])
                pg.dma("sp", lambda e: e.dma_start(out=xi[48:64, :], in_=meta[:, :]), writes=[xi_b])
            else:
                pg.dma("sp", lambda e: e.dma_start(out=xi[:], in_=x[(t - 1) * P:t * P, :]), writes=[xi_b])
            rms_rstd(cx, xi[:], xi_b, junk[:], junk_b, ss[:, 0:1], ss_b, rstd[:, 0:1], rstd_b, D)
            pg.op("act", lambda e: e.activation(out=hn_[:], in_=xi[:], func=AF.Copy, scale=rstd[:, 0:1]),
                  reads=[xi_b, rstd_b], writes=[hn_b])
            for k in range(KC):
                pg.op("pe", lambda e, k=k: e.transpose(out=pTv[:, k, :], in_=hn_[:, k * P:(k + 1) * P], identity=ident[:]),
                      reads=[hn_b, ident_b], writes=[pT_b], signal=(k == KC - 1))
            pg.op("dve", lambda e: e.tensor_copy(out=hT[:], in_=pTv), reads=[pT_b], writes=[hT_b])

        def proj_groups(t):
            hT, hT_b = hnT[t % 2]
            q_, q_b = qT[t % 2]
            kt_, kt_b = kt[t % 2]
            v_, v_b = vsb[t % 2]
            sr_, sr_b = sr[t % 2]
            et_, et_b = etot[t % 2]
            ci = 1 if t == 0 else 0
            gs = []

            def g_q():
                pb, pb_b = nextR()
                for h in range(H):
                    for k in range(KC):
                        pg.op("pe", lambda e, h=h, k=k: e.matmul(pb[:, h * P:(h + 1) * P], lhsT=Win[:, k, h * P:(h + 1) * P], rhs=hT[:, k, :],
                                                                  start=(k == 0), stop=(k == KC - 1)),
                              reads=[Win_b, hT_b], writes=[pb_b], signal=(h == H - 1 and k == KC - 1))
                pg.op("act", lambda e: e.activation(out=q_[:].rearrange("p h t -> p (h t)"), in_=pb[:, :], func=AF.Copy, scale=float(128 ** -0.5)),
                      reads=[pb_b], writes=[q_b])
            gs.append(g_q)

            def mm512(col0, evac):
                def g():
                    pb, pb_b = nextR()
                    for k in range(KC):
                        pg.op("pe", lambda e, k=k: e.matmul(pb[:, :], lhsT=hT[:, k, :], rhs=Win[:, k, col0:col0 + 512], start=(k == 0), stop=(k == KC - 1)),
                              reads=[Win_b, hT_b], writes=[pb_b], signal=(k == KC - 1))
                    evac(pb, pb_b)
                return g
            gs.append(mm512(512, lambda pb, pb_b: pg.op("dve", lambda e: e.tensor_copy(out=ksb[:], in_=pb[:, :]), reads=[pb_b], writes=[ksb_b])))
            gs.append(mm512(1024, lambda pb, pb_b: pg.op("act", lambda e: e.copy(out=v_[:, 0:512], in_=pb[:, :]), reads=[pb_b], writes=[v_b])))
            gs.append(mm512(1536, lambda pb, pb_b: pg.op("dve", lambda e: e.tensor_copy(out=v_[:, 512:1024], in_=pb[:, :]), reads=[pb_b], writes=[v_b])))
            gs.append(mm512(2048, lambda pb, pb_b: pg.op("act", lambda e: e.activation(out=sr_[:, 0:512], in_=pb[:, :], func=AF.Silu), reads=[pb_b], writes=[sr_b])))
            gs.append(mm512(2560, lambda pb, pb_b: pg.op("act", lambda e: e.activation(out=sr_[:, 512:1024], in_=pb[:, :], func=AF.Silu), reads=[pb_b], writes=[sr_b])))

            def g_a1():
                pb, pb_b = nextR()
                for k in range(KC):
                    pg.op("pe", lambda e, k=k: e.matmul(pb[0:16, 0:P], lhsT=Wa1[:, k, :], rhs=hT[:, k, :], start=(k == 0), stop=(k == KC - 1)),
                          reads=[Wa1_b, hT_b], writes=[pb_b], signal=(k == KC - 1))
                pg.op("dve", lambda e: e.tensor_copy(out=a1T[0:16, :], in_=pb[0:16, 0:P]), reads=[pb_b], writes=[a1T_b])
            gs.append(g_a1)

            def g_z():
                pb, pb_b = nextR()
                pg.op("pe", lambda e: e.matmul(pb[:, :], lhsT=a1T[:, :], rhs=Wa2[:, :], start=True, stop=True), reads=[a1T_b, Wa2_b], writes=[pb_b])
                pg.op("act", lambda e: e.activation(out=e1[:], in_=pb[:, :], func=AF.Exp, scale=-1.0), reads=[pb_b], writes=[e1_b])
                pg.op("act", lambda e: e.activation(out=sp_[:], in_=e1[:], func=AF.Ln, bias=1.0), reads=[e1_b], writes=[sp_b])
            gs.append(g_z)

            def g_dec():
                pb, pb_b = nextR()
                pg.op("pe", lambda e: e.matmul(pb[:, :], lhsT=Dm[:, ci, :], rhs=sp_[:], start=True, stop=True), reads=[Dm_b, sp_b], writes=[pb_b])
                for ch in range(2):
                    pg.op("act", lambda e, ch=ch: e.activation(out=ed[:, ch, :], in_=pb[:, :], func=AF.Exp, bias=mb[:, ch:ch + 1]), reads=[pb_b], writes=[ed_b])
                for ch in range(2):
                    pg.op("dve", lambda e, ch=ch: e.tensor_tensor(out=kt_[:, ch, :], in0=ksb[:], in1=ed[:, ch, :], op=ALU.mult), reads=[ksb_b, ed_b], writes=[kt_b])
            gs.append(g_dec)

            def g_tot(h0):
                def g():
                    for h in (h0, h0 + 1):
                        pb, pb_b = nextR()
                        pg.op("pe", lambda e, h=h, pb=pb: e.matmul(pb[:, 0:2], lhsT=sp_[:, h * P:(h + 1) * P], rhs=Ind[:, ci, :], start=True, stop=True),
                              reads=[sp_b, Ind_b], writes=[pb_b])
                        pg.op("act", lambda e, h=h, pb=pb: e.activation(out=et_[:, h, :], in_=pb[:, 0:2], func=AF.Exp), reads=[pb_b], writes=[et_b])
                return g
            gs.append(g_tot(0))
            gs.append(g_tot(2))
            gs = gs[0:4] + gs[6:11] + gs[4:6]
            return gs

        def scan_steps(t):
            q_, q_b = qT[t % 2]
            kt_, kt_b = kt[t % 2]
            v_, v_b = vsb[t % 2]
            et_, et_b = etot[t % 2]
            steps = []
            for ch in range(1 if t == 0 else 2):
                r0, r1 = ch * 64, (ch + 1) * 64
                for h in range(H):
                    def sa(h=h, ch=ch):
                        kvp, kvb = KVB[h % 2]
                        pg.op("pe", lambda e: e.matmul(kvp[:, 0:256], lhsT=kt_[:, ch, h * P:(h + 1) * P], rhs=v_[:, h * 256:(h + 1) * 256], start=True, stop=True),
                              reads=[kt_b, v_b], writes=[kvb])
                        pg.op("dve", lambda e: e.scalar_tensor_tensor(out=S[:, h, :], in0=S[:, h, :], scalar=et_[:, h, ch:ch + 1], in1=kvp[:, 0:256],
                                                                       op0=ALU.mult, op1=ALU.add), reads=[kvb, et_b, S_b[h]], writes=[S_b[h]])

                    def sb_(h=h, r0=r0, r1=r1):
                        op_, op_b = OB[h % 2]
                        pg.op("pe", lambda e: e.matmul(op_[:, 0:256], lhsT=q_[:, h, :], rhs=S[:, h, :], start=True, stop=True),
                              reads=[q_b, S_b[h]], writes=[op_b])
                        if h % 2 == 0:
                            pg.op("act", lambda e: e.copy(out=osb[r0:r1, h, :], in_=op_[r0:r1, 0:256]), reads=[op_b], writes=[osb_b])
                        else:
                            pg.op("dve", lambda e: e.tensor_copy(out=osb[r0:r1, h, :], in_=op_[r0:r1, 0:256]), reads=[op_b], writes=[osb_b])
                    steps.append(sa)
                    steps.append(sb_)
            return steps

        def tail_a(t):
            xi, xi_b = xin[t % 3]
            sr_, sr_b = sr[t % 2]
            for h in range(H):
                pg.op("act", lambda e, h=h: e.activation(out=junk[:, 0:256], in_=osb[:, h, :], func=AF.Square, accum_out=ss4[:, h:h + 1]),
                      reads=[osb_b], writes=[junk_b, ss4_b])
            pg.op("act", lambda e: e.activation(out=ss4[:], in_=ss4[:], func=AF.Sqrt, scale=1.0 / 256, bias=cx.eps_ap), reads=[ss4_b], writes=[ss4_b])
            pg.op("dve", lambda e: e.reciprocal(out=rs4[:], in_=ss4[:]), reads=[ss4_b], writes=[rs4_b])
            for h in range(H):
                pg.op("dve", lambda e, h=h: e.scalar_tensor_tensor(
                    out=y[:, h * 256:(h + 1) * 256], in0=osb[:, h, :], scalar=rs4[:, h:h + 1], in1=sr_[:, h * 256:(h + 1) * 256],
                    op0=ALU.mult, op1=ALU.mult), reads=[osb_b, rs4_b, sr_b], writes=[y_b])

        def tail_b(t):
            xi, xi_b = xin[t % 3]
            for k in range(KC):
                pg.op("pe", lambda e, k=k: e.transpose(out=pTv[:, k, :], in_=y[:, k * P:(k + 1) * P], identity=ident[:]),
                      reads=[y_b, ident_b], writes=[pT_b], signal=(k == KC - 1))
            pg.op("act", lambda e: e.copy(out=yT[:], in_=pTv), reads=[pT_b], writes=[yT_b])
            for n in range(2):
                pp, pp_b = OB[n]
                for k in range(KC):
                    pg.op("pe", lambda e, k=k, n=n, pp=pp: e.matmul(pp[:, :], lhsT=yT[:, k, :], rhs=Wout[:, k, n * 512:(n + 1) * 512],
                                                                    start=(k == 0), stop=(k == KC - 1)),
                          reads=[Wout_b, yT_b], writes=[pp_b], signal=(k == KC - 1))
                pg.op("dve", lambda e, n=n, pp=pp: e.tensor_tensor(out=xi[:, n * 512:(n + 1) * 512], in0=xi[:, n * 512:(n + 1) * 512], in1=pp[:, :], op=ALU.add),
                      reads=[pp_b, xi_b], writes=[xi_b])
            pg.dma("sp", lambda e: e.dma_start(out=h_out[t * P:(t + 1) * P, :], in_=xi[:]), reads=[xi_b])

        front(0)
        for g in proj_groups(0):
            g()
        for t in range(NTT):
            nxt = t + 1 < NTT
            if nxt:
                front(t + 1)
            A_ = scan_steps(t)
            B_all = proj_groups(t + 1) if nxt else []
            B_, held = B_all[:-2], B_all[-2:]
            ia = ib = 0
            while ia < len(A_) or ib < len(B_):
                if ia < len(A_):
                    A_[ia](); ia += 1
                if ib < len(B_) and (ia % 2 == 1 or ia >= len(A_)):
                    B_[ib](); ib += 1
            tail_a(t)
            for g in held:
                g()
            tail_b(t)
        pg.barrier()


DSA_IN = 1864
IDX_C0 = float((64 ** -0.5) * (8 ** -0.5))


def phase_dsa_proj(nc, pg, NTT, h_in, W, consts, caches, scr):
    KC = D // P
    kidx2, kidx2_b = caches["kidx2"]
    cT, cT_b = caches["cT"]
    ctok, ctok_b = caches["ctok"]
    wabs, wabs_b = caches["wabs"]
    sgn, sgn_b = caches["sgn"]
    with contextlib.ExitStack() as stack:
        cx = Ctx(nc, pg, stack)
        NW = DSA_IN + 64
        Win = cx.sb([P, KC, NW], BF16, "Win"); Win_b = Buf("Win")
        Wuk = cx.sb([P, 1, 2048], BF16, "Wuk"); Wuk_b = Buf("Wuk")
        stage = [(cx.sb([P, 1024], F32, "stg"), Buf("stg")) for _ in range(5)]
        gT = cx.sb([P, 48], F32, "gT"); gT_b = Buf("gT")
        gkvB = cx.sb([P, 256], F32, "gkvB"); gkvB_b = Buf("gkvB")
        ident = cx.sb([P, P], BF16, "ident"); ident_b = Buf("ident")
        identf = cx.sb([P, P], F32, "identf"); identf_b = Buf("identf")
        epsT = cx.sb([P, 1], F32, "eps"); eps_b = Buf("eps")
        cx.eps_ap = epsT[:, 0:1]
        pg.op("pool", lambda e: e.memset(epsT[:], EPS), writes=[eps_b])
        pg.dma("sp", lambda e: e.dma_start(out=gT[:], in_=W["gainT"][:, :]), writes=[gT_b])
        pg.dma("sp", lambda e: e.dma_start(out=gkvB[:], in_=W["gkvB"][:, :]), writes=[gkvB_b])
        pg.dma("sp", lambda e: e.dma_start(out=identf[:], in_=W["identf"][:, :]), writes=[identf_b])
        pg.op("dve", lambda e: e.tensor_copy(out=ident[:], in_=identf[:]), reads=[identf_b], writes=[ident_b])
        for en in ("dve", "act", "pool"):
            pg.wait_tok(en, gT_b.last_w)
        pg.wait_tok("act", eps_b.last_w)
        g0 = GAIN_COLS["mix1"]
        load_weight(cx, Win, Win_b, W["dsa_w_in"], KC, DSA_IN, gain=gT[:, g0:g0 + KC], stage=stage)
        for k in range(KC):
            pg.op("pool", lambda e, k=k: e.tensor_copy(out=Win[:, k, DSA_IN:DSA_IN + 64], in_=Win[:, k, 1792:1856]),
                  reads=[Win_b], writes=[Win_b])
        Wk2 = cx.sb([P, KC, P], BF16, "Wk2"); Wk2_b = Buf("Wk2")
        for k in range(KC):
            pg.op("pool", lambda e, k=k: e.tensor_copy(out=Wk2[:, k, 0:64], in_=Win[:, k, 1792:1856]), reads=[Win_b], writes=[Wk2_b])
            pg.op("pool", lambda e, k=k: e.tensor_copy(out=Wk2[:, k, 64:128], in_=Win[:, k, 1792:1856]), reads=[Win_b], writes=[Wk2_b])
        load_weight(cx, Wuk, Wuk_b, W["dsa_w_ukT"], 1, 2048, gain=None, stage=stage)
        pg.op("pool", lambda e: e.memset(ctok[:, :, 256:258], 1.0), writes=[ctok_b])

        xin = [(cx.sb([P, D], F32, "xin"), Buf("xin")) for _ in range(2)]
        hn2 = [(cx.sb([P, D], BF16, "hn"), Buf("hn")) for _ in range(2)]
        junk = cx.sb([P, D], BF16, "junk"); junk_b = Buf("junk")
        ss = cx.sb([P, 1], F32, "ss"); ss_b = Buf("ss")
        rstd = cx.sb([P, 1], F32, "rstd"); rstd_b = Buf("rstd")
        hnT2 = [(cx.sb([P, KC, P], BF16, "hnT"), Buf("hnT")) for _ in range(2)]
        qT = cx.sb([P, 8, P], BF16, "qT"); qT_b = Buf("qT")
        qlat = [(cx.sb([P, 2, 8, P], BF16, "qlat"), Buf("qlat")) for _ in range(2)]
        qi = [(cx.sb([P, 8, P], BF16, "qi"), Buf("qi")) for _ in range(2)]
        for qit_, qib_ in qi:
            pg.op("pool", lambda e, qit_=qit_: e.memset(qit_[:], 0.0), writes=[qib_])
        csb = cx.sb([P, 256], F32, "csb"); csb_b = Buf("csb")
        ssc = cx.sb([P, 1], F32, "ssc"); ssc_b = Buf("ssc")
        rsc = cx.sb([P, 1], F32, "rsc"); rsc_b = Buf("rsc")
        wsb = cx.sb([P, 8], F32, "wsb"); wsb_b = Buf("wsb")

        pA32 = cx.ps([P, 512], F32, "pA"); pA_b = Buf("pA")
        pA = pA32[:, :].bitcast(BF16).rearrange("p (k t) -> p k t", k=KC)
        pQ = [(cx.ps([P, 512], F32, "pQ"), Buf("pQ")) for _ in range(2)]
        pL = [(cx.ps([P, 512], F32, "pL"), Buf("pL")) for _ in range(2)]
        pM = cx.ps([P, 512], F32, "pM"); pM_b = Buf("pM")
        pI = cx.ps([P, 512], F32, "pI"); pI_b = Buf("pI")

        def front(t):
            xi, xi_b = xin[t % 2]
            hn, hn_b = hn2[t % 2]
            hnT, hnT_b = hnT2[t % 2]
            pg.dma("sp", lambda e: e.dma_start(out=xi[:], in_=h_in[t * P:(t + 1) * P, :]), writes=[xi_b])
            rms_rstd(cx, xi[:], xi_b, junk[:], junk_b, ss[:, 0:1], ss_b, rstd[:, 0:1], rstd_b, D)
            pg.op("act", lambda e: e.activation(out=hn[:], in_=xi[:], func=AF.Copy, scale=rstd[:, 0:1]),
                  reads=[xi_b, rstd_b], writes=[hn_b])
            for k in range(KC):
                pg.op("pe", lambda e, k=k: e.transpose(out=pA[:, k, :], in_=hn[:, k * P:(k + 1) * P], identity=ident[:]),
                      reads=[hn_b, ident_b], writes=[pA_b], signal=(k == KC - 1))
            pg.op("dve", lambda e: e.tensor_copy(out=hnT[:], in_=pA), reads=[pA_b], writes=[hnT_b])

        def tile_body(t):
            hnT, hnT_b = hnT2[t % 2]
            ql, ql_b = qlat[t % 2]
            qit, qi_b = qi[t % 2]
            for half in range(2):
                pq, pq_b = pQ[half]
                for hl in range(4):
                    h = half * 4 + hl
                    for k in range(KC):
                        pg.op("pe", lambda e, pq=pq, hl=hl, h=h, k=k: e.matmul(pq[:, hl * P:(hl + 1) * P], lhsT=Win[:, k, h * P:(h + 1) * P], rhs=hnT[:, k, :],
                                                                              start=(k == 0), stop=(k == KC - 1)),
                              reads=[Win_b, hnT_b], writes=[pq_b], signal=(hl == 3 and k == KC - 1))
                if half == 0:
                    pg.op("act", lambda e, pq=pq: e.copy(out=qT[:, 0:4, :].rearrange("p h t -> p (h t)"), in_=pq[:, :]), reads=[pq_b], writes=[qT_b])
                else:
                    pg.op("dve", lambda e, pq=pq: e.tensor_copy(out=qT[:, 4:8, :].rearrange("p h t -> p (h t)"), in_=pq[:, :]), reads=[pq_b], writes=[qT_b])
            if t + 1 < NTT:
                front(t + 1)
            for cc in range(2):
                for half in range(2):
                    pl, pl_b = pL[half]
                    for hl in range(4):
                        h = half * 4 + hl
                        pg.op("pe", lambda e, pl=pl, hl=hl, h=h, cc=cc: e.matmul(
                            pl[:, hl * P:(hl + 1) * P], lhsT=Wuk[:, 0, h * 256 + cc * P:h * 256 + (cc + 1) * P], rhs=qT[:, h, :], start=True, stop=True),
                            reads=[Wuk_b, qT_b], writes=[pl_b], signal=(hl == 3))
                    if half == 0:
                        pg.op("act", lambda e, pl=pl, ql=ql, cc=cc: e.activation(out=ql[:, cc, 0:4, :].rearrange("p h t -> p (h t)"), in_=pl[:, :],
                                                                               func=AF.Copy, scale=float(128 ** -0.5)), reads=[pl_b], writes=[ql_b])
                    else:
                        pg.op("dve", lambda e, pl=pl, ql=ql, cc=cc: e.tensor_scalar(out=ql[:, cc, 4:8, :].rearrange("p h t -> p (h t)"), in0=pl[:, :],
                                                                                  scalar1=float(128 ** -0.5), scalar2=None, op0=ALU.mult), reads=[pl_b], writes=[ql_b])
            pg.dma("sp", lambda e, ql=ql, t=t: e.dma_start(out=scr["qlat"][t], in_=ql[:].rearrange("p a h t -> p (a h t)")), reads=[ql_b])
            for k in range(KC):
                pg.op("pe", lambda e, k=k: e.matmul(pM[:, 0:256], lhsT=hnT[:, k, :], rhs=Win[:, k, 1024:1280], start=(k == 0), stop=(k == KC - 1)),
                      reads=[Win_b, hnT_b], writes=[pM_b], signal=(k == KC - 1))
            pg.op("dve", lambda e: e.tensor_copy(out=csb[:], in_=pM[:, 0:256]), reads=[pM_b], writes=[csb_b])
            rms_rstd(cx, csb[:], csb_b, junk[:, 0:256], junk_b, ssc[:, 0:1], ssc_b, rsc[:, 0:1], rsc_b, 256)
            pg.op("dve", lambda e, t=t: e.scalar_tensor_tensor(out=ctok[:, t, 0:256], in0=csb[:], scalar=rsc[:, 0:1], in1=gkvB[:],
                                                               op0=ALU.mult, op1=ALU.mult), reads=[csb_b, rsc_b, gkvB_b], writes=[ctok_b])
            for cc in range(2):
                pg.op("pe", lambda e, cc=cc, t=t: e.transpose(out=pA[:, cc, :], in_=ctok[:, t, cc * P:(cc + 1) * P], identity=ident[:]),
                      reads=[ctok_b, ident_b], writes=[pA_b], signal=(cc == 1))
            pg.op("act", lambda e, t=t: e.copy(out=cT[:, :, t * P:(t + 1) * P], in_=pA[:, 0:2, :]), reads=[pA_b], writes=[cT_b])
            for p in range(4):
                for k in range(KC):
                    pg.op("pe", lambda e, p=p, k=k: e.matmul(pI[:, p * P:(p + 1) * P], lhsT=Win[:, k, 1280 + p * P:1280 + (p + 1) * P], rhs=hnT[:, k, :],
                                                              start=(k == 0), stop=(k == KC - 1)),
                          reads=[Win_b, hnT_b], writes=[pI_b], signal=(p == 3 and k == KC - 1))
            pg.op("act", lambda e, qit=qit: e.copy(out=qit[0:64, :, :].rearrange("p (a two) t -> p a two t", two=2)[:, :, 0, :],
                                                   in_=pI[0:64, :].rearrange("p (a t) -> p a t", a=4)), reads=[pI_b], writes=[qi_b])
            pg.op("dve", lambda e, qit=qit: e.tensor_copy(out=qit[64:128, :, :].rearrange("p (a two) t -> p a two t", two=2)[:, :, 1, :],
                                                          in_=pI[64:128, :].rearrange("p (a t) -> p a t", a=4)), reads=[pI_b], writes=[qi_b])
            pg.dma("sp", lambda e, qit=qit, t=t: e.dma_start(out=scr["qi"][t], in_=qit[:].rearrange("p a t -> p (a t)")), reads=[qi_b])
            for k in range(KC):
                pg.op("pe", lambda e, k=k: e.matmul(pM[:, 256:384], lhsT=Wk2[:, k, :], rhs=hnT[:, k, :], start=(k == 0), stop=(k == KC - 1)),
                      reads=[Wk2_b, hnT_b], writes=[pM_b], signal=(k == KC - 1))
            pg.op("dve", lambda e, t=t: e.tensor_copy(out=kidx2[:, t * P:(t + 1) * P], in_=pM[:, 256:384]), reads=[pM_b], writes=[kidx2_b])
            for k in range(KC):
                pg.op("pe", lambda e, k=k: e.matmul(pM[:, 384:392], lhsT=hnT[:, k, :], rhs=Win[:, k, 1856:1864], start=(k == 0), stop=(k == KC - 1)),
                      reads=[Win_b, hnT_b], writes=[pM_b], signal=(k == KC - 1))
            pg.op("dve", lambda e: e.tensor_copy(out=wsb[:], in_=pM[:, 384:392]), reads=[pM_b], writes=[wsb_b])
            pg.op("act", lambda e, t=t: e.activation(out=wabs[:, t, :], in_=wsb[:], func=AF.Abs, scale=IDX_C0),
                  reads=[wsb_b], writes=[wabs_b])
            pg.op("act", lambda e, t=t: e.activation(out=sgn[:, t, :], in_=wsb[:], func=AF.Sign), reads=[wsb_b], writes=[sgn_b])
        front(0)
        for t in range(NTT):
            tile_body(t)
        pg.barrier()


NIT = 14
TOPK = 256


def phase_dsa_attn(nc, pg, NTT, W, consts, caches, scr):
    kidx2, kidx2_b = caches["kidx2"]
    cT, cT_b = caches["cT"]
    ctok, ctok_b = caches["ctok"]
    wabs, wabs_b = caches["wabs"]
    sgn, sgn_b = caches["sgn"]
    SMAX = NTT * P
    AX = mybir.AxisListType.X
    with contextlib.ExitStack() as stack:
        cx = Ctx(nc, pg, stack)
        Wuv = cx.sb([P, 2, 8, P], BF16, "Wuv"); Wuv_b = Buf("Wuv")
        ident = cx.sb([P, P], BF16, "ident"); ident_b = Buf("ident")
        identf = cx.sb([P, P], F32, "identf"); identf_b = Buf("identf")
        m0 = cx.sb([P, P], F32, "m0"); m0_b = Buf("m0")
        mdiag = cx.sb([P, P], F32, "mdiag"); mdiag_b = Buf("mdiag")
        scores = cx.sb([P, max(SMAX, 2048)], F32, "scores"); sc_b = Buf("scores")
        biasf = scores[:, 0:1024]; biasf_b = Buf("biasf")
        b15f = scores[:, 1024:2048]; b15f_b = Buf("b15f")
        bias = cx.sb([P, 3, 2, 1024], BF16, "bias"); bias_b = Buf("bias")
        mone = cx.sb([P, 8], F32, "mone"); mone_b = Buf("mone")
        pg.op("pool", lambda e: e.memset(mone[:], -1.0), writes=[mone_b])
        pg.dma("sp", lambda e: e.dma_start(out=identf[:], in_=W["identf"][:, :]), writes=[identf_b])
        pg.dma("sp", lambda e: e.dma_start(out=m0[:], in_=consts["m0"][:, :]), writes=[m0_b])
        pg.dma("sp", lambda e: e.dma_start(out=mdiag[:], in_=consts["mdiag"][:, :]), writes=[mdiag_b])
        pg.dma("sp", lambda e: e.dma_start(out=b15f, in_=consts["b15"][:, :]), writes=[b15f_b])
        pg.op("dve", lambda e: e.tensor_copy(out=ident[:], in_=identf[:]), reads=[identf_b], writes=[ident_b])
        for wi in range(3):
            pg.dma("sp", lambda e, wi=wi: e.dma_start(out=biasf, in_=consts["biasT"][wi]), writes=[biasf_b])
            pg.op("dve", lambda e: e.tensor_tensor(out=biasf, in0=biasf, in1=b15f, op=ALU.subtract), reads=[biasf_b, b15f_b], writes=[biasf_b])
            pg.op("dve", lambda e, wi=wi: e.tensor_copy(out=bias[:, wi, 0, :], in_=biasf), reads=[biasf_b], writes=[bias_b])
            pg.op("dve", lambda e, wi=wi: e.tensor_tensor(out=biasf, in0=biasf, in1=bias[:, wi, 0, :], op=ALU.subtract), reads=[biasf_b, bias_b], writes=[biasf_b])
            pg.op("dve", lambda e, wi=wi: e.tensor_copy(out=bias[:, wi, 1, :], in_=biasf), reads=[biasf_b], writes=[bias_b])
        wuv_v = W["dsa_w_uv"].rearrange("h (cc p) d -> p cc h d", p=P)
        stage = [(biasf, biasf_b), (b15f, b15f_b)]
        for cc in range(2):
            st, stb = stage[cc]
            pg.dma("sp", lambda e, st=st, cc=cc: e.dma_start(out=st.rearrange("p (h d) -> p h d", h=8), in_=wuv_v[:, cc, :, :]), writes=[stb])
            pg.op("dve", lambda e, st=st, cc=cc: e.tensor_copy(out=Wuv[:, cc, :, :].rearrange("p h d -> p (h d)"), in_=st), reads=[stb], writes=[Wuv_b])

        nm = cx.sb([P, SMAX], BF16, "nm"); nm_b = Buf("nm")
        nmT = cx.sb([P, NTT, P], BF16, "nmT"); nmT_b = Buf("nmT")
        qlat = [(cx.sb([P, 2, 8, P], BF16, "qlat"), Buf("qlat")) for _ in range(2)]
        qi = [(cx.sb([P, 8, P], BF16, "qi"), Buf("qi")) for _ in range(2)]
        dsg = [(cx.sb([P, 8, P], BF16, "dsg"), [Buf("dsg%d" % h) for h in range(8)]) for _ in range(2)]
        NA = 2
        A = [(cx.sb([P, 1024], BF16, "A"), Buf("A")) for _ in range(NA)]
        NKB = (SMAX + 511) // 512
        mxs = cx.sb([P, NKB], F32, "mxs"); mxs_b = Buf("mxs")
        mns = cx.sb([P, NKB], F32, "mns"); mns_b = Buf("mns")
        pow2 = cx.sb([P, 32], F32, "pow2"); pow2_b = Buf("pow2")
        ds = cx.sb([P, 32], F32, "ds"); ds_b = Buf("ds")
        tq = cx.sb([P, 1], F32, "tq"); tq_b = Buf("tq")
        pg.dma("sp", lambda e: e.dma_start(out=pow2[:], in_=consts["pow2"][:, :]), writes=[pow2_b])
        NPT = 5
        PT = [(cx.sb([P, 512], BF16, "PT"), Buf("PT")) for _ in range(NPT)]
        lo = cx.sb([P, 1], F32, "lo"); lo_b = Buf("lo")
        wd = cx.sb([P, 1], F32, "wd"); wd_b = Buf("wd")
        mid = cx.sb([P, 1], F32, "mid"); mid_b = Buf("mid")
        cnt = cx.sb([P, 1], F32, "cnt"); cnt_b = Buf("cnt")
        pw = cx.sb([P, 1], F32, "pw"); pw_b = Buf("pw")
        den = cx.sb([P, 8], F32, "den"); den_b = Buf("den")
        rden = cx.sb([P, 8], F32, "rden"); rden_b = Buf("rden")
        Un = cx.sb([P, 8, 256], BF16, "Un"); Un_b = Buf("Un")
        UnT = cx.sb([P, 16, P], BF16, "UnT"); UnT_b = Buf("UnT")
        oT = [(cx.sb([P, 8, P], BF16, "oT"), Buf("oT")) for _ in range(2)]

        def bank(nm_):
            t_ = cx.ps([P, 512], F32, nm_)
            return (t_, Buf(nm_), t_[:, :].bitcast(BF16).rearrange("p (k t) -> p k t", k=8))
        def bank2(nm_):
            t_ = cx.ps([P, 1024], F32, nm_)
            b0 = (t_[:, 0:512], Buf(nm_ + "a"), t_[:, 0:512].bitcast(BF16).rearrange("p (k t) -> p k t", k=8))
            b1 = (t_[:, 512:1024], Buf(nm_ + "b"), t_[:, 512:1024].bitcast(BF16).rearrange("p (k t) -> p k t", k=8))
            return t_, b0, b1
        pXX, pX0, pX1 = bank2("pXX")
        pUU, pU0, pU1 = bank2("pUU")
        pS, pU2, pU3, pDn = [bank(n_) for n_ in ("pS", "pU2", "pU3", "pDn")]
        X2 = [(pXX, pX0, pX1), (pUU, pU0, pU1)]
        SCB = [pS, pU2, pU3, pDn]
        TB = [pU3, pDn]
        LB = [pX0, pX1, pS]
        UB = [pU0, pU1, pU2, pU3]

        def build_dsg(j):
            dt_, db_ = dsg[j % 2]
            for h in range(8):
                pg.op("dve", lambda e, h=h, j=j, dt_=dt_: e.tensor_scalar(out=dt_[:, h, :], in0=ident[:], scalar1=sgn[:, j, h:h + 1], scalar2=None, op0=ALU.mult),
                      reads=[ident_b, sgn_b], writes=[db_[h]])

        def load_q(j):
            ql, ql_b = qlat[j % 2]
            qit, qi_b = qi[j % 2]
            pg.dma("sp", lambda e, qit=qit, j=j: e.dma_start(out=qit[:].rearrange("p a t -> p (a t)"), in_=scr["qi"][j]), writes=[qi_b])
            pg.dma("sp", lambda e, ql=ql, j=j: e.dma_start(out=ql[:].rearrange("p a h t -> p (a h t)"), in_=scr["qlat"][j]), writes=[ql_b])

        def stage_I(j):
            S = (j + 1) * P
            qit, qi_b = qi[j % 2]
            dt_, db_ = dsg[j % 2]
            nkb = (S + 511) // 512
            npair = (nkb + 1) // 2
            units = [(kp, h) for kp in range(npair) for h in range(8)]
            DX = 1
            n = len(units)

            def cols(kp):
                c0 = kp * 1024
                return c0, min(1024, S - c0)

            def emit_X(i):
                kp, h = units[i]
                c0, cw = cols(kp)
                xx, xa, xb = X2[i % 2]
                for q_, xq in enumerate((xa, xb)):
                    w_ = min(512, cw - q_ * 512)
                    if w_ <= 0:
                        continue
                    pg.op("pe", lambda e, xq=xq, h=h, c0=c0, q_=q_, w_=w_: e.matmul(
                        xq[0][:, 0:w_], lhsT=qit[:, h, :], rhs=kidx2[:, c0 + q_ * 512:c0 + q_ * 512 + w_], start=True, stop=True),
                        reads=[qi_b, kidx2_b], writes=[xq[1]])

            def emit_R(i):
                kp, h = units[i]
                c0, cw = cols(kp)
                xx, xa, xb = X2[i % 2]
                At, A_b = A[i % NA]
                rb = [xa[1]] + ([xb[1]] if cw > 512 else [])
                pg.op("act", lambda e, xx=xx, At=At, cw=cw, h=h: e.activation(out=At[:, 0:cw], in_=xx[:, 0:cw], func=AF.Relu, scale=wabs[:, j, h:h + 1]),
                      reads=rb + [wabs_b], writes=[A_b])
                for q_ in range(2):
                    w_ = min(512, cw - q_ * 512)
                    if w_ <= 0:
                        continue
                    sc, sc_pb, _ = SCB[(2 * kp + q_) % 4]
                    pg.op("pe", lambda e, At=At, w_=w_, h=h, sc=sc, q_=q_: e.matmul(sc[:, 0:w_], lhsT=dt_[:, h, :], rhs=At[:, q_ * 512:q_ * 512 + w_],
                                                                                  start=(h == 0), stop=(h == 7)),
                          reads=[A_b, db_[h]], writes=[sc_pb], signal=(h == 7))
                    if h == 7:
                        kb = 2 * kp + q_
                        cc0 = c0 + q_ * 512
                        pg.op("dve", lambda e, cc0=cc0, w_=w_, sc=sc, kb=kb: e.tensor_scalar(out=scores[:, cc0:cc0 + w_], in0=sc[:, 0:w_], scalar1=1.0, scalar2=None,
                                                                                         op0=ALU.mult, op1=ALU.max, accum_out=mxs[:, kb:kb + 1]),
                              reads=[sc_pb], writes=[sc_b, mxs_b])
                        pg.op("dve", lambda e, cc0=cc0, w_=w_, kb=kb: e.tensor_reduce(out=mns[:, kb:kb + 1], in_=scores[:, cc0:cc0 + w_], op=ALU.min, axis=AX),
                              reads=[sc_b], writes=[mns_b])

            for i in range(n + DX):
                if i < n:
                    emit_X(i)
                if i - DX >= 0:
                    emit_R(i - DX)
            pg.op("pool", lambda e: e.tensor_tensor(out=scores[:, 0:P], in0=scores[:, 0:P], in1=m0[:], op=ALU.add), reads=[sc_b, m0_b, mns_b], writes=[sc_b])
            if j >= 1:
                pg.op("pool", lambda e: e.tensor_tensor(out=scores[:, j * P:(j + 1) * P], in0=scores[:, j * P:(j + 1) * P], in1=mdiag[:], op=ALU.add),
                      reads=[sc_b, mdiag_b], writes=[sc_b])
            pg.op("dve", lambda e: e.tensor_reduce(out=lo[:], in_=mns[:, 0:nkb], op=ALU.min, axis=AX), reads=[mns_b], writes=[lo_b])
            pg.op("dve", lambda e: e.tensor_reduce(out=wd[:], in_=mxs[:, 0:nkb], op=ALU.max, axis=AX), reads=[mxs_b], writes=[wd_b])
            pg.op("dve", lambda e: e.tensor_tensor(out=wd[:], in0=wd[:], in1=lo[:], op=ALU.subtract), reads=[wd_b, lo_b], writes=[wd_b])
            pg.op("dve", lambda e: e.tensor_scalar(out=tq[:], in0=wd[:], scalar1=0.001, scalar2=1e-6, op0=ALU.mult, op1=ALU.add), reads=[wd_b], writes=[tq_b])
            pg.op("dve", lambda e: e.tensor_tensor(out=lo[:], in0=lo[:], in1=tq[:], op=ALU.subtract), reads=[lo_b, tq_b], writes=[lo_b])
            pg.op("dve", lambda e: e.tensor_scalar(out=wd[:], in0=wd[:], scalar1=1.002, scalar2=2e-6, op0=ALU.mult, op1=ALU.add), reads=[wd_b], writes=[wd_b])
            pg.op("dve", lambda e: e.tensor_scalar(out=ds[:], in0=pow2[:], scalar1=wd[:, 0:1], scalar2=None, op0=ALU.mult), reads=[wd_b, pow2_b], writes=[ds_b])
            pg.op("dve", lambda e: e.tensor_tensor(out=mid[:], in0=lo[:], in1=ds[:, 0:1], op=ALU.add), reads=[lo_b, ds_b], writes=[mid_b])

        def stage_B(j):
            S = (j + 1) * P
            for it in range(NIT):
                pg.op("dve", lambda e: e.tensor_scalar(out=nm[:, 0:S], in0=scores[:, 0:S], scalar1=mid[:, 0:1], scalar2=None, op0=ALU.is_ge, op1=ALU.add,
                                                       accum_out=cnt[:, 0:1]), reads=[sc_b, mid_b], writes=[nm_b, cnt_b])
                pg.op("dve", lambda e, it=it: e.tensor_scalar(out=tq[:], in0=cnt[:], scalar1=TOPK - 0.5, scalar2=ds[:, it:it + 1], op0=ALU.is_ge, op1=ALU.mult),
                      reads=[cnt_b, ds_b], writes=[tq_b])
                pg.op("dve", lambda e, it=it: e.scalar_tensor_tensor(out=mid[:], in0=tq[:], scalar=ds[:, it + 1:it + 2], in1=mid[:], op0=ALU.subtract, op1=ALU.add),
                      reads=[tq_b, ds_b, mid_b], writes=[mid_b])
            pg.op("dve", lambda e: e.tensor_scalar(out=nm[:, 0:S], in0=scores[:, 0:S], scalar1=ds[:, NIT:NIT + 1], scalar2=mid[:, 0:1], op0=ALU.add, op1=ALU.is_ge),
                  reads=[sc_b, ds_b, mid_b], writes=[nm_b])

        def stage_T(j):
            ib = 0
            for k0 in range(0, j + 1, 8):
                kn = min(8, j + 1 - k0)
                tb, tb_b, tbv = TB[ib % 2]
                ib += 1
                for kk in range(kn):
                    pg.op("pe", lambda e, kk=kk, k0=k0, tbv=tbv: e.transpose(out=tbv[:, kk, :], in_=nm[:, (k0 + kk) * P:(k0 + kk + 1) * P], identity=ident[:]),
                          reads=[nm_b, ident_b], writes=[tb_b], signal=(kk == kn - 1))
                pg.op("act", lambda e, k0=k0, kn=kn, tbv=tbv: e.copy(out=nmT[:, k0:k0 + kn, :], in_=tbv[:, 0:kn, :]), reads=[tb_b], writes=[nmT_b])

        def stage_W(j):
            ql, ql_b = qlat[j % 2]
            units = [(kt, half) for kt in range(j + 1) for half in range(2)]
            n = len(units)
            DL = 2

            def emit_QK(i):
                kt, half = units[i]
                wi = None
                if kt == j:
                    wi = 0
                elif kt == j - 1 and j >= 2:
                    wi = 1
                elif j == 1 and kt == 0:
                    wi = 2
                px, px_b, _ = LB[i % 3]
                for cc in range(2):
                    pg.op("pe", lambda e, px=px, cc=cc, kt=kt, half=half: e.matmul(
                        px[:, :], lhsT=cT[:, cc, kt * P:(kt + 1) * P], rhs=ql[:, cc, half * 4:half * 4 + 4, :].rearrange("p h t -> p (h t)"),
                        start=(cc == 0), stop=(cc == 1 and wi is None)), reads=[cT_b, ql_b], writes=[px_b], signal=(cc == 1 and wi is None))
                if wi is not None:
                    for hl in range(2):
                        pg.op("pe", lambda e, px=px, wi=wi, hl=hl, half=half: e.matmul(px[:, :], lhsT=ident[:], rhs=bias[:, wi, hl, half * 512:(half + 1) * 512],
                                                                                    start=False, stop=(hl == 1)), reads=[ident_b, bias_b], writes=[px_b], signal=(hl == 1))

            def emit_PV(i):
                kt, half = units[i]
                px, px_b, _ = LB[i % 3]
                Pt, Pt_b = PT[i % NPT]
                pg.op("act", lambda e, px=px, Pt=Pt: e.activation(out=Pt[:], in_=px[:, :], func=AF.Exp), reads=[px_b], writes=[Pt_b])
                pg.op("pool", lambda e, Pt=Pt, kt=kt: e.tensor_tensor(out=Pt[:].rearrange("p (h t) -> p h t", h=4), in0=Pt[:].rearrange("p (h t) -> p h t", h=4),
                                                                    in1=nmT[:, kt:kt + 1, :].broadcast_to([P, 4, P]), op=ALU.mult),
                      reads=[Pt_b, nmT_b], writes=[Pt_b])
                for hl in range(4):
                    h = half * 4 + hl
                    pu, pu_b, _ = UB[h // 2]
                    first = (kt == 0 and h % 2 == 0)
                    pg.op("pe", lambda e, pu=pu, h=h, hl=hl, Pt=Pt, kt=kt, first=first: e.matmul(
                        pu[:, (h % 2) * 256:(h % 2 + 1) * 256], lhsT=Pt[:, hl * P:(hl + 1) * P], rhs=ctok[:, kt, 0:256], start=first, stop=(kt == j),
                        skip_group_check=True), reads=[Pt_b, ctok_b], writes=[pu_b], signal=False)
                    firstd = (kt == 0 and h == 0)
                    pg.op("pe", lambda e, h=h, hl=hl, Pt=Pt, kt=kt, firstd=firstd: e.matmul(
                        pDn[0][:, 2 * h:2 * h + 2], lhsT=Pt[:, hl * P:(hl + 1) * P], rhs=ctok[:, kt, 256:258], start=firstd, stop=(kt == j),
                        skip_group_check=True), reads=[Pt_b, ctok_b], writes=[pDn[1]], signal=(hl == 3))

            for i in range(n + DL):
                if i < n:
                    emit_QK(i)
                if i - DL >= 0:
                    emit_PV(i - DL)

        def stage_Z(j):
            pg.op("act", lambda e: e.copy(out=den[:], in_=pDn[0][:, 0:16].rearrange("p (h two) -> p h two", two=2)[:, :, 0]), reads=[pDn[1]], writes=[den_b])
            pg.op("pool", lambda e: e.tensor_tensor(out=rden[:], in0=den[:], in1=mone[:], op=ALU.pow), reads=[den_b, mone_b], writes=[rden_b])
            for h in range(8):
                pu, pu_b, _ = UB[h // 2]
                pg.op("act", lambda e, pu=pu, h=h: e.activation(out=Un[:, h, :], in_=pu[:, (h % 2) * 256:(h % 2 + 1) * 256], func=AF.Copy, scale=rden[:, h:h + 1]),
                      reads=[pu_b, rden_b], writes=[Un_b])
            for g in range(2):
                tb, tb_b, tbv = (pX0, pX1)[g]
                for kk in range(8):
                    idx = g * 8 + kk
                    h, cc = idx // 2, idx % 2
                    pg.op("pe", lambda e, kk=kk, h=h, cc=cc, tbv=tbv: e.transpose(out=tbv[:, kk, :], in_=Un[:, h, cc * P:(cc + 1) * P], identity=ident[:]),
                          reads=[Un_b, ident_b], writes=[tb_b], signal=(kk == 7))
                pg.op("act", lambda e, g=g, tbv=tbv: e.copy(out=UnT[:, g * 8:(g + 1) * 8, :], in_=tbv[:, :, :]), reads=[tb_b], writes=[UnT_b])
            ot, ot_b = oT[j % 2]
            for half in range(2):
                px, px_b, _ = (pS, pX0)[half]
                for hl in range(4):
                    h = half * 4 + hl
                    for cc in range(2):
                        pg.op("pe", lambda e, px=px, hl=hl, h=h, cc=cc: e.matmul(px[:, hl * P:(hl + 1) * P], lhsT=Wuv[:, cc, h, :], rhs=UnT[:, h * 2 + cc, :],
                                                                              start=(cc == 0), stop=(cc == 1)), reads=[Wuv_b, UnT_b], writes=[px_b],
                              signal=(hl == 3 and cc == 1))
                pg.op("act", lambda e, px=px, ot=ot, half=half: e.copy(out=ot[:, half * 4:half * 4 + 4, :].rearrange("p h t -> p (h t)"), in_=px[:, :]),
                      reads=[px_b], writes=[ot_b])
            pg.dma("sp", lambda e, ot=ot, j=j: e.dma_start(out=scr["oT"][j], in_=ot[:].rearrange("p h t -> p (h t)")), reads=[ot_b])

        build_dsg(0)
        load_q(0)
        stage_I(0)
        stage_B(0)
        stage_T(0)
        for j in range(NTT):
            if j + 1 < NTT:
                build_dsg(j + 1)
                load_q(j + 1)
                stage_I(j + 1)
            stage_W(j)
            if j + 1 < NTT:
                stage_B(j + 1)
            stage_Z(j)
            if j + 1 < NTT:
                stage_T(j + 1)
        pg.barrier()


def phase_dsa_out(nc, pg, NTT, h_in, h_out, W, scr):
    with contextlib.ExitStack() as stack:
        cx = Ctx(nc, pg, stack)
        Wo = cx.sb([P, 8, D], BF16, "Wo"); Wo_b = Buf("Wo")
        stage = [(cx.sb([P, 1024], F32, "stg"), Buf("stg")) for _ in range(5)]
        load_weight(cx, Wo, Wo_b, W["dsa_w_out"], 8, D, gain=None, stage=stage)
        xin = [(cx.sb([P, D], F32, "xin"), Buf("xin")) for _ in range(3)]
        ot = [(cx.sb([P, 8, P], BF16, "ot"), Buf("ot")) for _ in range(3)]
        po = [(cx.ps([P, 512], F32, "po"), Buf("po")) for _ in range(4)]
        ic = 0
        for t in range(1, NTT):
            xi, xi_b = xin[t % 3]
            o_, o_b = ot[t % 3]
            pg.dma("sp", lambda e, xi=xi, t=t: e.dma_start(out=xi[:], in_=h_in[t * P:(t + 1) * P, :]), writes=[xi_b])
            pg.dma("sp", lambda e, o_=o_, t=t: e.dma_start(out=o_[:].rearrange("p h t -> p (h t)"), in_=scr["oT"][t]), writes=[o_b])
            for n in range(2):
                pp, pp_b = po[ic % 4]
                ic += 1
                for h in range(8):
                    pg.op("pe", lambda e, pp=pp, h=h, n=n, o_=o_: e.matmul(pp[:, :], lhsT=o_[:, h, :], rhs=Wo[:, h, n * 512:(n + 1) * 512], start=(h == 0), stop=(h == 7)),
                          reads=[o_b, Wo_b], writes=[pp_b], signal=(h == 7))
                pg.op("dve", lambda e, pp=pp, n=n, xi=xi: e.tensor_tensor(out=xi[:, n * 512:(n + 1) * 512], in0=xi[:, n * 512:(n + 1) * 512], in1=pp[:, :], op=ALU.add),
                      reads=[pp_b, xi_b], writes=[xi_b])
            pg.dma("sp", lambda e, xi=xi, t=t: e.dma_start(out=h_out[t * P:(t + 1) * P, :], in_=xi[:]), reads=[xi_b])
        pg.barrier()


import math


def _rel_bucket(rel):
    rel = np.asarray(rel, np.int64)
    nb = 16
    max_exact = 8
    ret = np.where(rel > 0, nb, 0)
    n = np.abs(rel)
    nf = np.maximum(n, 1).astype(np.float32)
    large = max_exact + (np.log(nf / np.float32(max_exact)) / np.float32(math.log(128 / max_exact))
                         * np.float32(nb - max_exact)).astype(np.int32)
    large = np.minimum(large, nb - 1)
    return ret + np.where(n < max_exact, n, large)


def _index_consts():
    c = {}
    Dm = np.zeros((128, 2, 128), np.float32)
    cp = np.arange(128)[:, None]
    cc = np.arange(128)[None, :]
    Dm[:, 0, :] = np.where((cp // 64 == cc // 64) & (cp > cc), -1.0 / 16, 0.0)
    Dm[:, 1, :] = np.where((cp >= 48) & (cp < 64) & (cc >= 48) & (cc < 64) & (cp > cc), -1.0 / 16, 0.0)
    Ind = np.zeros((128, 2, 2), np.float32)
    Ind[np.arange(128), 0, np.arange(128) // 64] = -1.0 / 16
    Ind[48:64, 1, 0] = -1.0 / 16
    c["c_Dm"] = Dm
    c["c_Ind"] = Ind
    m0 = np.full((128, 128), NEG, np.float32)
    m0[:, 48:64] = 0
    md = np.zeros((128, 128), np.float32)
    md[0:64, 64:128] = NEG
    c["c_m0"] = m0
    c["c_mdiag"] = md
    c["identf"] = np.eye(128, dtype=np.float32)
    c["c_pow2"] = np.ascontiguousarray(np.broadcast_to((2.0 ** -(np.arange(32) + 1.0)).astype(np.float32)[None, :], (128, 32)))
    return c


def _colchunk(g):
    return np.ascontiguousarray(np.asarray(g, np.float32).reshape(-1, 128).T)


def _prep_shared(inp):
    f = lambda a: np.ascontiguousarray(np.asarray(a, dtype=np.float32))
    sh = _index_consts()
    gainT = np.zeros((128, 48), np.float32)
    gainT[:, 0:8] = _colchunk(inp["norm_mix"][0])
    gainT[:, 8:16] = _colchunk(inp["norm_ffn"][0])
    gainT[:, 16:24] = _colchunk(inp["norm_mix"][1])
    gainT[:, 24:32] = _colchunk(inp["norm_ffn"][1])
    gainT[:, 32:40] = _colchunk(inp["gla_g_out"][0])
    sh["gainT"] = gainT
    sh["ffn_w_in0"] = f(inp["ffn_w_in"][0]); sh["ffn_w_out0"] = f(inp["ffn_w_out"][0])
    sh["ffn_w_in1"] = f(inp["ffn_w_in"][1]); sh["ffn_w_out1"] = f(inp["ffn_w_out"][1])
    sh["gfinB"] = np.ascontiguousarray(np.broadcast_to(f(inp["norm_final"])[None, :], (128, D)))
    sh["gla_w_in"] = f(inp["gla_w_in"][0]); sh["gla_w_a1"] = f(inp["gla_w_a1"][0])
    sh["gla_w_a2aug"] = np.ascontiguousarray(np.concatenate([f(inp["gla_w_a2"][0]), f(inp["gla_b_a"][0])[None, :]], 0))
    sh["gla_w_out"] = f(inp["gla_w_out"][0])
    sh["meta"] = f(inp["meta"])
    sh["dsa_w_in"] = f(inp["dsa_w_in"][0])
    sh["dsa_w_ukT"] = np.ascontiguousarray(f(inp["dsa_w_uk"][0]).transpose(2, 0, 1)).reshape(128, 2048)
    sh["dsa_w_uv"] = f(inp["dsa_w_uv"][0])
    sh["dsa_w_out"] = f(inp["dsa_w_out"][0])
    sh["gkvB"] = np.ascontiguousarray(np.broadcast_to(f(inp["dsa_g_kv"][0])[None, :], (128, 256)))
    rb = f(inp["rel_bias"])
    s = np.arange(128)[:, None]
    t = np.arange(128)[None, :]
    bt = np.zeros((3, 128, 8, 128), np.float32)
    for wi, off in enumerate((0, -128, -64)):
        bt[wi] = rb[_rel_bucket(s - t + off)].transpose(0, 2, 1)
    sh["c_biasT"] = np.ascontiguousarray(bt.reshape(3, 128, 1024))
    sh["c_b15"] = np.ascontiguousarray(np.broadcast_to(rb[15][None, :, None], (128, 8, 128)).reshape(128, 1024))
    return sh


_NC_CACHE = {}


def kernel(**inputs):
    x = np.asarray(inputs["x"], dtype=np.float32)
    B, SEQ, _ = x.shape
    sh = _prep_shared(inputs)
    key = (SEQ,)
    if key not in _NC_CACHE:
        _NC_CACHE[key] = build(SEQ, "ABCDFE")
    nc = _NC_CACHE[key]
    in_maps = []
    for b in range(B):
        m = dict(sh)
        m["x"] = np.ascontiguousarray(x[b])
        in_maps.append(m)
    res = run_bass_kernel_spmd(nc, in_maps, core_ids=list(range(B)))
    return np.stack([np.asarray(r["out"], dtype=np.float32) for r in res.results], 0)
```

```python
import contextlib
import numpy as np
import concourse.bass as bass
import concourse.mybir as mybir
from concourse.bass_utils import run_bass_kernel_spmd

F32 = mybir.dt.float32
BF16 = mybir.dt.bfloat16
AF = mybir.ActivationFunctionType
ALU = mybir.AluOpType

D = 1024
DFF = 2816
EPS = 1e-6
P = 128
NEG = -30000.0

ENGS = ("pe", "act", "dve", "pool", "sp")
LIMIT = [10 ** 9]
LAST = {}
UID = [0]
DEBUG = False


class Buf:
    __slots__ = ("name", "last_w", "readers")

    def __init__(self, name):
        self.name = name
        self.last_w = None
        self.readers = {}


class Prog:
    NSLOT = 6

    def __init__(self, nc):
        self.nc = nc
        self.streams = {e: [] for e in ENGS}
        self.sems = {}
        self.cnt = {}
        for e in ("pe", "act", "dve", "pool"):
            self.sems[e] = nc.alloc_semaphore("s_" + e)
            self.cnt[e] = 0
        self.slots = {}
        self.slot_rr = {}
        for e in ("sp", "pool", "act"):
            self.slots[e] = []
            for i in range(self.NSLOT):
                k = "d_%s%d" % (e, i)
                self.sems[k] = nc.alloc_semaphore(k)
                self.cnt[k] = 0
                self.slots[e].append(k)
            self.slot_rr[e] = 0
        self.waited = {}
        self.pending = {e: False for e in ENGS}
        self.nops = 0

    def _wait(self, eng, tok):
        if tok is None:
            return
        teng, key, val = tok
        if self.waited.get((eng, key), 0) >= val:
            return
        self.waited[(eng, key)] = val
        self.streams[eng].append(("wait", key, val))

    def _deps(self, eng, reads, writes):
        for b in reads:
            t = b.last_w
            if t is not None:
                if t[0] == eng and t[1] == eng and eng == "pe":
                    continue
                self._wait(eng, t)
        for b in writes:
            t = b.last_w
            if t is not None and not (t[0] == eng and t[1] == eng):
                self._wait(eng, t)
            for re, rt in b.readers.items():
                if re == eng and rt[1] == eng:
                    continue
                self._wait(eng, rt)

    def _mark(self, eng, tok, reads, writes):
        for b in reads:
            old = b.readers.get(eng)
            if old is None or old[1] != tok[1] or old[2] < tok[2]:
                if old is not None and old[1] != tok[1]:
                    pass
                b.readers[eng] = tok
        for b in writes:
            b.last_w = tok
            b.readers = {}

    def op(self, eng, fn, reads=(), writes=(), signal=True):
        if self.nops >= LIMIT[0]:
            if not (eng == "pe" and self.pending["pe"]):
                return None
        self._deps(eng, reads, writes)
        if signal:
            self.cnt[eng] += 1
            tok = (eng, eng, self.cnt[eng])
            self.pending[eng] = False
        else:
            tok = (eng, eng, self.cnt[eng] + 1)
            self.pending[eng] = True
        self.streams[eng].append(("op", fn, eng if signal else None, 1))
        if DEBUG:
            import sys as _s
            LAST.setdefault("log", []).append((self.nops, eng, _s._getframe(1).f_lineno))
        self._mark(eng, tok, reads, writes)
        self.nops += 1
        return tok

    def dma(self, eng, fn, reads=(), writes=()):
        if self.nops >= LIMIT[0]:
            return None
        self._deps(eng, reads, writes)
        for b in reads:
            old = b.readers.get(eng)
            if old is not None and old[1] != eng:
                self._wait(eng, old)
        slot = self.slots[eng][self.slot_rr[eng] % self.NSLOT]
        self.slot_rr[eng] += 1
        if self.cnt[slot] > 0:
            self._wait(eng, (eng, slot, self.cnt[slot]))
        self.cnt[slot] += 16
        tok = (eng, slot, self.cnt[slot])
        self.streams[eng].append(("op", fn, slot, 16))
        if DEBUG:
            import sys as _s
            LAST.setdefault("log", []).append((self.nops, eng + "-dma", _s._getframe(1).f_lineno))
        self._mark(eng, tok, reads, writes)
        self.nops += 1
        return tok

    def wait_tok(self, eng, tok):
        self._wait(eng, tok)

    def barrier(self, bufs=()):
        toks = []
        for e in ("pe", "act", "dve", "pool"):
            if self.cnt[e] > 0:
                toks.append((e, e, self.cnt[e]))
        for e, sl in self.slots.items():
            for k in sl:
                if self.cnt[k] > 0:
                    toks.append((e, k, self.cnt[k]))
        for e in ENGS:
            for t in toks:
                self._wait(e, t)

    def emit(self):
        nc = self.nc
        for e in ENGS:
            assert not self.pending[e], "unsignalled trailing op on " + e
        streams = self.streams
        sems = self.sems

        def run(engobj, lst):
            for it in lst:
                if it[0] == "wait":
                    engobj.wait_ge(sems[it[1]], it[2])
                else:
                    ins = it[1](engobj)
                    if it[2] is not None:
                        ins.then_inc(sems[it[2]], it[3])

        with nc.Block() as block:
            @block.tensor
            def _(e):
                run(e, streams["pe"])

            @block.scalar
            def _(e):
                run(e, streams["act"])

            @block.vector
            def _(e):
                run(e, streams["dve"])

            @block.gpsimd
            def _(e):
                run(e, streams["pool"])

            @block.sync
            def _(e):
                run(e, streams["sp"])


class Ctx:
    def __init__(self, nc, pg, stack):
        self.nc, self.pg, self.stack = nc, pg, stack
        self.n = 0

    def sb(self, shape, dt, name=None):
        UID[0] += 1
        t = self.stack.enter_context(self.nc.sbuf_tensor("%s_%d" % (name or "t", UID[0]), list(shape), dt))
        return t

    def ps(self, shape, dt, name=None):
        UID[0] += 1
        t = self.stack.enter_context(self.nc.psum_tensor("%s_%d" % (name or "p", UID[0]), list(shape), dt))
        return t


def load_weight(cx, dst, dst_buf, src, KC, N, gain=None, stage=None, rr=[0]):
    pg = cx.pg
    CB = 1024
    srcv = src.rearrange("(k p) n -> p k n", p=P)
    for k in range(KC):
        for c0 in range(0, N, CB):
            cw = min(CB, N - c0)
            st, stb = stage[rr[0] % len(stage)]
            pg.dma("sp", lambda e, st=st, k=k, c0=c0, cw=cw: e.dma_start(out=st[:, 0:cw], in_=srcv[:, k, c0:c0 + cw]),
                   writes=[stb])
            which = rr[0] % 2
            rr[0] += 1
            o = dst[:, k, c0:c0 + cw]
            i = st[:, 0:cw]
            if gain is None:
                if which == 0:
                    pg.op("dve", lambda e, o=o, i=i: e.tensor_copy(out=o, in_=i), reads=[stb], writes=[dst_buf])
                elif which == 1:
                    pg.op("act", lambda e, o=o, i=i: e.copy(out=o, in_=i), reads=[stb], writes=[dst_buf])
                else:
                    pg.op("pool", lambda e, o=o, i=i: e.tensor_copy(out=o, in_=i), reads=[stb], writes=[dst_buf])
            else:
                g = gain[:, k:k + 1]
                if which == 0:
                    pg.op("dve", lambda e, o=o, i=i, g=g: e.tensor_scalar(out=o, in0=i, scalar1=g, scalar2=None, op0=ALU.mult),
                          reads=[stb], writes=[dst_buf])
                elif which == 1:
                    pg.op("act", lambda e, o=o, i=i, g=g: e.activation(out=o, in_=i, func=AF.Copy, scale=g),
                          reads=[stb], writes=[dst_buf])
                else:
                    pg.op("pool", lambda e, o=o, i=i, g=g: e.tensor_scalar(out=o, in0=i, scalar1=g, scalar2=None, op0=ALU.mult),
                          reads=[stb], writes=[dst_buf])


def rms_rstd(cx, x_ap, xbuf, junk, junkb, ss, ssb, rstd, rstdb, n):
    pg = cx.pg
    pg.op("act", lambda e: e.activation(out=junk, in_=x_ap, func=AF.Square, accum_out=ss),
          reads=[xbuf], writes=[junkb, ssb])
    pg.op("act", lambda e: e.activation(out=ss, in_=ss, func=AF.Sqrt, scale=1.0 / n, bias=cx.eps_ap),
          reads=[ssb], writes=[ssb])
    pg.op("dve", lambda e: e.reciprocal(out=rstd, in_=ss), reads=[ssb], writes=[rstdb])


def phase_ffn(nc, pg, NTT, h_in, h_out, w_in, w_out, gainT, gcol, final=None, tiles=None):
    G = 4
    KC = D // P
    FC = DFF // P
    with contextlib.ExitStack() as stack:
        cx = Ctx(nc, pg, stack)
        Win = cx.sb([P, KC, 2 * DFF], BF16, "Win"); Win_b = Buf("Win")
        Wout = cx.sb([P, FC, D], BF16, "Wout"); Wout_b = Buf("Wout")
        aT = cx.sb([P, FC, G * P], BF16, "aT"); aT_b = Buf("aT")
        stg_v = aT[:, :, :].rearrange("p f n -> p (f n)").bitcast(F32)
        stage = [(stg_v[:, i * 1024:(i + 1) * 1024], Buf("stg%d" % i)) for i in range(5)]
        gT = cx.sb([P, gainT.shape[1]], F32, "gT"); gT_b = Buf("gT")
        ident = cx.sb([P, P], BF16, "ident"); ident_b = Buf("ident")
        identf = cx.sb([P, P], F32, "identf"); identf_b = Buf("identf")
        epsT = cx.sb([P, 1], F32, "eps"); eps_b = Buf("eps")
        cx.eps_ap = epsT[:, 0:1]
        pg.op("pool", lambda e: e.memset(epsT[:], EPS), writes=[eps_b])
        pg.dma("sp", lambda e: e.dma_start(out=gT[:], in_=gainT[:, :]), writes=[gT_b])
        pg.dma("sp", lambda e: e.dma_start(out=identf[:], in_=cx_ident(nc)[:, :]), writes=[identf_b])
        pg.op("dve", lambda e: e.tensor_copy(out=ident[:], in_=identf[:]), reads=[identf_b], writes=[ident_b])
        pg.wait_tok("dve", gT_b.last_w); pg.wait_tok("act", gT_b.last_w); pg.wait_tok("pool", gT_b.last_w)
        pg.wait_tok("act", eps_b.last_w)
        load_weight(cx, Win, Win_b, w_in, KC, 2 * DFF, gain=gT[:, gcol:gcol + KC], stage=stage)
        load_weight(cx, Wout, Wout_b, w_out, FC, D, gain=None, stage=stage)
        if final is not None:
            gfin = cx.sb([P, D], F32, "gfin"); gfin_b = Buf("gfin")
            pg.dma("sp", lambda e: e.dma_start(out=gfin[:], in_=final["gB"][:, :]), writes=[gfin_b])

        NB = 2
        hin = [(cx.sb([P, G, D], F32, "hin"), Buf("hin")) for _ in range(NB)]
        hn = [(cx.sb([P, D], BF16, "hn"), Buf("hn")) for _ in range(2)]
        ss = [(cx.sb([P, 1], F32, "ss"), Buf("ss")) for _ in range(2)]
        rstd = [(cx.sb([P, 1], F32, "rstd"), Buf("rstd")) for _ in range(2)]
        hnT = cx.sb([P, KC, G * P], BF16, "hnT"); hnT_b = Buf("hnT")
        sg = [(cx.sb([P, G * P], F32, "sg"), Buf("sg")) for _ in range(2)]
        junk_t = sg[0][0][:, :].bitcast(BF16); junk_b = sg[0][1]
        pT = [(cx.ps([P, KC, P], BF16, "pT"), Buf("pT")) for _ in range(2)]
        pg_ = [(cx.ps([P, 512], F32, "pg"), Buf("pg")) for _ in range(2)]
        pu_ = [(cx.ps([P, 512], F32, "pu"), Buf("pu")) for _ in range(2)]
        po_ = [(cx.ps([P, 512], F32, "po"), Buf("po")) for _ in range(2)]

        groups = []
        t = 0 if final is None else 1
        while t < NTT:
            g = min(G, NTT - t)
            groups.append((t, g))
            t += g
        cnt = {"t": 0, "c": 0, "o": 0}

        def front(gi):
            t0, g = groups[gi]
            hi, hi_b = hin[gi % NB]
            pg.dma("sp", lambda e: e.dma_start(
                out=hi[:, 0:g, :], in_=h_in[t0 * P:(t0 + g) * P, :].rearrange("(g p) d -> p g d", p=P)),
                writes=[hi_b])
            for j in range(g):
                x_ap = hi[:, j, :]
                s_, s_b = ss[cnt["t"] % 2]; r_, r_b = rstd[cnt["t"] % 2]
                h_, h_b = hn[cnt["t"] % 2]; p_, p_b = pT[cnt["t"] % 2]
                cnt["t"] += 1
                rms_rstd(cx, x_ap, hi_b, junk_t, junk_b, s_[:, 0:1], s_b, r_[:, 0:1], r_b, D)
                pg.op("act", lambda e, h_=h_, x_ap=x_ap, r_=r_: e.activation(out=h_[:], in_=x_ap, func=AF.Copy, scale=r_[:, 0:1]),
                      reads=[hi_b, r_b], writes=[h_b])
                for k in range(KC):
                    pg.op("pe", lambda e, p_=p_, h_=h_, k=k: e.transpose(out=p_[:, k, :], in_=h_[:, k * P:(k + 1) * P], identity=ident[:]),
                          reads=[h_b, ident_b], writes=[p_b], signal=(k == KC - 1))
                pg.op("dve", lambda e, p_=p_, j=j: e.tensor_copy(out=hnT[:, :, j * P:(j + 1) * P], in_=p_[:]),
                      reads=[p_b], writes=[hnT_b])

        def mm1(gi):
            t0, g = groups[gi]
            N = g * P
            for i in range(FC):
                pgt, pg_b = pg_[cnt["c"] % 2]; put, pu_b = pu_[cnt["c"] % 2]
                sgt, sg_b = sg[cnt["c"] % 2]
                cnt["c"] += 1
                for k in range(KC):
                    pg.op("pe", lambda e, pgt=pgt, k=k, i=i: e.matmul(
                        pgt[:, 0:N], lhsT=Win[:, k, i * P:(i + 1) * P], rhs=hnT[:, k, 0:N], start=(k == 0), stop=(k == KC - 1)),
                        reads=[Win_b, hnT_b], writes=[pg_b], signal=(k == KC - 1))
                for k in range(KC):
                    pg.op("pe", lambda e, put=put, k=k, i=i: e.matmul(
                        put[:, 0:N], lhsT=Win[:, k, DFF + i * P:DFF + (i + 1) * P], rhs=hnT[:, k, 0:N], start=(k == 0), stop=(k == KC - 1)),
                        reads=[Win_b, hnT_b], writes=[pu_b], signal=(k == KC - 1))
                pg.op("act", lambda e, sgt=sgt, pgt=pgt: e.activation(out=sgt[:, 0:N], in_=pgt[:, 0:N], func=AF.Silu),
                      reads=[pg_b], writes=[sg_b])
                pg.op("dve", lambda e, sgt=sgt, put=put, i=i: e.tensor_tensor(out=aT[:, i, 0:N], in0=sgt[:, 0:N], in1=put[:, 0:N], op=ALU.mult),
                      reads=[sg_b, pu_b], writes=[aT_b])

        def mm2(gi):
            t0, g = groups[gi]
            hi, hi_b = hin[gi % NB]
            for j in range(g):
                for n in range(2):
                    pot, po_b = po_[cnt["o"] % 2]
                    cnt["o"] += 1
                    for i in range(FC):
                        pg.op("pe", lambda e, pot=pot, i=i, j=j, n=n: e.matmul(
                            pot[:, :], lhsT=aT[:, i, j * P:(j + 1) * P], rhs=Wout[:, i, n * 512:(n + 1) * 512], start=(i == 0), stop=(i == FC - 1)),
                            reads=[aT_b, Wout_b], writes=[po_b], signal=(i == FC - 1))
                    pg.op("dve", lambda e, j=j, n=n, pot=pot: e.tensor_tensor(
                        out=hi[:, j, n * 512:(n + 1) * 512], in0=hi[:, j, n * 512:(n + 1) * 512], in1=pot[:, :], op=ALU.add),
                        reads=[po_b, hi_b], writes=[hi_b])
            if final is None:
                pg.dma("sp", lambda e: e.dma_start(
                    out=h_out[t0 * P:(t0 + g) * P, :].rearrange("(g p) d -> p g d", p=P), in_=hi[:, 0:g, :]),
                    reads=[hi_b])
            else:
                for j in range(g):
                    tt = t0 + j
                    if tt == 0:
                        continue
                    x_ap = hi[:, j, :]
                    s_, s_b = ss[cnt["t"] % 2]; r_, r_b = rstd[cnt["t"] % 2]
                    cnt["t"] += 1
                    rms_rstd(cx, x_ap, hi_b, junk_t, junk_b, s_[:, 0:1], s_b, r_[:, 0:1], r_b, D)
                    pg.op("dve", lambda e, x_ap=x_ap, r_=r_: e.scalar_tensor_tensor(
                        out=x_ap, in0=x_ap, scalar=r_[:, 0:1], in1=gfin[:], op0=ALU.mult, op1=ALU.mult),
                        reads=[hi_b, r_b, gfin_b], writes=[hi_b])
                    pg.dma("sp", lambda e, x_ap=x_ap, tt=tt: e.dma_start(out=final["out"][(tt - 1) * P:tt * P, :], in_=x_ap),
                           reads=[hi_b])

        front(0)
        for gi in range(len(groups)):
            mm1(gi)
            if gi + 1 < len(groups):
                front(gi + 1)
            mm2(gi)
        pg.barrier()


_IDENT = {}


def cx_ident(nc):
    return _IDENT[id(nc)]


GAIN_COLS = {"mix0": 0, "ffn0": 8, "mix1": 16, "ffn1": 24, "gout": 32, "gkv": 40}


def build(SEQ, phases, ext_in=(), ext_out=()):
    nc = bass.Bass("TRN2", target_bir_lowering=False)
    NTT = 1 + SEQ // P
    R = NTT * P

    def dram(name, shape, dt, kind="Internal"):
        if name in ext_in:
            kind = "ExternalInput"
        elif name in ext_out:
            kind = "ExternalOutput"
        return nc.dram_tensor(name, list(shape), dt, kind=kind).ap()

    W = {}
    W["gainT"] = dram("gainT", [P, 48], F32, "ExternalInput")
    W["identf"] = dram("identf", [P, P], F32, "ExternalInput")
    _IDENT[id(nc)] = W["identf"]
    W["ffn_w_in0"] = dram("ffn_w_in0", [D, 2 * DFF], F32, "ExternalInput")
    W["ffn_w_out0"] = dram("ffn_w_out0", [DFF, D], F32, "ExternalInput")
    W["ffn_w_in1"] = dram("ffn_w_in1", [D, 2 * DFF], F32, "ExternalInput")
    W["ffn_w_out1"] = dram("ffn_w_out1", [DFF, D], F32, "ExternalInput")
    W["gfinB"] = dram("gfinB", [P, D], F32, "ExternalInput")
    W["gla_w_in"] = dram("gla_w_in", [D, 3072], F32, "ExternalInput")
    W["gla_w_a1"] = dram("gla_w_a1", [D, 16], F32, "ExternalInput")
    W["gla_w_a2aug"] = dram("gla_w_a2aug", [17, 512], F32, "ExternalInput")
    W["gla_w_out"] = dram("gla_w_out", [D, D], F32, "ExternalInput")
    consts = {}
    consts["Dm"] = dram("c_Dm", [P, 2, P], F32, "ExternalInput")
    consts["Ind"] = dram("c_Ind", [P, 2, 2], F32, "ExternalInput")
    x = dram("x", [SEQ, D], F32, "ExternalInput")
    meta = dram("meta", [16, D], F32, "ExternalInput")
    W["dsa_w_in"] = dram("dsa_w_in", [D, DSA_IN], F32, "ExternalInput")
    W["dsa_w_ukT"] = dram("dsa_w_ukT", [P, 2048], F32, "ExternalInput")
    W["dsa_w_uv"] = dram("dsa_w_uv", [8, 256, P], F32, "ExternalInput")
    W["dsa_w_out"] = dram("dsa_w_out", [D, D], F32, "ExternalInput")
    W["gkvB"] = dram("gkvB", [P, 256], F32, "ExternalInput")
    consts["m0"] = dram("c_m0", [P, P], F32, "ExternalInput")
    consts["mdiag"] = dram("c_mdiag", [P, P], F32, "ExternalInput")
    consts["b15"] = dram("c_b15", [P, 1024], F32, "ExternalInput")
    consts["pow2"] = dram("c_pow2", [P, 32], F32, "ExternalInput")
    consts["biasT"] = dram("c_biasT", [3, P, 1024], F32, "ExternalInput")
    scr = {}
    scr["qlat"] = dram("s_qlat", [NTT, P, 2048], BF16)
    scr["qi"] = dram("s_qi", [NTT, P, 1024], BF16)
    scr["oT"] = dram("s_oT", [NTT, P, 1024], BF16)
    h1 = dram("h1", [R, D], F32)
    h2 = dram("h2", [R, D], F32)
    h3 = dram("h3", [R, D], F32)
    out = dram("out", [SEQ, D], F32, "ExternalOutput" if "E" in phases else "Internal")

    pg = Prog(nc)
    if "A" in phases:
        phase_gla(nc, pg, NTT, x, meta, h1, W, consts)
    if "B" in phases:
        phase_ffn(nc, pg, NTT, h1, h2, W["ffn_w_in0"], W["ffn_w_out0"], W["gainT"], GAIN_COLS["ffn0"])
    if "C" in phases or "D" in phases:
        with contextlib.ExitStack() as cstack:
            ccx = Ctx(nc, pg, cstack)
            caches = {
                "kidx2": (ccx.sb([P, R], BF16, "kidx2"), Buf("kidx2")),
                "cT": (ccx.sb([P, 2, R], BF16, "cT"), Buf("cT")),
                "ctok": (ccx.sb([P, NTT, 258], BF16, "ctok"), Buf("ctok")),
                "wabs": (ccx.sb([P, NTT, 8], F32, "wabs"), Buf("wabs")),
                "sgn": (ccx.sb([P, NTT, 8], F32, "sgn"), Buf("sgn")),
            }
            if "C" in phases:
                phase_dsa_proj(nc, pg, NTT, h2, W, consts, caches, scr)
            if "D" in phases:
                phase_dsa_attn(nc, pg, NTT, W, consts, caches, scr)
    if "F" in phases:
        phase_dsa_out(nc, pg, NTT, h2, h3, W, scr)
    if "E" in phases:
        phase_ffn(nc, pg, NTT, h3, None, W["ffn_w_in1"], W["ffn_w_out1"], W["gainT"], GAIN_COLS["ffn1"],
                  final=dict(out=out, gB=W["gfinB"]))
    pg.emit()
    LAST['nops'] = pg.nops
    return nc


def phase_gla(nc, pg, NTT, x, meta, h_out, W, consts):
    KC = D // P
    H = 4
    with contextlib.ExitStack() as stack:
        cx = Ctx(nc, pg, stack)
        Win = cx.sb([P, KC, 3072], BF16, "Win"); Win_b = Buf("Win")
        Wa1 = cx.sb([P, KC, 16], BF16, "Wa1"); Wa1_b = Buf("Wa1")
        Wa2 = cx.sb([17, 512], F32, "Wa2"); Wa2_b = Buf("Wa2")
        Wout = cx.sb([P, KC, D], BF16, "Wout"); Wout_b = Buf("Wout")
        stage = [(cx.sb([P, 1024], F32, "stg"), Buf("stg")) for _ in range(5)]
        gT = cx.sb([P, 48], F32, "gT"); gT_b = Buf("gT")
        ident = cx.sb([P, P], BF16, "ident"); ident_b = Buf("ident")
        identf = cx.sb([P, P], F32, "identf"); identf_b = Buf("identf")
        Dm = cx.sb([P, 2, P], F32, "Dm"); Dm_b = Buf("Dm")
        Ind = cx.sb([P, 2, 2], F32, "Ind"); Ind_b = Buf("Ind")
        epsT = cx.sb([P, 1], F32, "eps"); eps_b = Buf("eps")
        cx.eps_ap = epsT[:, 0:1]
        pg.op("pool", lambda e: e.memset(epsT[:], EPS), writes=[eps_b])
        pg.dma("sp", lambda e: e.dma_start(out=gT[:], in_=W["gainT"][:, :]), writes=[gT_b])
        pg.dma("sp", lambda e: e.dma_start(out=identf[:], in_=W["identf"][:, :]), writes=[identf_b])
        pg.dma("sp", lambda e: e.dma_start(out=Dm[:], in_=consts["Dm"][:, :, :]), writes=[Dm_b])
        pg.dma("sp", lambda e: e.dma_start(out=Ind[:], in_=consts["Ind"][:, :, :]), writes=[Ind_b])
        pg.dma("sp", lambda e: e.dma_start(out=Wa2[:], in_=W["gla_w_a2aug"][:, :]), writes=[Wa2_b])
        pg.op("dve", lambda e: e.tensor_copy(out=ident[:], in_=identf[:]), reads=[identf_b], writes=[ident_b])
        for en in ("dve", "act", "pool"):
            pg.wait_tok(en, gT_b.last_w)
        pg.wait_tok("act", eps_b.last_w)
        g0 = GAIN_COLS["mix0"]
        load_weight(cx, Win, Win_b, W["gla_w_in"], KC, 3072, gain=gT[:, g0:g0 + KC], stage=stage)
        load_weight(cx, Wa1, Wa1_b, W["gla_w_a1"], KC, 16, gain=gT[:, g0:g0 + KC], stage=stage)
        go = GAIN_COLS["gout"]
        load_weight(cx, Wout, Wout_b, W["gla_w_out"], KC, D, gain=gT[:, go:go + KC], stage=stage)

        def dbl(shape, dt, nm_, n=2):
            return [(cx.sb(shape, dt, nm_), Buf(nm_)) for _ in range(n)]
        xin = dbl([P, D], F32, "xin", 3)
        hn = dbl([P, D], BF16, "hn")
        hnT = dbl([P, KC, P], BF16, "hnT")
        qT = dbl([P, H, P], F32, "qT")
        kt = dbl([P, 2, 512], F32, "kt")
        vsb = dbl([P, D], F32, "vsb")
        sr = dbl([P, D], F32, "sr")
        etot = dbl([P, H, 2], F32, "etot")
        junk = cx.sb([P, D], BF16, "junk"); junk_b = Buf("junk")
        ss = cx.sb([P, 1], F32, "ss"); ss_b = Buf("ss")
        rstd = cx.sb([P, 1], F32, "rstd"); rstd_b = Buf("rstd")
        ksb = cx.sb([P, 512], F32, "ksb"); ksb_b = Buf("ksb")
        mb = cx.sb([P, 2], F32, "mb"); mb_b = Buf("mb")
        pg.op("pool", lambda e: e.memset(mb[:], NEG), writes=[mb_b])
        pg.op("pool", lambda e: e.memset(mb[0:64, 0:1], 0.0), writes=[mb_b])
        pg.op("pool", lambda e: e.memset(mb[64:128, 1:2], 0.0), writes=[mb_b])
        pg.wait_tok("act", mb_b.last_w)
        a1T = cx.sb([17, P], F32, "a1T"); a1T_b = Buf("a1T")
        e1 = cx.sb([P, 512], F32, "e1"); e1_b = Buf("e1")
        sp_ = cx.sb([P, 512], F32, "sp"); sp_b = Buf("sp")
        ed = cx.sb([P, 2, 512], F32, "ed"); ed_b = Buf("ed")
        S = cx.sb([P, H, 256], F32, "S"); S_b = [Buf("S%d" % h) for h in range(H)]
        osb = cx.sb([P, H, 256], F32, "osb"); osb_b = Buf("osb")
        ss4 = cx.sb([P, H], F32, "ss4"); ss4_b = Buf("ss4")
        rs4 = cx.sb([P, H], F32, "rs4"); rs4_b = Buf("rs4")
        y = cx.sb([P, D], BF16, "y"); y_b = Buf("y")
        yT = cx.sb([P, KC, P], BF16, "yT"); yT_b = Buf("yT")

        def bank(nm_):
            t_ = cx.ps([P, 512], F32, nm_)
            return (t_, Buf(nm_))
        pT32, pT_b = bank("pT")
        pTv = pT32[:, :].bitcast(BF16).rearrange("p (k t) -> p k t", k=KC)
        RB = [bank("pR%d" % i) for i in range(3)]
        KVB = [bank("pK%d" % i) for i in range(2)]
        OB = [bank("pO%d" % i) for i in range(2)]
        rcnt = [0]

        def nextR():
            r = RB[rcnt[0] % 3]
            rcnt[0] += 1
            return r

        pg.op("pool", lambda e: e.memset(S[:], 0.0), writes=S_b)
        pg.op("pool", lambda e: e.memset(a1T[:], 1.0), writes=[a1T_b])

        def front(t):
            xi, xi_b = xin[t % 3]
            hn_, hn_b = hn[t % 2]
            hT, hT_b = hnT[t % 2]
            if t == 0:
                pg.op("pool", lambda e: e.memset(xi[:], 0.0), writes=[xi_b])
                pg.dma("sp", lambda e: e.dma_start(out=xi[48:64, :], in_=meta[:, :]), writes=[xi_b])
            else:
                pg.dma("sp", lambda e: e.dma_start(out=xi[:], in_=x[(t - 1) * P:t * P, :]), writes=[xi_b])
            rms_rstd(cx, xi[:], xi_b, junk[:], junk_b, ss[:, 0:1], ss_b, rstd[:, 0:1], rstd_b, D)
            pg.op("act", lambda e: e.activation(out=hn_[:], in_=xi[:], func=AF.Copy, scale=rstd[:, 0:1]),
                  reads=[xi_b, rstd_b], writes=[hn_b])
            for k in range(KC):
                pg.op("pe", lambda e, k=k: e.transpose(out=pTv[:, k, :], in_=hn_[:, k * P:(k + 1) * P], identity=ident[:]),
                      reads=[hn_b, ident_b], writes=[pT_b], signal=(k == KC - 1))
            pg.op("dve", lambda e: e.tensor_copy(out=hT[:], in_=pTv), reads=[pT_b], writes=[hT_b])

        def proj_groups(t):
            hT, hT_b = hnT[t % 2]
            q_, q_b = qT[t % 2]
            kt_, kt_b = kt[t % 2]
            v_, v_b = vsb[t % 2]
            sr_, sr_b = sr[t % 2]
            et_, et_b = etot[t % 2]
            ci = 1 if t == 0 else 0
            gs = []

            def g_q():
                pb, pb_b = nextR()
                for h in range(H):
                    for k in range(KC):
                        pg.op("pe", lambda e, h=h, k=k: e.matmul(pb[:, h * P:(h + 1) * P], lhsT=Win[:, k, h * P:(h + 1) * P], rhs=hT[:, k, :],
                                                                  start=(k == 0), stop=(k == KC - 1)),
                              reads=[Win_b, hT_b], writes=[pb_b], signal=(h == H - 1 and k == KC - 1))
                pg.op("act", lambda e: e.activation(out=q_[:].rearrange("p h t -> p (h t)"), in_=pb[:, :], func=AF.Copy, scale=float(128 ** -0.5)),
                      reads=[pb_b], writes=[q_b])
            gs.append(g_q)

            def mm512(col0, evac):
                def g():
                    pb, pb_b = nextR()
                    for k in range(KC):
                        pg.op("pe", lambda e, k=k: e.matmul(pb[:, :], lhsT=hT[:, k, :], rhs=Win[:, k, col0:col0 + 512], start=(k == 0), stop=(k == KC - 1)),
                              reads=[Win_b, hT_b], writes=[pb_b], signal=(k == KC - 1))
                    evac(pb, pb_b)
                return g
            gs.append(mm512(512, lambda pb, pb_b: pg.op("dve", lambda e: e.tensor_copy(out=ksb[:], in_=pb[:, :]), reads=[pb_b], writes=[ksb_b])))
            gs.append(mm512(1024, lambda pb, pb_b: pg.op("act", lambda e: e.copy(out=v_[:, 0:512], in_=pb[:, :]), reads=[pb_b], writes=[v_b])))
            gs.append(mm512(1536, lambda pb, pb_b: pg.op("dve", lambda e: e.tensor_copy(out=v_[:, 512:1024], in_=pb[:, :]), reads=[pb_b], writes=[v_b])))
            gs.append(mm512(2048, lambda pb, pb_b: pg.op("act", lambda e: e.activation(out=sr_[:, 0:512], in_=pb[:, :], func=AF.Silu), reads=[pb_b], writes=[sr_b])))
            gs.append(mm512(2560, lambda pb, pb_b: pg.op("act", lambda e: e.activation(out=sr_[:, 512:1024], in_=pb[:, :], func=AF.Silu), reads=[pb_b], writes=[sr_b])))

            def g_a1():
                pb, pb_b = nextR()
                for k in range(KC):
                    pg.op("pe", lambda e, k=k: e.matmul(pb[0:16, 0:P], lhsT=Wa1[:, k, :], rhs=hT[:, k, :], start=(k == 0), stop=(k == KC - 1)),
                          reads=[Wa1_b, hT_b], writes=[pb_b], signal=(k == KC - 1))
                pg.op("dve", lambda e: e.tensor_copy(out=a1T[0:16, :], in_=pb[0:16, 0:P]), reads=[pb_b], writes=[a1T_b])
            gs.append(g_a1)

            def g_z():
                pb, pb_b = nextR()
                pg.op("pe", lambda e: e.matmul(pb[:, :], lhsT=a1T[:, :], rhs=Wa2[:, :], start=True, stop=True), reads=[a1T_b, Wa2_b], writes=[pb_b])
                pg.op("act", lambda e: e.activation(out=e1[:], in_=pb[:, :], func=AF.Exp, scale=-1.0), reads=[pb_b], writes=[e1_b])
                pg.op("act", lambda e: e.activation(out=sp_[:], in_=e1[:], func=AF.Ln, bias=1.0), reads=[e1_b], writes=[sp_b])
            gs.append(g_z)

            def g_dec():
                pb, pb_b = nextR()
                pg.op("pe", lambda e: e.matmul(pb[:, :], lhsT=Dm[:, ci, :], rhs=sp_[:], start=True, stop=True), reads=[Dm_b, sp_b], writes=[pb_b])
                for ch in range(2):
                    pg.op("act", lambda e, ch=ch: e.activation(out=ed[:, ch, :], in_=pb[:, :], func=AF.Exp, bias=mb[:, ch:ch + 1]), reads=[pb_b], writes=[ed_b])
                for ch in range(2):
                    pg.op("dve", lambda e, ch=ch: e.tensor_tensor(out=kt_[:, ch, :], in0=ksb[:], in1=ed[:, ch, :], op=ALU.mult), reads=[ksb_b, ed_b], writes=[kt_b])
            gs.append(g_dec)

            def g_tot(h0):
                def g():
                    for h in (h0, h0 + 1):
                        pb, pb_b = nextR()
                        pg.op("pe", lambda e, h=h, pb=pb: e.matmul(pb[:, 0:2], lhsT=sp_[:, h * P:(h + 1) * P], rhs=Ind[:, ci, :], start=True, stop=True),
                              reads=[sp_b, Ind_b], writes=[pb_b])
                        pg.op("act", lambda e, h=h, pb=pb: e.activation(out=et_[:, h, :], in_=pb[:, 0:2], func=AF.Exp), reads=[pb_b], writes=[et_b])
                return g
            gs.append(g_tot(0))
            gs.append(g_tot(2))
            gs = gs[0:4] + gs[6:11] + gs[4:6]
            return gs

        def scan_steps(t):
            q_, q_b = qT[t % 2]
            kt_, kt_b = kt[t % 2]
            v_, v_b = vsb[t % 2]
            et_, et_b = etot[t % 2]
            steps = []
            for ch in range(1 if t == 0 else 2):
                r0, r1 = ch * 64, (ch + 1) * 64
                for h in range(H):
                    def sa(h=h, ch=ch):
                        kvp, kvb = KVB[h % 2]
                        pg.op("pe", lambda e: e.matmul(kvp[:, 0:256], lhsT=kt_[:, ch, h * P:(h + 1) * P], rhs=v_[:, h * 256:(h + 1) * 256], start=True, stop=True),
                              reads=[kt_b, v_b], writes=[kvb])
                        pg.op("dve", lambda e: e.scalar_tensor_tensor(out=S[:, h, :], in0=S[:, h, :], scalar=et_[:, h, ch:ch + 1], in1=kvp[:, 0:256],
                                                                       op0=ALU.mult, op1=ALU.add), reads=[kvb, et_b, S_b[h]], writes=[S_b[h]])

                    def sb_(h=h, r0=r0, r1=r1):
                        op_, op_b = OB[h % 2]
                        pg.op("pe", lambda e: e.matmul(op_[:, 0:256], lhsT=q_[:, h, :], rhs=S[:, h, :], start=True, stop=True),
                              reads=[q_b, S_b[h]], writes=[op_b])
                        if h % 2 == 0:
                            pg.op("act", lambda e: e.copy(out=osb[r0:r1, h, :], in_=op_[r0:r1, 0:256]), reads=[op_b], writes=[osb_b])
                        else:
                            pg.op("dve", lambda e: e.tensor_copy(out=osb[r0:r1, h, :], in_=op_[r0:r1, 0:256]), reads=[op_b], writes=[osb_b])
                    steps.append(sa)
                    steps.append(sb_)
            return steps

        def tail_a(t):
            xi, xi_b = xin[t % 3]
            sr_, sr_b = sr[t % 2]
            for h in range(H):
                pg.op("act", lambda e, h=h: e.activation(out=junk[:, 0:256], in_=osb[:, h, :], func=AF.Square, accum_out=ss4[:, h:h + 1]),
                      reads=[osb_b], writes=[junk_b, ss4_b])
            pg.op("act", lambda e: e.activation(out=ss4[:], in_=ss4[:], func=AF.Sqrt, scale=1.0 / 256, bias=cx.eps_ap), reads=[ss4_b], writes=[ss4_b])
            pg.op("dve", lambda e: e.reciprocal(out=rs4[:], in_=ss4[:]), reads=[ss4_b], writes=[rs4_b])
            for h in range(H):
                pg.op("dve", lambda e, h=h: e.scalar_tensor_tensor(
                    out=y[:, h * 256:(h + 1) * 256], in0=osb[:, h, :], scalar=rs4[:, h:h + 1], in1=sr_[:, h * 256:(h + 1) * 256],
                    op0=ALU.mult, op1=ALU.mult), reads=[osb_b, rs4_b, sr_b], writes=[y_b])

        def tail_b(t):
            xi, xi_b = xin[t % 3]
            for k in range(KC):
                pg.op("pe", lambda e, k=k: e.transpose(out=pTv[:, k, :], in_=y[:, k * P:(k + 1) * P], identity=ident[:]),
                      reads=[y_b, ident_b], writes=[pT_b], signal=(k == KC - 1))
            pg.op("act", lambda e: e.copy(out=yT[:], in_=pTv), reads=[pT_b], writes=[yT_b])
            for n in range(2):
                pp, pp_b = OB[n]
                for k in range(KC):
                    pg.op("pe", lambda e, k=k, n=n, pp=pp: e.matmul(pp[:, :], lhsT=yT[:, k, :], rhs=Wout[:, k, n * 512:(n + 1) * 512],
                                                                    start=(k == 0), stop=(k == KC - 1)),
                          reads=[Wout_b, yT_b], writes=[pp_b], signal=(k == KC - 1))
                pg.op("dve", lambda e, n=n, pp=pp: e.tensor_tensor(out=xi[:, n * 512:(n + 1) * 512], in0=xi[:, n * 512:(n + 1) * 512], in1=pp[:, :], op=ALU.add),
                      reads=[pp_b, xi_b], writes=[xi_b])
            pg.dma("sp", lambda e: e.dma_start(out=h_out[t * P:(t + 1) * P, :], in_=xi[:]), reads=[xi_b])

        front(0)
        for g in proj_groups(0):
            g()
        for t in range(NTT):
            nxt = t + 1 < NTT
            if nxt:
                front(t + 1)
            A_ = scan_steps(t)
            B_all = proj_groups(t + 1) if nxt else []
            B_, held = B_all[:-2], B_all[-2:]
            ia = ib = 0
            while ia < len(A_) or ib < len(B_):
                if ia < len(A_):
                    A_[ia](); ia += 1
                if ib < len(B_) and (ia % 2 == 1 or ia >= len(A_)):
                    B_[ib](); ib += 1
            tail_a(t)
            for g in held:
                g()
            tail_b(t)
        pg.barrier()


DSA_IN = 1864
IDX_C0 = float((64 ** -0.5) * (8 ** -0.5))


def phase_dsa_proj(nc, pg, NTT, h_in, W, consts, caches, scr):
    KC = D // P
    kidx2, kidx2_b = caches["kidx2"]
    cT, cT_b = caches["cT"]
    ctok, ctok_b = caches["ctok"]
    wabs, wabs_b = caches["wabs"]
    sgn, sgn_b = caches["sgn"]
    with contextlib.ExitStack() as stack:
        cx = Ctx(nc, pg, stack)
        NW = DSA_IN + 64
        Win = cx.sb([P, KC, NW], BF16, "Win"); Win_b = Buf("Win")
        Wuk = cx.sb([P, 1, 2048], BF16, "Wuk"); Wuk_b = Buf("Wuk")
        stage = [(cx.sb([P, 1024], F32, "stg"), Buf("stg")) for _ in range(5)]
        gT = cx.sb([P, 48], F32, "gT"); gT_b = Buf("gT")
        gkvB = cx.sb([P, 256], F32, "gkvB"); gkvB_b = Buf("gkvB")
        ident = cx.sb([P, P], BF16, "ident"); ident_b = Buf("ident")
        identf = cx.sb([P, P], F32, "identf"); identf_b = Buf("identf")
        epsT = cx.sb([P, 1], F32, "eps"); eps_b = Buf("eps")
        cx.eps_ap = epsT[:, 0:1]
        pg.op("pool", lambda e: e.memset(epsT[:], EPS), writes=[eps_b])
        pg.dma("sp", lambda e: e.dma_start(out=gT[:], in_=W["gainT"][:, :]), writes=[gT_b])
        pg.dma("sp", lambda e: e.dma_start(out=gkvB[:], in_=W["gkvB"][:, :]), writes=[gkvB_b])
        pg.dma("sp", lambda e: e.dma_start(out=identf[:], in_=W["identf"][:, :]), writes=[identf_b])
        pg.op("dve", lambda e: e.tensor_copy(out=ident[:], in_=identf[:]), reads=[identf_b], writes=[ident_b])
        for en in ("dve", "act", "pool"):
            pg.wait_tok(en, gT_b.last_w)
        pg.wait_tok("act", eps_b.last_w)
        g0 = GAIN_COLS["mix1"]
        load_weight(cx, Win, Win_b, W["dsa_w_in"], KC, DSA_IN, gain=gT[:, g0:g0 + KC], stage=stage)
        for k in range(KC):
            pg.op("pool", lambda e, k=k: e.tensor_copy(out=Win[:, k, DSA_IN:DSA_IN + 64], in_=Win[:, k, 1792:1856]),
                  reads=[Win_b], writes=[Win_b])
        Wk2 = cx.sb([P, KC, P], BF16, "Wk2"); Wk2_b = Buf("Wk2")
        for k in range(KC):
            pg.op("pool", lambda e, k=k: e.tensor_copy(out=Wk2[:, k, 0:64], in_=Win[:, k, 1792:1856]), reads=[Win_b], writes=[Wk2_b])
            pg.op("pool", lambda e, k=k: e.tensor_copy(out=Wk2[:, k, 64:128], in_=Win[:, k, 1792:1856]), reads=[Win_b], writes=[Wk2_b])
        load_weight(cx, Wuk, Wuk_b, W["dsa_w_ukT"], 1, 2048, gain=None, stage=stage)
        pg.op("pool", lambda e: e.memset(ctok[:, :, 256:258], 1.0), writes=[ctok_b])

        xin = [(cx.sb([P, D], F32, "xin"), Buf("xin")) for _ in range(2)]
        hn2 = [(cx.sb([P, D], BF16, "hn"), Buf("hn")) for _ in range(2)]
        junk = cx.sb([P, D], BF16, "junk"); junk_b = Buf("junk")
        ss = cx.sb([P, 1], F32, "ss"); ss_b = Buf("ss")
        rstd = cx.sb([P, 1], F32, "rstd"); rstd_b = Buf("rstd")
        hnT2 = [(cx.sb([P, KC, P], BF16, "hnT"), Buf("hnT")) for _ in range(2)]
        qT = cx.sb([P, 8, P], BF16, "qT"); qT_b = Buf("qT")
        qlat = [(cx.sb([P, 2, 8, P], BF16, "qlat"), Buf("qlat")) for _ in range(2)]
        qi = [(cx.sb([P, 8, P], BF16, "qi"), Buf("qi")) for _ in range(2)]
        for qit_, qib_ in qi:
            pg.op("pool", lambda e, qit_=qit_: e.memset(qit_[:], 0.0), writes=[qib_])
        csb = cx.sb([P, 256], F32, "csb"); csb_b = Buf("csb")
        ssc = cx.sb([P, 1], F32, "ssc"); ssc_b = Buf("ssc")
        rsc = cx.sb([P, 1], F32, "rsc"); rsc_b = Buf("rsc")
        wsb = cx.sb([P, 8], F32, "wsb"); wsb_b = Buf("wsb")

        pA32 = cx.ps([P, 512], F32, "pA"); pA_b = Buf("pA")
        pA = pA32[:, :].bitcast(BF16).rearrange("p (k t) -> p k t", k=KC)
        pQ = [(cx.ps([P, 512], F32, "pQ"), Buf("pQ")) for _ in range(2)]
        pL = [(cx.ps([P, 512], F32, "pL"), Buf("pL")) for _ in range(2)]
        pM = cx.ps([P, 512], F32, "pM"); pM_b = Buf("pM")
        pI = cx.ps([P, 512], F32, "pI"); pI_b = Buf("pI")

        def front(t):
            xi, xi_b = xin[t % 2]
            hn, hn_b = hn2[t % 2]
            hnT, hnT_b = hnT2[t % 2]
            pg.dma("sp", lambda e: e.dma_start(out=xi[:], in_=h_in[t * P:(t + 1) * P, :]), writes=[xi_b])
            rms_rstd(cx, xi[:], xi_b, junk[:], junk_b, ss[:, 0:1], ss_b, rstd[:, 0:1], rstd_b, D)
            pg.op("act", lambda e: e.activation(out=hn[:], in_=xi[:], func=AF.Copy, scale=rstd[:, 0:1]),
                  reads=[xi_b, rstd_b], writes=[hn_b])
            for k in range(KC):
                pg.op("pe", lambda e, k=k: e.transpose(out=pA[:, k, :], in_=hn[:, k * P:(k + 1) * P], identity=ident[:]),
                      reads=[hn_b, ident_b], writes=[pA_b], signal=(k == KC - 1))
            pg.op("dve", lambda e: e.tensor_copy(out=hnT[:], in_=pA), reads=[pA_b], writes=[hnT_b])

        def tile_body(t):
            hnT, hnT_b = hnT2[t % 2]
            ql, ql_b = qlat[t % 2]
            qit, qi_b = qi[t % 2]
            for half in range(2):
                pq, pq_b = pQ[half]
                for hl in range(4):
                    h = half * 4 + hl
                    for k in range(KC):
                        pg.op("pe", lambda e, pq=pq, hl=hl, h=h, k=k: e.matmul(pq[:, hl * P:(hl + 1) * P], lhsT=Win[:, k, h * P:(h + 1) * P], rhs=hnT[:, k, :],
                                                                              start=(k == 0), stop=(k == KC - 1)),
                              reads=[Win_b, hnT_b], writes=[pq_b], signal=(hl == 3 and k == KC - 1))
                if half == 0:
                    pg.op("act", lambda e, pq=pq: e.copy(out=qT[:, 0:4, :].rearrange("p h t -> p (h t)"), in_=pq[:, :]), reads=[pq_b], writes=[qT_b])
                else:
                    pg.op("dve", lambda e, pq=pq: e.tensor_copy(out=qT[:, 4:8, :].rearrange("p h t -> p (h t)"), in_=pq[:, :]), reads=[pq_b], writes=[qT_b])
            if t + 1 < NTT:
                front(t + 1)
            for cc in range(2):
                for half in range(2):
                    pl, pl_b = pL[half]
                    for hl in range(4):
                        h = half * 4 + hl
                        pg.op("pe", lambda e, pl=pl, hl=hl, h=h, cc=cc: e.matmul(
                            pl[:, hl * P:(hl + 1) * P], lhsT=Wuk[:, 0, h * 256 + cc * P:h * 256 + (cc + 1) * P], rhs=qT[:, h, :], start=True, stop=True),
                            reads=[Wuk_b, qT_b], writes=[pl_b], signal=(hl == 3))
                    if half == 0:
                        pg.op("act", lambda e, pl=pl, ql=ql, cc=cc: e.activation(out=ql[:, cc, 0:4, :].rearrange("p h t -> p (h t)"), in_=pl[:, :],
                                                                               func=AF.Copy, scale=float(128 ** -0.5)), reads=[pl_b], writes=[ql_b])
                    else:
                        pg.op("dve", lambda e, pl=pl, ql=ql, cc=cc: e.tensor_scalar(out=ql[:, cc, 4:8, :].rearrange("p h t -> p (h t)"), in0=pl[:, :],
                                                                                  scalar1=float(128 ** -0.5), scalar2=None, op0=ALU.mult), reads=[pl_b], writes=[ql_b])
            pg.dma("sp", lambda e, ql=ql, t=t: e.dma_start(out=scr["qlat"][t], in_=ql[:].rearrange("p a h t -> p (a h t)")), reads=[ql_b])
            for k in range(KC):
                pg.op("pe", lambda e, k=k: e.matmul(pM[:, 0:256], lhsT=hnT[:, k, :], rhs=Win[:, k, 1024:1280], start=(k == 0), stop=(k == KC - 1)),
                      reads=[Win_b, hnT_b], writes=[pM_b], signal=(k == KC - 1))
            pg.op("dve", lambda e: e.tensor_copy(out=csb[:], in_=pM[:, 0:256]), reads=[pM_b], writes=[csb_b])
            rms_rstd(cx, csb[:], csb_b, junk[:, 0:256], junk_b, ssc[:, 0:1], ssc_b, rsc[:, 0:1], rsc_b, 256)
            pg.op("dve", lambda e, t=t: e.scalar_tensor_tensor(out=ctok[:, t, 0:256], in0=csb[:], scalar=rsc[:, 0:1], in1=gkvB[:],
                                                               op0=ALU.mult, op1=ALU.mult), reads=[csb_b, rsc_b, gkvB_b], writes=[ctok_b])
            for cc in range(2):
                pg.op("pe", lambda e, cc=cc, t=t: e.transpose(out=pA[:, cc, :], in_=ctok[:, t, cc * P:(cc + 1) * P], identity=ident[:]),
                      reads=[ctok_b, ident_b], writes=[pA_b], signal=(cc == 1))
            pg.op("act", lambda e, t=t: e.copy(out=cT[:, :, t * P:(t + 1) * P], in_=pA[:, 0:2, :]), reads=[pA_b], writes=[cT_b])
            for p in range(4):
                for k in range(KC):
                    pg.op("pe", lambda e, p=p, k=k: e.matmul(pI[:, p * P:(p + 1) * P], lhsT=Win[:, k, 1280 + p * P:1280 + (p + 1) * P], rhs=hnT[:, k, :],
                                                              start=(k == 0), stop=(k == KC - 1)),
                          reads=[Win_b, hnT_b], writes=[pI_b], signal=(p == 3 and k == KC - 1))
            pg.op("act", lambda e, qit=qit: e.copy(out=qit[0:64, :, :].rearrange("p (a two) t -> p a two t", two=2)[:, :, 0, :],
                                                   in_=pI[0:64, :].rearrange("p (a t) -> p a t", a=4)), reads=[pI_b], writes=[qi_b])
            pg.op("dve", lambda e, qit=qit: e.tensor_copy(out=qit[64:128, :, :].rearrange("p (a two) t -> p a two t", two=2)[:, :, 1, :],
                                                          in_=pI[64:128, :].rearrange("p (a t) -> p a t", a=4)), reads=[pI_b], writes=[qi_b])
            pg.dma("sp", lambda e, qit=qit, t=t: e.dma_start(out=scr["qi"][t], in_=qit[:].rearrange("p a t -> p (a t)")), reads=[qi_b])
            for k in range(KC):
                pg.op("pe", lambda e, k=k: e.matmul(pM[:, 256:384], lhsT=Wk2[:, k, :], rhs=hnT[:, k, :], start=(k == 0), stop=(k == KC - 1)),
                      reads=[Wk2_b, hnT_b], writes=[pM_b], signal=(k == KC - 1))
            pg.op("dve", lambda e, t=t: e.tensor_copy(out=kidx2[:, t * P:(t + 1) * P], in_=pM[:, 256:384]), reads=[pM_b], writes=[kidx2_b])
            for k in range(KC):
                pg.op("pe", lambda e, k=k: e.matmul(pM[:, 384:392], lhsT=hnT[:, k, :], rhs=Win[:, k, 1856:1864], start=(k == 0), stop=(k == KC - 1)),
                      reads=[Win_b, hnT_b], writes=[pM_b], signal=(k == KC - 1))
            pg.op("dve", lambda e: e.tensor_copy(out=wsb[:], in_=pM[:, 384:392]), reads=[pM_b], writes=[wsb_b])
            pg.op("act", lambda e, t=t: e.activation(out=wabs[:, t, :], in_=wsb[:], func=AF.Abs, scale=IDX_C0),
                  reads=[wsb_b], writes=[wabs_b])
            pg.op("act", lambda e, t=t: e.activation(out=sgn[:, t, :], in_=wsb[:], func=AF.Sign), reads=[wsb_b], writes=[sgn_b])
        front(0)
        for t in range(NTT):
            tile_body(t)
        pg.barrier()


NIT = 13
TOPK = 256


def phase_dsa_attn(nc, pg, NTT, W, consts, caches, scr):
    kidx2, kidx2_b = caches["kidx2"]
    cT, cT_b = caches["cT"]
    ctok, ctok_b = caches["ctok"]
    wabs, wabs_b = caches["wabs"]
    sgn, sgn_b = caches["sgn"]
    SMAX = NTT * P
    AX = mybir.AxisListType.X
    with contextlib.ExitStack() as stack:
        cx = Ctx(nc, pg, stack)
        Wuv = cx.sb([P, 2, 8, P], BF16, "Wuv"); Wuv_b = Buf("Wuv")
        ident = cx.sb([P, P], BF16, "ident"); ident_b = Buf("ident")
        identf = cx.sb([P, P], F32, "identf"); identf_b = Buf("identf")
        m0 = cx.sb([P, P], F32, "m0"); m0_b = Buf("m0")
        mdiag = cx.sb([P, P], F32, "mdiag"); mdiag_b = Buf("mdiag")
        scores = cx.sb([P, max(SMAX, 2048)], F32, "scores"); sc_b = Buf("scores")
        biasf = scores[:, 0:1024]; biasf_b = Buf("biasf")
        b15f = scores[:, 1024:2048]; b15f_b = Buf("b15f")
        bias = cx.sb([P, 3, 2, 1024], BF16, "bias"); bias_b = Buf("bias")
        mone = cx.sb([P, 8], F32, "mone"); mone_b = Buf("mone")
        pg.op("pool", lambda e: e.memset(mone[:], -1.0), writes=[mone_b])
        pg.dma("sp", lambda e: e.dma_start(out=identf[:], in_=W["identf"][:, :]), writes=[identf_b])
        pg.dma("sp", lambda e: e.dma_start(out=m0[:], in_=consts["m0"][:, :]), writes=[m0_b])
        pg.dma("sp", lambda e: e.dma_start(out=mdiag[:], in_=consts["mdiag"][:, :]), writes=[mdiag_b])
        pg.dma("sp", lambda e: e.dma_start(out=b15f, in_=consts["b15"][:, :]), writes=[b15f_b])
        pg.op("dve", lambda e: e.tensor_copy(out=ident[:], in_=identf[:]), reads=[identf_b], writes=[ident_b])
        for wi in range(3):
            pg.dma("sp", lambda e, wi=wi: e.dma_start(out=biasf, in_=consts["biasT"][wi]), writes=[biasf_b])
            pg.op("dve", lambda e: e.tensor_tensor(out=biasf, in0=biasf, in1=b15f, op=ALU.subtract), reads=[biasf_b, b15f_b], writes=[biasf_b])
            pg.op("dve", lambda e, wi=wi: e.tensor_copy(out=bias[:, wi, 0, :], in_=biasf), reads=[biasf_b], writes=[bias_b])
            pg.op("dve", lambda e, wi=wi: e.tensor_tensor(out=biasf, in0=biasf, in1=bias[:, wi, 0, :], op=ALU.subtract), reads=[biasf_b, bias_b], writes=[biasf_b])
            pg.op("dve", lambda e, wi=wi: e.tensor_copy(out=bias[:, wi, 1, :], in_=biasf), reads=[biasf_b], writes=[bias_b])
        wuv_v = W["dsa_w_uv"].rearrange("h (cc p) d -> p cc h d", p=P)
        stage = [(biasf, biasf_b), (b15f, b15f_b)]
        for cc in range(2):
            st, stb = stage[cc]
            pg.dma("sp", lambda e, st=st, cc=cc: e.dma_start(out=st.rearrange("p (h d) -> p h d", h=8), in_=wuv_v[:, cc, :, :]), writes=[stb])
            pg.op("dve", lambda e, st=st, cc=cc: e.tensor_copy(out=Wuv[:, cc, :, :].rearrange("p h d -> p (h d)"), in_=st), reads=[stb], writes=[Wuv_b])

        nm = cx.sb([P, SMAX], BF16, "nm"); nm_b = Buf("nm")
        nmT = cx.sb([P, NTT, P], BF16, "nmT"); nmT_b = Buf("nmT")
        qlat = [(cx.sb([P, 2, 8, P], BF16, "qlat"), Buf("qlat")) for _ in range(2)]
        qi = [(cx.sb([P, 8, P], BF16, "qi"), Buf("qi")) for _ in range(2)]
        dsg = [(cx.sb([P, 8, P], BF16, "dsg"), [Buf("dsg%d" % h) for h in range(8)]) for _ in range(2)]
        NA = 2
        A = [(cx.sb([P, 1024], BF16, "A"), Buf("A")) for _ in range(NA)]
        NKB = (SMAX + 511) // 512
        mxs = cx.sb([P, NKB], F32, "mxs"); mxs_b = Buf("mxs")
        mns = cx.sb([P, NKB], F32, "mns"); mns_b = Buf("mns")
        pow2 = cx.sb([P, 32], F32, "pow2"); pow2_b = Buf("pow2")
        ds = cx.sb([P, 32], F32, "ds"); ds_b = Buf("ds")
        tq = cx.sb([P, 1], F32, "tq"); tq_b = Buf("tq")
        pg.dma("sp", lambda e: e.dma_start(out=pow2[:], in_=consts["pow2"][:, :]), writes=[pow2_b])
        NPT = 5
        PT = [(cx.sb([P, 512], BF16, "PT"), Buf("PT")) for _ in range(NPT)]
        lo = cx.sb([P, 1], F32, "lo"); lo_b = Buf("lo")
        wd = cx.sb([P, 1], F32, "wd"); wd_b = Buf("wd")
        mid = cx.sb([P, 1], F32, "mid"); mid_b = Buf("mid")
        cnt = cx.sb([P, 1], F32, "cnt"); cnt_b = Buf("cnt")
        pw = cx.sb([P, 1], F32, "pw"); pw_b = Buf("pw")
        den = cx.sb([P, 8], F32, "den"); den_b = Buf("den")
        rden = cx.sb([P, 8], F32, "rden"); rden_b = Buf("rden")
        Un = cx.sb([P, 8, 256], BF16, "Un"); Un_b = Buf("Un")
        UnT = cx.sb([P, 16, P], BF16, "UnT"); UnT_b = Buf("UnT")
        oT = [(cx.sb([P, 8, P], BF16, "oT"), Buf("oT")) for _ in range(2)]

        def bank(nm_):
            t_ = cx.ps([P, 512], F32, nm_)
            return (t_, Buf(nm_), t_[:, :].bitcast(BF16).rearrange("p (k t) -> p k t", k=8))
        def bank2(nm_):
            t_ = cx.ps([P, 1024], F32, nm_)
            b0 = (t_[:, 0:512], Buf(nm_ + "a"), t_[:, 0:512].bitcast(BF16).rearrange("p (k t) -> p k t", k=8))
            b1 = (t_[:, 512:1024], Buf(nm_ + "b"), t_[:, 512:1024].bitcast(BF16).rearrange("p (k t) -> p k t", k=8))
            return t_, b0, b1
        pXX, pX0, pX1 = bank2("pXX")
        pUU, pU0, pU1 = bank2("pUU")
        pS, pU2, pU3, pDn = [bank(n_) for n_ in ("pS", "pU2", "pU3", "pDn")]
        X2 = [(pXX, pX0, pX1), (pUU, pU0, pU1)]
        SCB = [pS, pU2, pU3, pDn]
        TB = [pU3, pDn]
        LB = [pX0, pX1, pS]
        UB = [pU0, pU1, pU2, pU3]

        def build_dsg(j):
            dt_, db_ = dsg[j % 2]
            for h in range(8):
                pg.op("dve", lambda e, h=h, j=j, dt_=dt_: e.tensor_scalar(out=dt_[:, h, :], in0=ident[:], scalar1=sgn[:, j, h:h + 1], scalar2=None, op0=ALU.mult),
                      reads=[ident_b, sgn_b], writes=[db_[h]])

        def load_q(j):
            ql, ql_b = qlat[j % 2]
            qit, qi_b = qi[j % 2]
            pg.dma("sp", lambda e, qit=qit, j=j: e.dma_start(out=qit[:].rearrange("p a t -> p (a t)"), in_=scr["qi"][j]), writes=[qi_b])
            pg.dma("sp", lambda e, ql=ql, j=j: e.dma_start(out=ql[:].rearrange("p a h t -> p (a h t)"), in_=scr["qlat"][j]), writes=[ql_b])

        def stage_I(j):
            S = (j + 1) * P
            qit, qi_b = qi[j % 2]
            dt_, db_ = dsg[j % 2]
            nkb = (S + 511) // 512
            npair = (nkb + 1) // 2
            units = [(kp, h) for kp in range(npair) for h in range(8)]
            DX = 1
            n = len(units)

            def cols(kp):
                c0 = kp * 1024
                return c0, min(1024, S - c0)

            def emit_X(i):
                kp, h = units[i]
                c0, cw = cols(kp)
                xx, xa, xb = X2[i % 2]
                for q_, xq in enumerate((xa, xb)):
                    w_ = min(512, cw - q_ * 512)
                    if w_ <= 0:
                        continue
                    pg.op("pe", lambda e, xq=xq, h=h, c0=c0, q_=q_, w_=w_: e.matmul(
                        xq[0][:, 0:w_], lhsT=qit[:, h, :], rhs=kidx2[:, c0 + q_ * 512:c0 + q_ * 512 + w_], start=True, stop=True),
                        reads=[qi_b, kidx2_b], writes=[xq[1]])

            def emit_R(i):
                kp, h = units[i]
                c0, cw = cols(kp)
                xx, xa, xb = X2[i % 2]
                At, A_b = A[i % NA]
                rb = [xa[1]] + ([xb[1]] if cw > 512 else [])
                pg.op("act", lambda e, xx=xx, At=At, cw=cw, h=h: e.activation(out=At[:, 0:cw], in_=xx[:, 0:cw], func=AF.Relu, scale=wabs[:, j, h:h + 1]),
                      reads=rb + [wabs_b], writes=[A_b])
                for q_ in range(2):
                    w_ = min(512, cw - q_ * 512)
                    if w_ <= 0:
                        continue
                    sc, sc_pb, _ = SCB[(2 * kp + q_) % 4]
                    pg.op("pe", lambda e, At=At, w_=w_, h=h, sc=sc, q_=q_: e.matmul(sc[:, 0:w_], lhsT=dt_[:, h, :], rhs=At[:, q_ * 512:q_ * 512 + w_],
                                                                                  start=(h == 0), stop=(h == 7)),
                          reads=[A_b, db_[h]], writes=[sc_pb], signal=(h == 7))
                    if h == 7:
                        kb = 2 * kp + q_
                        cc0 = c0 + q_ * 512
                        pg.op("dve", lambda e, cc0=cc0, w_=w_, sc=sc, kb=kb: e.tensor_scalar(out=scores[:, cc0:cc0 + w_], in0=sc[:, 0:w_], scalar1=1.0, scalar2=None,
                                                                                         op0=ALU.mult, op1=ALU.max, accum_out=mxs[:, kb:kb + 1]),
                              reads=[sc_pb], writes=[sc_b, mxs_b])
                        pg.op("dve", lambda e, cc0=cc0, w_=w_, kb=kb: e.tensor_reduce(out=mns[:, kb:kb + 1], in_=scores[:, cc0:cc0 + w_], op=ALU.min, axis=AX),
                              reads=[sc_b], writes=[mns_b])

            for i in range(n + DX):
                if i < n:
                    emit_X(i)
                if i - DX >= 0:
                    emit_R(i - DX)
            pg.op("pool", lambda e: e.tensor_tensor(out=scores[:, 0:P], in0=scores[:, 0:P], in1=m0[:], op=ALU.add), reads=[sc_b, m0_b, mns_b], writes=[sc_b])
            if j >= 1:
                pg.op("pool", lambda e: e.tensor_tensor(out=scores[:, j * P:(j + 1) * P], in0=scores[:, j * P:(j + 1) * P], in1=mdiag[:], op=ALU.add),
                      reads=[sc_b, mdiag_b], writes=[sc_b])
            pg.op("dve", lambda e: e.tensor_reduce(out=lo[:], in_=mns[:, 0:nkb], op=ALU.min, axis=AX), reads=[mns_b], writes=[lo_b])
            pg.op("dve", lambda e: e.tensor_reduce(out=wd[:], in_=mxs[:, 0:nkb], op=ALU.max, axis=AX), reads=[mxs_b], writes=[wd_b])
            pg.op("dve", lambda e: e.tensor_tensor(out=wd[:], in0=wd[:], in1=lo[:], op=ALU.subtract), reads=[wd_b, lo_b], writes=[wd_b])
            pg.op("dve", lambda e: e.tensor_scalar(out=tq[:], in0=wd[:], scalar1=0.001, scalar2=1e-6, op0=ALU.mult, op1=ALU.add), reads=[wd_b], writes=[tq_b])
            pg.op("dve", lambda e: e.tensor_tensor(out=lo[:], in0=lo[:], in1=tq[:], op=ALU.subtract), reads=[lo_b, tq_b], writes=[lo_b])
            pg.op("dve", lambda e: e.tensor_scalar(out=wd[:], in0=wd[:], scalar1=1.002, scalar2=2e-6, op0=ALU.mult, op1=ALU.add), reads=[wd_b], writes=[wd_b])
            pg.op("dve", lambda e: e.tensor_scalar(out=ds[:], in0=pow2[:], scalar1=wd[:, 0:1], scalar2=None, op0=ALU.mult), reads=[wd_b, pow2_b], writes=[ds_b])
            pg.op("dve", lambda e: e.tensor_tensor(out=mid[:], in0=lo[:], in1=ds[:, 0:1], op=ALU.add), reads=[lo_b, ds_b], writes=[mid_b])

        def stage_B(j):
            S = (j + 1) * P
            for it in range(NIT):
                pg.op("dve", lambda e: e.tensor_scalar(out=nm[:, 0:S], in0=scores[:, 0:S], scalar1=mid[:, 0:1], scalar2=None, op0=ALU.is_ge, op1=ALU.add,
                                                       accum_out=cnt[:, 0:1]), reads=[sc_b, mid_b], writes=[nm_b, cnt_b])
                pg.op("dve", lambda e, it=it: e.tensor_scalar(out=tq[:], in0=cnt[:], scalar1=TOPK - 0.5, scalar2=ds[:, it:it + 1], op0=ALU.is_ge, op1=ALU.mult),
                      reads=[cnt_b, ds_b], writes=[tq_b])
                pg.op("dve", lambda e, it=it: e.scalar_tensor_tensor(out=mid[:], in0=tq[:], scalar=ds[:, it + 1:it + 2], in1=mid[:], op0=ALU.subtract, op1=ALU.add),
                      reads=[tq_b, ds_b, mid_b], writes=[mid_b])
            pg.op("dve", lambda e: e.tensor_scalar(out=nm[:, 0:S], in0=scores[:, 0:S], scalar1=ds[:, NIT:NIT + 1], scalar2=mid[:, 0:1], op0=ALU.add, op1=ALU.is_ge),
                  reads=[sc_b, ds_b, mid_b], writes=[nm_b])

        def stage_T(j):
            ib = 0
            for k0 in range(0, j + 1, 8):
                kn = min(8, j + 1 - k0)
                tb, tb_b, tbv = TB[ib % 2]
                ib += 1
                for kk in range(kn):
                    pg.op("pe", lambda e, kk=kk, k0=k0, tbv=tbv: e.transpose(out=tbv[:, kk, :], in_=nm[:, (k0 + kk) * P:(k0 + kk + 1) * P], identity=ident[:]),
                          reads=[nm_b, ident_b], writes=[tb_b], signal=(kk == kn - 1))
                pg.op("act", lambda e, k0=k0, kn=kn, tbv=tbv: e.copy(out=nmT[:, k0:k0 + kn, :], in_=tbv[:, 0:kn, :]), reads=[tb_b], writes=[nmT_b])

        def stage_W(j):
            ql, ql_b = qlat[j % 2]
            units = [(kt, half) for kt in range(j + 1) for half in range(2)]
            n = len(units)
            DL = 2

            def emit_QK(i):
                kt, half = units[i]
                wi = None
                if kt == j:
                    wi = 0
                elif kt == j - 1 and j >= 2:
                    wi = 1
                elif j == 1 and kt == 0:
                    wi = 2
                px, px_b, _ = LB[i % 3]
                for cc in range(2):
                    pg.op("pe", lambda e, px=px, cc=cc, kt=kt, half=half: e.matmul(
                        px[:, :], lhsT=cT[:, cc, kt * P:(kt + 1) * P], rhs=ql[:, cc, half * 4:half * 4 + 4, :].rearrange("p h t -> p (h t)"),
                        start=(cc == 0), stop=(cc == 1 and wi is None)), reads=[cT_b, ql_b], writes=[px_b], signal=(cc == 1 and wi is None))
                if wi is not None:
                    for hl in range(2):
                        pg.op("pe", lambda e, px=px, wi=wi, hl=hl, half=half: e.matmul(px[:, :], lhsT=ident[:], rhs=bias[:, wi, hl, half * 512:(half + 1) * 512],
                                                                                    start=False, stop=(hl == 1)), reads=[ident_b, bias_b], writes=[px_b], signal=(hl == 1))

            def emit_PV(i):
                kt, half = units[i]
                px, px_b, _ = LB[i % 3]
                Pt, Pt_b = PT[i % NPT]
                pg.op("act", lambda e, px=px, Pt=Pt: e.activation(out=Pt[:], in_=px[:, :], func=AF.Exp), reads=[px_b], writes=[Pt_b])
                pg.op("pool", lambda e, Pt=Pt, kt=kt: e.tensor_tensor(out=Pt[:].rearrange("p (h t) -> p h t", h=4), in0=Pt[:].rearrange("p (h t) -> p h t", h=4),
                                                                    in1=nmT[:, kt:kt + 1, :].broadcast_to([P, 4, P]), op=ALU.mult),
                      reads=[Pt_b, nmT_b], writes=[Pt_b])
                for hl in range(4):
                    h = half * 4 + hl
                    pu, pu_b, _ = UB[h // 2]
                    first = (kt == 0 and h % 2 == 0)
                    pg.op("pe", lambda e, pu=pu, h=h, hl=hl, Pt=Pt, kt=kt, first=first: e.matmul(
                        pu[:, (h % 2) * 256:(h % 2 + 1) * 256], lhsT=Pt[:, hl * P:(hl + 1) * P], rhs=ctok[:, kt, 0:256], start=first, stop=(kt == j),
                        skip_group_check=True), reads=[Pt_b, ctok_b], writes=[pu_b], signal=False)
                    firstd = (kt == 0 and h == 0)
                    pg.op("pe", lambda e, h=h, hl=hl, Pt=Pt, kt=kt, firstd=firstd: e.matmul(
                        pDn[0][:, 2 * h:2 * h + 2], lhsT=Pt[:, hl * P:(hl + 1) * P], rhs=ctok[:, kt, 256:258], start=firstd, stop=(kt == j),
                        skip_group_check=True), reads=[Pt_b, ctok_b], writes=[pDn[1]], signal=(hl == 3))

            for i in range(n + DL):
                if i < n:
                    emit_QK(i)
                if i - DL >= 0:
                    emit_PV(i - DL)

        def stage_Z(j):
            pg.op("act", lambda e: e.copy(out=den[:], in_=pDn[0][:, 0:16].rearrange("p (h two) -> p h two", two=2)[:, :, 0]), reads=[pDn[1]], writes=[den_b])
            pg.op("pool", lambda e: e.tensor_tensor(out=rden[:], in0=den[:], in1=mone[:], op=ALU.pow), reads=[den_b, mone_b], writes=[rden_b])
            for h in range(8):
                pu, pu_b, _ = UB[h // 2]
                pg.op("act", lambda e, pu=pu, h=h: e.activation(out=Un[:, h, :], in_=pu[:, (h % 2) * 256:(h % 2 + 1) * 256], func=AF.Copy, scale=rden[:, h:h + 1]),
                      reads=[pu_b, rden_b], writes=[Un_b])
            for g in range(2):
                tb, tb_b, tbv = (pX0, pX1)[g]
                for kk in range(8):
                    idx = g * 8 + kk
                    h, cc = idx // 2, idx % 2
                    pg.op("pe", lambda e, kk=kk, h=h, cc=cc, tbv=tbv: e.transpose(out=tbv[:, kk, :], in_=Un[:, h, cc * P:(cc + 1) * P], identity=ident[:]),
                          reads=[Un_b, ident_b], writes=[tb_b], signal=(kk == 7))
                pg.op("act", lambda e, g=g, tbv=tbv: e.copy(out=UnT[:, g * 8:(g + 1) * 8, :], in_=tbv[:, :, :]), reads=[tb_b], writes=[UnT_b])
            ot, ot_b = oT[j % 2]
            for half in range(2):
                px, px_b, _ = (pS, pX0)[half]
                for hl in range(4):
                    h = half * 4 + hl
                    for cc in range(2):
                        pg.op("pe", lambda e, px=px, hl=hl, h=h, cc=cc: e.matmul(px[:, hl * P:(hl + 1) * P], lhsT=Wuv[:, cc, h, :], rhs=UnT[:, h * 2 + cc, :],
                                                                              start=(cc == 0), stop=(cc == 1)), reads=[Wuv_b, UnT_b], writes=[px_b],
                              signal=(hl == 3 and cc == 1))
                pg.op("act", lambda e, px=px, ot=ot, half=half: e.copy(out=ot[:, half * 4:half * 4 + 4, :].rearrange("p h t -> p (h t)"), in_=px[:, :]),
                      reads=[px_b], writes=[ot_b])
            pg.dma("sp", lambda e, ot=ot, j=j: e.dma_start(out=scr["oT"][j], in_=ot[:].rearrange("p h t -> p (h t)")), reads=[ot_b])

        build_dsg(0)
        load_q(0)
        stage_I(0)
        stage_B(0)
        stage_T(0)
        for j in range(NTT):
            if j + 1 < NTT:
                build_dsg(j + 1)
                load_q(j + 1)
                stage_I(j + 1)
            stage_W(j)
            if j + 1 < NTT:
                stage_B(j + 1)
            stage_Z(j)
            if j + 1 < NTT:
                stage_T(j + 1)
        pg.barrier()


def phase_dsa_out(nc, pg, NTT, h_in, h_out, W, scr):
    with contextlib.ExitStack() as stack:
        cx = Ctx(nc, pg, stack)
        Wo = cx.sb([P, 8, D], BF16, "Wo"); Wo_b = Buf("Wo")
        stage = [(cx.sb([P, 1024], F32, "stg"), Buf("stg")) for _ in range(5)]
        load_weight(cx, Wo, Wo_b, W["dsa_w_out"], 8, D, gain=None, stage=stage)
        xin = [(cx.sb([P, D], F32, "xin"), Buf("xin")) for _ in range(3)]
        ot = [(cx.sb([P, 8, P], BF16, "ot"), Buf("ot")) for _ in range(3)]
        po = [(cx.ps([P, 512], F32, "po"), Buf("po")) for _ in range(4)]
        ic = 0
        for t in range(1, NTT):
            xi, xi_b = xin[t % 3]
            o_, o_b = ot[t % 3]
            pg.dma("sp", lambda e, xi=xi, t=t: e.dma_start(out=xi[:], in_=h_in[t * P:(t + 1) * P, :]), writes=[xi_b])
            pg.dma("sp", lambda e, o_=o_, t=t: e.dma_start(out=o_[:].rearrange("p h t -> p (h t)"), in_=scr["oT"][t]), writes=[o_b])
            for n in range(2):
                pp, pp_b = po[ic % 4]
                ic += 1
                for h in range(8):
                    pg.op("pe", lambda e, pp=pp, h=h, n=n, o_=o_: e.matmul(pp[:, :], lhsT=o_[:, h, :], rhs=Wo[:, h, n * 512:(n + 1) * 512], start=(h == 0), stop=(h == 7)),
                          reads=[o_b, Wo_b], writes=[pp_b], signal=(h == 7))
                pg.op("dve", lambda e, pp=pp, n=n, xi=xi: e.tensor_tensor(out=xi[:, n * 512:(n + 1) * 512], in0=xi[:, n * 512:(n + 1) * 512], in1=pp[:, :], op=ALU.add),
                      reads=[pp_b, xi_b], writes=[xi_b])
            pg.dma("sp", lambda e, xi=xi, t=t: e.dma_start(out=h_out[t * P:(t + 1) * P, :], in_=xi[:]), reads=[xi_b])
        pg.barrier()


import math


def _rel_bucket(rel):
    rel = np.asarray(rel, np.int64)
    nb = 16
    max_exact = 8
    ret = np.where(rel > 0, nb, 0)
    n = np.abs(rel)
    nf = np.maximum(n, 1).astype(np.float32)
    large = max_exact + (np.log(nf / np.float32(max_exact)) / np.float32(math.log(128 / max_exact))
                         * np.float32(nb - max_exact)).astype(np.int32)
    large = np.minimum(large, nb - 1)
    return ret + np.where(n < max_exact, n, large)


def _index_consts():
    c = {}
    Dm = np.zeros((128, 2, 128), np.float32)
    cp = np.arange(128)[:, None]
    cc = np.arange(128)[None, :]
    Dm[:, 0, :] = np.where((cp // 64 == cc // 64) & (cp > cc), -1.0 / 16, 0.0)
    Dm[:, 1, :] = np.where((cp >= 48) & (cp < 64) & (cc >= 48) & (cc < 64) & (cp > cc), -1.0 / 16, 0.0)
    Ind = np.zeros((128, 2, 2), np.float32)
    Ind[np.arange(128), 0, np.arange(128) // 64] = -1.0 / 16
    Ind[48:64, 1, 0] = -1.0 / 16
    c["c_Dm"] = Dm
    c["c_Ind"] = Ind
    m0 = np.full((128, 128), NEG, np.float32)
    m0[:, 48:64] = 0
    md = np.zeros((128, 128), np.float32)
    md[0:64, 64:128] = NEG
    c["c_m0"] = m0
    c["c_mdiag"] = md
    c["identf"] = np.eye(128, dtype=np.float32)
    c["c_pow2"] = np.ascontiguousarray(np.broadcast_to((2.0 ** -(np.arange(32) + 1.0)).astype(np.float32)[None, :], (128, 32)))
    return c


def _colchunk(g):
    return np.ascontiguousarray(np.asarray(g, np.float32).reshape(-1, 128).T)


def _prep_shared(inp):
    f = lambda a: np.ascontiguousarray(np.asarray(a, dtype=np.float32))
    sh = _index_consts()
    gainT = np.zeros((128, 48), np.float32)
    gainT[:, 0:8] = _colchunk(inp["norm_mix"][0])
    gainT[:, 8:16] = _colchunk(inp["norm_ffn"][0])
    gainT[:, 16:24] = _colchunk(inp["norm_mix"][1])
    gainT[:, 24:32] = _colchunk(inp["norm_ffn"][1])
    gainT[:, 32:40] = _colchunk(inp["gla_g_out"][0])
    sh["gainT"] = gainT
    sh["ffn_w_in0"] = f(inp["ffn_w_in"][0]); sh["ffn_w_out0"] = f(inp["ffn_w_out"][0])
    sh["ffn_w_in1"] = f(inp["ffn_w_in"][1]); sh["ffn_w_out1"] = f(inp["ffn_w_out"][1])
    sh["gfinB"] = np.ascontiguousarray(np.broadcast_to(f(inp["norm_final"])[None, :], (128, D)))
    sh["gla_w_in"] = f(inp["gla_w_in"][0]); sh["gla_w_a1"] = f(inp["gla_w_a1"][0])
    sh["gla_w_a2aug"] = np.ascontiguousarray(np.concatenate([f(inp["gla_w_a2"][0]), f(inp["gla_b_a"][0])[None, :]], 0))
    sh["gla_w_out"] = f(inp["gla_w_out"][0])
    sh["meta"] = f(inp["meta"])
    sh["dsa_w_in"] = f(inp["dsa_w_in"][0])
    sh["dsa_w_ukT"] = np.ascontiguousarray(f(inp["dsa_w_uk"][0]).transpose(2, 0, 1)).reshape(128, 2048)
    sh["dsa_w_uv"] = f(inp["dsa_w_uv"][0])
    sh["dsa_w_out"] = f(inp["dsa_w_out"][0])
    sh["gkvB"] = np.ascontiguousarray(np.broadcast_to(f(inp["dsa_g_kv"][0])[None, :], (128, 256)))
    rb = f(inp["rel_bias"])
    s = np.arange(128)[:, None]
    t = np.arange(128)[None, :]
    bt = np.zeros((3, 128, 8, 128), np.float32)
    for wi, off in enumerate((0, -128, -64)):
        bt[wi] = rb[_rel_bucket(s - t + off)].transpose(0, 2, 1)
    sh["c_biasT"] = np.ascontiguousarray(bt.reshape(3, 128, 1024))
    sh["c_b15"] = np.ascontiguousarray(np.broadcast_to(rb[15][None, :, None], (128, 8, 128)).reshape(128, 1024))
    return sh


_NC_CACHE = {}


def kernel(**inputs):
    x = np.asarray(inputs["x"], dtype=np.float32)
    B, SEQ, _ = x.shape
    sh = _prep_shared(inputs)
    key = (SEQ,)
    if key not in _NC_CACHE:
        _NC_CACHE[key] = build(SEQ, "ABCDFE")
    nc = _NC_CACHE[key]
    in_maps = []
    for b in range(B):
        m = dict(sh)
        m["x"] = np.ascontiguousarray(x[b])
        in_maps.append(m)
    res = run_bass_kernel_spmd(nc, in_maps, core_ids=list(range(B)))
    return np.stack([np.asarray(r["out"], dtype=np.float32) for r in res.results], 0)
```
